# Optimizing a Trainium2 kernel written in Bass

```python
import math
import jax
import jax.numpy as jnp
from jax import lax
import numpy as np

D_MODEL = 1024
BATCH = 32
SEQ = 2048
DEPTH = 2

GRID_W = 64
CTX_LEN = 256
EPS = 1e-6

D_MIX = D_MODEL
W_CONV = D_MIX // 4
W_POOL = D_MIX // 4
W_SSM = D_MIX // 4
W_MLA = D_MIX - W_CONV - W_POOL - W_SSM

CONV_K = 31

POOL_WINDOWS = (2, 4, 8, 16)
POOL_CH = W_POOL // len(POOL_WINDOWS)

SSM_CH = 16
SSM_GROUPS = W_SSM // SSM_CH
SSM_STATE = 64

MLA_V = 64
MLA_HEADS = W_MLA // MLA_V
MLA_NOPE = 64
MLA_ROPE = 32
MLA_QK = MLA_NOPE + MLA_ROPE
MLA_Q_RANK = 192
MLA_KV_RANK = 128
ROPE_AXIS = MLA_ROPE // 2
ROPE_BASE = 10000.0
Q_BLOCK = 128

N_EXPERTS = 32
TOP_K = 4
D_EXPERT = D_MODEL
SWIGLU_LIMIT = 7.0
SWIGLU_ALPHA = 1.702
MOE_BLOCK = 512

OFF_POOL = 2 * W_CONV
OFF_Q = OFF_POOL + W_POOL
OFF_SSM = OFF_Q + MLA_Q_RANK
OFF_KV = OFF_SSM + W_SSM
OFF_KPE = OFF_KV + MLA_KV_RANK
N_IN = OFF_KPE + MLA_ROPE
SPLIT_AT = (OFF_POOL, OFF_Q, OFF_SSM, OFF_KV, OFF_KPE)

kernel_name = 'hybrid_dit_conv_pool_s5_mla_moe'


def rmsnorm(x, g):
    xf = x.astype(jnp.float32)
    y = xf * lax.rsqrt(jnp.mean(xf * xf, axis=-1, keepdims=True) + EPS)
    return y.astype(x.dtype) * g


def layernorm(x, g, b):
    xf = x.astype(jnp.float32)
    mu = jnp.mean(xf, axis=-1, keepdims=True)
    var = jnp.mean(jnp.square(xf - mu), axis=-1, keepdims=True)
    return ((xf - mu) * lax.rsqrt(var + EPS)).astype(x.dtype) * g + b


def conv_module(p, dw, dw_b, ln_g, ln_b, pw):
    val, gate = jnp.split(p, 2, axis=-1)
    u = val * jax.nn.sigmoid(gate)
    u = lax.conv_general_dilated(
        u, dw[:, None, :], window_strides=(1,),
        padding=[(CONV_K // 2, CONV_K // 2)],
        dimension_numbers=('NWC', 'WIO', 'NWC'),
        feature_group_count=u.shape[-1]) + dw_b
    u = jax.nn.silu(layernorm(u, ln_g, ln_b))
    return u @ pw


def multiscale_pool(u, w, scale):
    bsz, length, _ = u.shape
    n_g = len(POOL_WINDOWS)
    ug = u.reshape(bsz, length, n_g, POOL_CH)
    cs = jnp.concatenate([jnp.zeros((bsz, 1, n_g, POOL_CH), jnp.float32),
                          jnp.cumsum(ug.astype(jnp.float32), axis=1)], axis=1)
    t = jnp.arange(length)
    groups = []
    for g, win in enumerate(POOL_WINDOWS):
        lo = jnp.maximum(t - win // 2, 0)
        hi = jnp.minimum(t + win - 1 - win // 2, length - 1)
        csg = cs[:, :, g]
        cnt = (hi - lo + 1).astype(jnp.float32)[None, :, None]
        mean = (csg[:, hi + 1] - csg[:, lo]) / cnt
        groups.append(mean.astype(u.dtype) - ug[:, :, g])
    pooled = jnp.stack(groups, axis=2)
    y = jnp.einsum('blgc,gcd->blgd', pooled, w)
    return y.reshape(bsz, length, W_POOL) * scale


def s5_discretise(a_re, a_im, log_dt, b_re, b_im):
    a_re = jnp.minimum(a_re.astype(jnp.float32), -1e-4)
    a_im = a_im.astype(jnp.float32)
    dt = jnp.exp(log_dt.astype(jnp.float32))[:, None]
    mag = jnp.exp(a_re * dt)
    ab_re = mag * jnp.cos(a_im * dt)
    ab_im = mag * jnp.sin(a_im * dt)
    den = a_re * a_re + a_im * a_im
    f_re = ((ab_re - 1.0) * a_re + ab_im * a_im) / den
    f_im = (ab_im * a_re - (ab_re - 1.0) * a_im) / den
    b_re = b_re.astype(jnp.float32)
    b_im = b_im.astype(jnp.float32)
    bb_re = f_re[..., None] * b_re - f_im[..., None] * b_im
    bb_im = f_re[..., None] * b_im + f_im[..., None] * b_re
    return ab_re, ab_im, bb_re, bb_im


def _complex_affine_combine(e1, e2):
    a1r, a1i, b1r, b1i = e1
    a2r, a2i, b2r, b2i = e2
    return (a1r * a2r - a1i * a2i, a1r * a2i + a1i * a2r,
            a2r * b1r - a2i * b1i + b2r, a2r * b1i + a2i * b1r + b2i)


def diag_scan(ab_re, ab_im, bb_re, bb_im, u, h0_re, h0_im, reverse):
    bu_re = jnp.einsum('blgc,gpc->blgp', u, bb_re)
    bu_im = jnp.einsum('blgc,gpc->blgp', u, bb_im)
    first = u.shape[1] - 1 if reverse else 0
    bu_re = bu_re.at[:, first].add(ab_re * h0_re - ab_im * h0_im)
    bu_im = bu_im.at[:, first].add(ab_re * h0_im + ab_im * h0_re)
    a_re = jnp.broadcast_to(ab_re, bu_re.shape)
    a_im = jnp.broadcast_to(ab_im, bu_im.shape)
    _, _, h_re, h_im = lax.associative_scan(
        _complex_affine_combine, (a_re, a_im, bu_re, bu_im), reverse=reverse, axis=1)
    return h_re, h_im


def s5_readout(h_re, h_im, c_re, c_im):
    return (jnp.einsum('blgp,gcp->blgc', h_re, c_re.astype(jnp.float32))
            - jnp.einsum('blgp,gcp->blgc', h_im, c_im.astype(jnp.float32)))


def s5_mixer(ux, uc, a_re, a_im, log_dt, b_re, b_im, c_re, c_im, d, glu_w, glu_b, need_ctx):
    def grouped(u):
        return u.astype(jnp.float32).reshape(u.shape[0], u.shape[1], SSM_GROUPS, SSM_CH)
    gx, gc = grouped(ux), grouped(uc)
    zero = jnp.zeros((ux.shape[0], SSM_GROUPS, SSM_STATE), jnp.float32)
    d_g = d.astype(jnp.float32).reshape(SSM_GROUPS, SSM_CH)
    yx = d_g * gx
    yc = d_g * gc if need_ctx else None
    for direction in range(2):
        reverse = direction == 1
        ab_re, ab_im, bb_re, bb_im = s5_discretise(
            a_re[direction], a_im[direction], log_dt[direction], b_re[direction], b_im[direction])
        hc_re, hc_im = diag_scan(ab_re, ab_im, bb_re, bb_im, gc, zero, zero, reverse)
        end = 0 if reverse else -1
        hx_re, hx_im = diag_scan(ab_re, ab_im, bb_re, bb_im, gx,
                                 hc_re[:, end], hc_im[:, end], reverse)
        yx = yx + s5_readout(hx_re, hx_im, c_re[direction], c_im[direction])
        if need_ctx:
            yc = yc + s5_readout(hc_re, hc_im, c_re[direction], c_im[direction])

    def glu(y, like):
        z = jax.nn.gelu(y.reshape(like.shape)).astype(like.dtype)
        return z * jax.nn.sigmoid(z @ glu_w + glu_b)
    return glu(yx, ux), (glu(yc, uc) if need_ctx else None)


def axial_rope_tables(rows, dtype):
    row = jnp.repeat(jnp.arange(rows), GRID_W).astype(jnp.float32)
    col = jnp.tile(jnp.arange(GRID_W), rows).astype(jnp.float32)
    inv = ROPE_BASE ** (-jnp.arange(0, ROPE_AXIS, 2, dtype=jnp.float32) / ROPE_AXIS)
    ang_r = row[:, None] * inv
    ang_c = col[:, None] * inv
    return tuple(t.astype(dtype)[:, None, :] for t in
                 (jnp.cos(ang_r), jnp.sin(ang_r), jnp.cos(ang_c), jnp.sin(ang_c)))


def _rotate_half(x, cos, sin):
    x1, x2 = jnp.split(x, 2, axis=-1)
    return jnp.concatenate([x1 * cos - x2 * sin, x2 * cos + x1 * sin], axis=-1)


def apply_axial_rope(t, tables):
    cos_r, sin_r, cos_c, sin_c = tables
    return jnp.concatenate([
        t[..., :MLA_NOPE],
        _rotate_half(t[..., MLA_NOPE:MLA_NOPE + ROPE_AXIS], cos_r, sin_r),
        _rotate_half(t[..., MLA_NOPE + ROPE_AXIS:], cos_c, sin_c)], axis=-1)


def mla_query(cq, q_a_g, wq_b, q_g):
    q = rmsnorm(cq, q_a_g) @ wq_b
    return rmsnorm(q.reshape(*cq.shape[:-1], MLA_HEADS, MLA_QK), q_g)


def mla_key_value(ckv, kpe, kv_a_g, wkv_b, k_g):
    lead = ckv.shape[:-1]
    kv = (rmsnorm(ckv, kv_a_g) @ wkv_b).reshape(*lead, MLA_HEADS, MLA_NOPE + MLA_V)
    k_pe = jnp.broadcast_to(kpe[..., None, :], (*lead, MLA_HEADS, MLA_ROPE))
    k = rmsnorm(jnp.concatenate([kv[..., :MLA_NOPE], k_pe], axis=-1), k_g)
    return k, kv[..., MLA_NOPE:]


def attend(q, k, v):
    s = jnp.einsum('bqhd,bkhd->bhqk', q, k).astype(jnp.float32) * (MLA_QK ** -0.5)
    p = jax.nn.softmax(s, axis=-1).astype(v.dtype)
    return jnp.einsum('bhqk,bkhv->bqhv', p, v)


def blocked_attention(q, k, v):
    bsz, length = q.shape[:2]
    qb = q.reshape(bsz, length // Q_BLOCK, Q_BLOCK, MLA_HEADS, MLA_QK).swapaxes(0, 1)
    o = lax.map(lambda qi: attend(qi, k, v), qb)
    return o.swapaxes(0, 1).reshape(bsz, length, MLA_HEADS * MLA_V)


def moe(h, router_w, router_b, w_in, b_in, w_out, b_out):
    n_tok, dim = h.shape
    logits = (h @ router_w + router_b).astype(jnp.float32)
    top_v, top_i = lax.top_k(logits, TOP_K)
    top_w = jax.nn.softmax(top_v, axis=-1)
    flat_e = top_i.reshape(-1)
    flat_t = jnp.arange(n_tok * TOP_K, dtype=jnp.int32) // TOP_K
    flat_w = top_w.reshape(-1)
    order = jnp.argsort(flat_e)
    sorted_e = flat_e[order]
    counts = jnp.bincount(flat_e, length=N_EXPERTS)
    padded = (counts + MOE_BLOCK - 1) // MOE_BLOCK * MOE_BLOCK
    pad_end = jnp.cumsum(padded)
    pad_start = pad_end - padded
    start = jnp.cumsum(counts) - counts
    dest = pad_start[sorted_e] + (jnp.arange(n_tok * TOP_K) - start[sorted_e])
    n_blocks = -(-(n_tok * TOP_K) // MOE_BLOCK) + N_EXPERTS
    n_pad = n_blocks * MOE_BLOCK
    tok_pad = jnp.full((n_pad,), n_tok, jnp.int32).at[dest].set(flat_t[order])
    w_pad = jnp.zeros((n_pad,), jnp.float32).at[dest].set(flat_w[order])
    block_e = jnp.minimum(jnp.searchsorted(pad_end, jnp.arange(n_blocks) * MOE_BLOCK, side='right'),
                          N_EXPERTS - 1)
    h_ext = jnp.concatenate([h, jnp.zeros((1, dim), h.dtype)], axis=0)

    def expert_block(args):
        idx, e = args
        xb = h_ext[idx]
        gu = xb @ w_in[e] + b_in[e]
        gate, up = jnp.split(gu, 2, axis=-1)
        gate = jnp.minimum(gate, SWIGLU_LIMIT)
        up = jnp.clip(up, -SWIGLU_LIMIT, SWIGLU_LIMIT)
        act = (up + 1.0) * gate * jax.nn.sigmoid(SWIGLU_ALPHA * gate)
        return act @ w_out[e] + b_out[e]

    y = lax.map(expert_block, (tok_pad.reshape(n_blocks, MOE_BLOCK), block_e)).reshape(n_pad, dim)
    y = y * w_pad.astype(y.dtype)[:, None]
    return jnp.zeros((n_tok + 1, dim), y.dtype).at[tok_pad].add(y)[:n_tok]


def setup_inputs(seed: int = 0) -> dict:
    key = jax.random.key(seed)
    keys = iter(jax.random.split(key, 48))

    def nrm(shape, std):
        return std * jax.random.normal(next(keys), shape, jnp.float32)

    def gain(shape):
        return 1.0 + nrm(shape, 0.02)

    L = DEPTH
    G, P = SSM_GROUPS, SSM_STATE
    state_idx = jnp.arange(P, dtype=jnp.float32)
    return {
        'x': nrm((BATCH, SEQ, D_MODEL), 1.0),
        'c': nrm((BATCH, D_MODEL), 1.0),
        'ctx': nrm((BATCH, CTX_LEN, D_MODEL), 1.0),
        'c_ctx': nrm((D_MODEL,), 1.0),
        'ada_w': nrm((L, D_MODEL, 6 * D_MODEL), 0.5 * D_MODEL ** -0.5),
        'ada_b': nrm((L, 6 * D_MODEL), 0.02),
        'norm1_g': gain((L, D_MODEL)),
        'norm2_g': gain((L, D_MODEL)),
        'w_mix_in': nrm((L, D_MODEL, N_IN), D_MODEL ** -0.5),
        'w_mix_out': nrm((L, D_MIX, D_MODEL), D_MIX ** -0.5),
        'conv_dw': nrm((L, CONV_K, W_CONV), CONV_K ** -0.5),
        'conv_dw_b': nrm((L, W_CONV), 0.02),
        'conv_ln_g': gain((L, W_CONV)),
        'conv_ln_b': nrm((L, W_CONV), 0.02),
        'conv_pw': nrm((L, W_CONV, W_CONV), W_CONV ** -0.5),
        'pool_w': nrm((L, len(POOL_WINDOWS), POOL_CH, POOL_CH), POOL_CH ** -0.5),
        'pool_scale': gain((L, W_POOL)),
        'ssm_a_re': -0.5 + nrm((L, 2, G, P), 0.01),
        'ssm_a_im': math.pi * state_idx + nrm((L, 2, G, P), 0.01),
        'ssm_log_dt': jax.random.uniform(next(keys), (L, 2, G), jnp.float32,
                                         math.log(1e-3), math.log(1e-1)),
        'ssm_b_re': nrm((L, 2, G, P, SSM_CH), (2 * SSM_CH) ** -0.5),
        'ssm_b_im': nrm((L, 2, G, P, SSM_CH), (2 * SSM_CH) ** -0.5),
        'ssm_c_re': nrm((L, 2, G, SSM_CH, P), P ** -0.5),
        'ssm_c_im': nrm((L, 2, G, SSM_CH, P), P ** -0.5),
        'ssm_d': nrm((L, W_SSM), 0.5),
        'ssm_glu_w': nrm((L, W_SSM, W_SSM), W_SSM ** -0.5),
        'ssm_glu_b': nrm((L, W_SSM), 0.02),
        'mla_q_a_g': gain((L, MLA_Q_RANK)),
        'mla_wq_b': nrm((L, MLA_Q_RANK, MLA_HEADS * MLA_QK), MLA_Q_RANK ** -0.5),
        'mla_kv_a_g': gain((L, MLA_KV_RANK)),
        'mla_wkv_b': nrm((L, MLA_KV_RANK, MLA_HEADS * (MLA_NOPE + MLA_V)), MLA_KV_RANK ** -0.5),
        'mla_q_g': gain((L, MLA_QK)),
        'mla_k_g': gain((L, MLA_QK)),
        'router_w': nrm((L, D_MODEL, N_EXPERTS), D_MODEL ** -0.5),
        'router_b': nrm((L, N_EXPERTS), 0.01),
        'exp_w_in': nrm((L, N_EXPERTS, D_MODEL, 2 * D_EXPERT), D_MODEL ** -0.5),
        'exp_b_in': nrm((L, N_EXPERTS, 2 * D_EXPERT), 0.01),
        'exp_w_out': nrm((L, N_EXPERTS, D_EXPERT, D_MODEL), D_EXPERT ** -0.5),
        'exp_b_out': nrm((L, N_EXPERTS, D_MODEL), 0.01),
    }


def reference(x, c, ctx, c_ctx, ada_w, ada_b, norm1_g, norm2_g, w_mix_in, w_mix_out,
              conv_dw, conv_dw_b, conv_ln_g, conv_ln_b, conv_pw, pool_w, pool_scale,
              ssm_a_re, ssm_a_im, ssm_log_dt, ssm_b_re, ssm_b_im, ssm_c_re, ssm_c_im,
              ssm_d, ssm_glu_w, ssm_glu_b,
              mla_q_a_g, mla_wq_b, mla_kv_a_g, mla_wkv_b, mla_q_g, mla_k_g,
              router_w, router_b, exp_w_in, exp_b_in, exp_w_out, exp_b_out):
    bsz, n_lat, dim = x.shape
    n_ctx = ctx.shape[1]
    rows = n_lat // GRID_W
    rope = axial_rope_tables(rows, x.dtype)
    silu_c = jax.nn.silu(c)
    silu_cc = jax.nn.silu(c_ctx)
    for l in range(DEPTH):
        last = l == DEPTH - 1
        mod = (silu_c @ ada_w[l] + ada_b[l])[:, None, :]
        mod_c = silu_cc @ ada_w[l] + ada_b[l]
        sh1, sc1, g1, sh2, sc2, g2 = jnp.split(mod, 6, axis=-1)
        sh1c, sc1c, g1c, sh2c, sc2c, g2c = jnp.split(mod_c, 6, axis=-1)

        hx = rmsnorm(x, norm1_g[l]) * (1.0 + sc1) + sh1
        hc = rmsnorm(ctx, norm1_g[l]) * (1.0 + sc1c) + sh1c
        x_conv, x_pool, x_q, x_ssm, x_kv, x_kpe = jnp.split(hx @ w_mix_in[l], SPLIT_AT, axis=-1)
        if last:
            c_ssm, c_kv, c_kpe = jnp.split(hc @ w_mix_in[l][:, OFF_SSM:],
                                           (W_SSM, W_SSM + MLA_KV_RANK), axis=-1)
        else:
            c_conv, c_pool, c_q, c_ssm, c_kv, c_kpe = jnp.split(hc @ w_mix_in[l], SPLIT_AT, axis=-1)

        conv_p = (conv_dw[l], conv_dw_b[l], conv_ln_g[l], conv_ln_b[l], conv_pw[l])
        ya_x = conv_module(x_conv, *conv_p)
        yp_x = multiscale_pool(x_pool, pool_w[l], pool_scale[l])
        ys_x, ys_c = s5_mixer(x_ssm, c_ssm, ssm_a_re[l], ssm_a_im[l], ssm_log_dt[l],
                              ssm_b_re[l], ssm_b_im[l], ssm_c_re[l], ssm_c_im[l],
                              ssm_d[l], ssm_glu_w[l], ssm_glu_b[l], not last)
        qx = apply_axial_rope(mla_query(x_q, mla_q_a_g[l], mla_wq_b[l], mla_q_g[l]), rope)
        kx, vx = mla_key_value(x_kv, x_kpe, mla_kv_a_g[l], mla_wkv_b[l], mla_k_g[l])
        kx = apply_axial_rope(kx, rope)
        kc, vc = mla_key_value(c_kv, c_kpe, mla_kv_a_g[l], mla_wkv_b[l], mla_k_g[l])
        ym_x = blocked_attention(qx, jnp.concatenate([kx, kc], axis=1),
                                 jnp.concatenate([vx, vc], axis=1))
        x = x + g1 * (jnp.concatenate([ya_x, yp_x, ys_x, ym_x], axis=-1) @ w_mix_out[l])
        if not last:
            ya_c = conv_module(c_conv, *conv_p)
            yp_c = multiscale_pool(c_pool, pool_w[l], pool_scale[l])
            qc = mla_query(c_q, mla_q_a_g[l], mla_wq_b[l], mla_q_g[l])
            ym_c = attend(qc, kc, vc).reshape(bsz, n_ctx, W_MLA)
            ctx = ctx + g1c * (jnp.concatenate([ya_c, yp_c, ys_c, ym_c], axis=-1) @ w_mix_out[l])

        moe_p = (router_w[l], router_b[l], exp_w_in[l], exp_b_in[l], exp_w_out[l], exp_b_out[l])
        h2x = (rmsnorm(x, norm2_g[l]) * (1.0 + sc2) + sh2).reshape(bsz * n_lat, dim)
        if last:
            x = x + g2 * moe(h2x, *moe_p).reshape(bsz, n_lat, dim)
        else:
            h2c = (rmsnorm(ctx, norm2_g[l]) * (1.0 + sc2c) + sh2c).reshape(bsz * n_ctx, dim)
            m = moe(jnp.concatenate([h2x, h2c], axis=0), *moe_p)
            x = x + g2 * m[:bsz * n_lat].reshape(bsz, n_lat, dim)
            ctx = ctx + g2c * m[bsz * n_lat:].reshape(bsz, n_ctx, dim)
    return x
```

```python
import math
from contextlib import ExitStack
import numpy as np
import ml_dtypes
import concourse.bass as bass
import concourse.mybir as mybir
from concourse.bass_utils import run_bass_kernel_spmd

F32 = mybir.dt.float32
BF16 = mybir.dt.bfloat16
I32 = mybir.dt.int32
U32 = mybir.dt.uint32
ALU = mybir.AluOpType
AF = mybir.ActivationFunctionType

D = 1024
LC = 256
LX = 2048
S = LC + LX
NIN = 1376
OFF_POOL, OFF_Q, OFF_SSM, OFF_KV, OFF_KPE = 512, 768, 960, 1216, 1344
NE = 32
EPS = 1e-6
TS5 = 128
PADW = 2352
NI = 2320
MB = 1536


SKIP = set()


class Buf:
    __slots__ = ("name", "w", "r", "dsem", "dcnt")

    def __init__(self, name):
        self.name = name
        self.w = {}
        self.r = {}
        self.dsem = None
        self.dcnt = 0


class KB:
    ENG = ("pe", "act", "dve", "pool", "sp")

    def __init__(self, nc, stack):
        self.nc = nc
        self.stack = stack
        self.prog = {e: [] for e in self.ENG}
        self.esem = {e: stack.enter_context(nc.semaphore("s_" + e)) for e in self.ENG}
        self.ecnt = {e: 0 for e in self.ENG}
        self.known = {e: {} for e in self.ENG}
        self.dbufs = []
        self.sempool = []
        self.mute = False
        self.nsem = 0
        self.n = 0

    def sb(self, st, shape, dt, name):
        self.n += 1
        nm = f"{name}_{self.n}"
        return st.enter_context(self.nc.sbuf_tensor(nm, list(shape), dt)), Buf(nm)

    def ps(self, st, shape, dt, name):
        self.n += 1
        nm = f"{name}_{self.n}"
        return st.enter_context(self.nc.psum_tensor(nm, list(shape), dt)), Buf(nm)

    def _need(self, e, ev):
        sem, val, who = ev
        kn = self.known[e]
        if kn.get(id(sem), 0) >= val:
            return
        kn[id(sem)] = val
        self.prog[e].append(("wait", sem, val))

    def _deps(self, e, r, w, skip_who=None):
        for b in r:
            for ev in b.w.values():
                self._need(e, ev)
        for b in w:
            for ev in b.w.values():
                if skip_who is not None and ev[2] == skip_who:
                    continue
                self._need(e, ev)
            for ev in b.r.values():
                self._need(e, ev)

    def _record(self, ev, r, w, multi):
        key = id(ev[0])
        for b in r:
            b.r[key] = ev
        for b in w:
            if multi:
                b.w[key] = ev
            else:
                b.w = {key: ev}
            b.r = {}

    def sec(self, name):
        self.mute = name is not None and name in SKIP

    def op(self, e, fn, r=(), w=()):
        if self.mute:
            return
        self._deps(e, r, w, skip_who="pe" if e == "pe" else None)
        self.ecnt[e] += 1
        ev = (self.esem[e], self.ecnt[e], e)
        self.prog[e].append(("ins", fn, self.esem[e], 1))
        self._record(ev, r, w, multi=False)

    def dma(self, q, fn, r=(), w=(), own=None, multi=False):
        if self.mute:
            return
        b0 = own if own is not None else w[0]
        if b0.dsem is None:
            if self.sempool:
                b0.dsem, b0.dcnt = self.sempool.pop()
            else:
                self.nsem += 1
                b0.dsem = self.stack.enter_context(self.nc.semaphore(f"dsem{self.nsem}"))
                b0.dcnt = 0
            self.dbufs.append(b0)
        self._deps(q, r, w, skip_who="dma" if multi else None)
        b0.dcnt += 16
        ev = (b0.dsem, b0.dcnt, "dma")
        self.prog[q].append(("ins", fn, b0.dsem, 16))
        self._record(ev, r, w, multi=multi)

    def raw(self, e, fn, r=()):
        self._deps(e, r, [])
        self.prog[e].append(("raw", fn))

    def barrier(self):
        for e in self.ENG:
            for e2 in self.ENG:
                if e2 != e and self.ecnt[e2] > 0:
                    self._need(e, (self.esem[e2], self.ecnt[e2], e2))
            for b in self.dbufs:
                if b.dcnt > 0:
                    self._need(e, (b.dsem, b.dcnt, "dma"))
        for b in self.dbufs:
            self.sempool.append((b.dsem, b.dcnt))
            b.dsem = None
        self.dbufs = []

    def finish(self):
        self.barrier()
        nc = self.nc
        with nc.Block() as block:
            def mk(ename):
                def body(eng):
                    for it in self.prog[ename]:
                        if it[0] == "wait":
                            eng.wait_ge(it[1], it[2])
                        elif it[0] == "raw":
                            it[1](eng)
                        else:
                            it[1](eng).then_inc(it[2], it[3])
                return body
            block.tensor(mk("pe"))
            block.scalar(mk("act"))
            block.vector(mk("dve"))
            block.gpsimd(mk("pool"))
            block.sync(mk("sp"))


class Emit:
    def __init__(self, k):
        self.k = k
        self.q = 0

    def mm(self, out, ob, lhsT, lb, rhs, rb, start=True, stop=True):
        self.k.op("pe", lambda e: e.matmul(out=out, lhsT=lhsT, rhs=rhs, start=start, stop=stop), r=[lb, rb], w=[ob])

    def tr(self, out, ob, in_, ib, ident, idb):
        self.k.op("pe", lambda e: e.transpose(out=out, in_=in_, identity=ident), r=[ib, idb], w=[ob])

    def act(self, out, ob, in_, ib, func, bias=None, scale=None, accum=None, xr=(), xw=()):
        kw = {}
        if bias is not None:
            kw["bias"] = bias
        if scale is not None:
            kw["scale"] = scale
        if accum is not None:
            kw["accum_out"] = accum
        self.k.op("act", lambda e: e.activation(out=out, in_=in_, func=func, **kw), r=[ib, *xr], w=[ob, *xw])

    def tt(self, eng, out, ob, a, ab, b, bb, op):
        self.k.op(eng, lambda e: e.tensor_tensor(out=out, in0=a, in1=b, op=op), r=[ab, bb], w=[ob])

    def ts(self, eng, out, ob, a, ab, s1, s2, op0, op1=None, xr=(), accum=None, xw=()):
        kw = {}
        if op1 is not None:
            kw["op1"] = op1
        if accum is not None:
            kw["accum_out"] = accum
        self.k.op(eng, lambda e: e.tensor_scalar(out=out, in0=a, scalar1=s1, scalar2=s2, op0=op0, **kw), r=[ab, *xr], w=[ob, *xw])

    def stt(self, out, ob, a, ab, scalar, b, bb, op0, op1, xr=()):
        self.k.op("dve", lambda e: e.scalar_tensor_tensor(out=out, in0=a, scalar=scalar, in1=b, op0=op0, op1=op1), r=[ab, bb, *xr], w=[ob])

    def cp(self, eng, out, ob, in_, ib):
        if eng == "act":
            self.k.op("act", lambda e: e.copy(out=out, in_=in_), r=[ib], w=[ob])
        else:
            self.k.op(eng, lambda e: e.tensor_copy(out=out, in_=in_), r=[ib], w=[ob])

    def memset(self, eng, ap, ob, val):
        self.k.op(eng, lambda e: e.memset(ap, val), w=[ob])

    def recip(self, out, ob, in_, ib):
        self.k.op("dve", lambda e: e.reciprocal(out=out, in_=in_), r=[ib], w=[ob])

    def scan(self, out, ob, d0, d0b, d1, d1b, init, initb):
        r = [d0b, d1b] + ([initb] if initb is not None else [])
        self.k.op("dve", lambda e: e.tensor_tensor_scan(out=out, data0=d0, data1=d1, initial=init, op0=ALU.mult, op1=ALU.add), r=r, w=[ob])

    def dma(self, q, out, ob, in_, ib, own=None, multi=False, **kw):
        r = [ib] if isinstance(ib, Buf) else list(ib)
        self.k.dma(q, lambda e: e.dma_start(out=out, in_=in_, **kw), r=r, w=[ob], own=own, multi=multi)

    def ld(self, out, ob, in_, ib=()):
        self.q ^= 1
        r = [ib] if isinstance(ib, Buf) else list(ib)
        self.k.dma("sp" if self.q else "act", lambda e: e.dma_start(out=out, in_=in_), r=r, w=[ob])


def _pk(v, nchunk):
    sh = v.shape[:-1]
    return np.ascontiguousarray(np.swapaxes(v.reshape(*sh, nchunk, 128), -1, -2))


def _consts():
    c = {}
    c["ident_f"] = np.eye(128, dtype=np.float32)
    c["ident_b"] = np.eye(128).astype(ml_dtypes.bfloat16)
    c["ustrict"] = np.triu(np.ones((128, 128), np.float32), 1).astype(ml_dtypes.bfloat16)
    c["iota32"] = np.broadcast_to(np.arange(NE, dtype=np.float32), (128, NE)).copy()
    t = np.arange(LX)
    row = (t // 64).astype(np.float32)
    col = (t % 64).astype(np.float32)
    inv = (10000.0 ** (-np.arange(0, 16, 2, dtype=np.float32) / 16)).astype(np.float32)
    ang_r = (row[:, None] * inv).astype(np.float32)
    ang_c = (col[:, None] * inv).astype(np.float32)
    cosT = np.ones((96, LX), np.float32)
    sinT = np.zeros((96, LX), np.float32)
    cosT[64:72] = np.cos(ang_r).T
    cosT[72:80] = np.cos(ang_r).T
    sinT[64:72] = np.sin(ang_r).T
    sinT[72:80] = np.sin(ang_r).T
    cosT[80:88] = np.cos(ang_c).T
    cosT[88:96] = np.cos(ang_c).T
    sinT[80:88] = np.sin(ang_c).T
    sinT[88:96] = np.sin(ang_c).T
    c["ropecos"] = cosT
    c["ropesin"] = sinT
    pm = np.zeros((96, 96), np.float32)
    for base in (64, 80):
        for j in range(8):
            pm[base + j, base + 8 + j] = -1.0
            pm[base + 8 + j, base + j] = 1.0
    c["prot"] = np.ascontiguousarray(pm.T).astype(ml_dtypes.bfloat16)
    wins = (2, 4, 8, 16)
    invw = np.zeros((128, 2), np.float32)
    corr = np.ones((128, 2, 4, 8), np.float32)
    for g, win in enumerate(wins):
        cch, half = g // 2, g % 2
        ps = slice(half * 64, half * 64 + 64)
        invw[ps, cch] = 1.0 / win
        for ri, L in ((0, LC), (2, LX)):
            for j in range(8):
                tt = j
                lo, hi = max(tt - win // 2, 0), min(tt + win - 1 - win // 2, L - 1)
                corr[ps, cch, ri, j] = win / float(hi - lo + 1)
                tt = L - 8 + j
                lo, hi = max(tt - win // 2, 0), min(tt + win - 1 - win // 2, L - 1)
                corr[ps, cch, ri + 1, j] = win / float(hi - lo + 1)
    c["pool_invw"] = invw
    c["pool_corr"] = corr
    tau = np.zeros((128, 2, TS5), np.float32)
    tau[:, 0, :] = np.arange(1, TS5 + 1, dtype=np.float32)
    tau[:, 1, :] = np.arange(TS5, 0, -1).astype(np.float32)
    c["tau"] = tau
    return c


def _prep_shared(inp):
    L = 2
    o = {}
    o["ada_w"] = inp["ada_w"]
    o["ada_bT"] = _pk(inp["ada_b"], 48)
    o["ada_brow"] = inp["ada_b"].reshape(L, 1, 6 * D)
    o["n1gT"] = _pk(inp["norm1_g"], 8)
    o["n2g"] = inp["norm2_g"].reshape(L, 1, D)
    o["w_mix_in"] = inp["w_mix_in"]
    o["w_mix_out"] = inp["w_mix_out"]
    o["conv_dwT"] = np.ascontiguousarray(np.transpose(inp["conv_dw"].reshape(L, 31, 2, 128), (0, 3, 2, 1)))
    o["conv_dwbT"] = _pk(inp["conv_dw_b"], 2)
    o["conv_lngT"] = _pk(inp["conv_ln_g"], 2)
    o["conv_lnbT"] = _pk(inp["conv_ln_b"], 2)
    o["conv_pw"] = inp["conv_pw"]
    pw = inp["pool_w"]
    pbd = np.zeros((L, 2, 128, 128), np.float32)
    for g in range(4):
        cch, half = g // 2, g % 2
        pbd[:, cch, half * 64:half * 64 + 64, half * 64:half * 64 + 64] = pw[:, g]
    o["pool_wbd"] = pbd
    o["pool_scT"] = _pk(inp["pool_scale"], 2)

    def st_layout(a):
        return np.ascontiguousarray(np.transpose(a.reshape(L, 2, 8, 2, 64), (0, 1, 3, 4, 2)).reshape(L, 2, 128, 8))
    o["s5_are"] = st_layout(inp["ssm_a_re"])
    o["s5_aim"] = st_layout(inp["ssm_a_im"])
    o["s5_ldt"] = st_layout(np.broadcast_to(inp["ssm_log_dt"][..., None], (L, 2, 16, 64)))
    bbd_re = np.zeros((L, 2, 8, 128, 32), np.float32)
    bbd_im = np.zeros((L, 2, 8, 128, 32), np.float32)
    cbd_re = np.zeros((L, 2, 8, 128, 128), np.float32)
    cbd_im = np.zeros((L, 2, 8, 128, 128), np.float32)
    for g in range(16):
        sc, gi = g // 2, g % 2
        bbd_re[:, :, sc, gi * 64:gi * 64 + 64, gi * 16:gi * 16 + 16] = inp["ssm_b_re"][:, :, g]
        bbd_im[:, :, sc, gi * 64:gi * 64 + 64, gi * 16:gi * 16 + 16] = inp["ssm_b_im"][:, :, g]
        c0 = (sc % 4) * 32 + gi * 16
        cbd_re[:, :, sc, gi * 64:gi * 64 + 64, c0:c0 + 16] = np.swapaxes(inp["ssm_c_re"][:, :, g], -1, -2)
        cbd_im[:, :, sc, gi * 64:gi * 64 + 64, c0:c0 + 16] = np.swapaxes(inp["ssm_c_im"][:, :, g], -1, -2)
    o["s5_bre"], o["s5_bim"], o["s5_cre"], o["s5_cim"] = bbd_re, bbd_im, cbd_re, cbd_im
    o["s5_dT"] = _pk(inp["ssm_d"], 2)
    o["s5_gluw"] = inp["ssm_glu_w"]
    o["s5_glubT"] = _pk(inp["ssm_glu_b"], 2)
    qag = np.zeros((L, 256), np.float32)
    qag[:, :192] = inp["mla_q_a_g"]
    o["qagT"] = _pk(qag, 2)
    o["wq_b"] = inp["mla_wq_b"]
    o["kvagT"] = inp["mla_kv_a_g"].reshape(L, 128, 1)
    o["wkv_b"] = inp["mla_wkv_b"]
    o["qgT"] = inp["mla_q_g"].reshape(L, 96, 1)
    o["kgT"] = inp["mla_k_g"].reshape(L, 96, 1)
    o["kgpeT"] = np.ascontiguousarray(inp["mla_k_g"][:, 64:96]).reshape(L, 32, 1)
    o["router_w"] = inp["router_w"]
    o["router_b"] = inp["router_b"].reshape(L, 1, NE)
    o["exp_w_in"] = inp["exp_w_in"]
    o["exp_b_inT"] = _pk(inp["exp_b_in"], 16)
    o["exp_w_out"] = inp["exp_w_out"]
    o["exp_b_out"] = inp["exp_b_out"].reshape(L, NE, 1, D)
    o.update(_consts())
    return {k_: np.ascontiguousarray(v) for k_, v in o.items()}


XBLK = [(0, 256)] + [(256 + 512 * j, 512) for j in range(4)]


def pcol(s0):
    return 16 + s0 if s0 < LC else 32 + s0


_DUMP = [None]


def build(NB, specs, dbg=(), depth=2):
    NB1 = NB + 1
    nc = bass.Bass("TRN2", target_bir_lowering=False)
    I = {}
    for name, (shape, dt) in specs.items():
        bdt = {np.dtype(np.float32): F32, np.dtype(ml_dtypes.bfloat16): BF16, np.dtype(np.int32): I32}[np.dtype(dt)]
        I[name] = nc.dram_tensor(name, list(shape), bdt, kind="ExternalInput").ap()

    def scr(name, shape, dt=F32):
        kind = "ExternalOutput" if name in dbg else "Internal"
        return nc.dram_tensor(name, list(shape), dt, kind=kind).ap()

    out = nc.dram_tensor("out", [NB, LX, D], F32, kind="ExternalOutput").ap()
    R = scr("R", [NB, S, D])
    modrows = scr("modrows", [2, NB1, 6 * D])
    Zin = scr("Zin", [NB, NIN, S])
    Ycat = scr("Ycat", [NB, D, S], BF16)
    NTOK0 = NB * S
    NBLK0 = -(-NTOK0 * 4 // MB) + NE
    H2 = scr("H2", [NTOK0, D], BF16)
    Xg = scr("Xg", [NBLK0 * MB, D], BF16)
    Yg = [scr(f"Yg{h_}", [NBLK0 * MB, D // 2], F32) for h_ in range(2)]
    Rb = [[Buf(f"R{b}_{t}") for t in range(S // 128)] for b in range(NB)]
    outb = [[Buf(f"o{b}_{t}") for t in range(LX // 128)] for b in range(NB)]
    modrows_b = Buf("modrows")
    Zb = [Buf(f"Z{b}") for b in range(NB)]
    Yb = [Buf(f"Yc{b}") for b in range(NB)]
    H2b = [Buf(f"H2_{t}") for t in range(NTOK0 // 128)]
    Xgb = Buf("Xg")
    Ygb = Buf("Yg")
    inb = Buf("inputs")

    with ExitStack() as top:
        k = KB(nc, top)
        E = Emit(k)

        def dump(name, ap, buf, shape, dt):
            if ("dbg_" + name) not in dbg:
                return
            t_ = nc.dram_tensor("dbg_" + name, list(shape), dt, kind="ExternalOutput").ap()
            E.dma("sp", t_, Buf("dbg_" + name), ap, buf, own=buf)
        _DUMP[0] = dump
        ident_b, ident_bb = k.sb(top, [128, 128], BF16, "identb")
        ident_f, ident_fb = k.sb(top, [128, 128], F32, "identf")
        ones_b, ones_bb = k.sb(top, [128, 128], BF16, "onesb")
        E.ld(ident_b[:], ident_bb, I["ident_b"][:, :])
        E.ld(ident_f[:], ident_fb, I["ident_f"][:, :])
        E.memset("pool", ones_b[:], ones_bb, 1.0)
        modT = [k.sb(top, [128, 48, NB1], F32, f"modT{l}") for l in range(2)]
        psum = [k.ps(top, [128, 512], F32, f"bank{i}") for i in range(8)]
        pi = [0]

        def bank():
            pi[0] = pi[0] % 7 + 1
            return psum[pi[0]]
        bank.acc = lambda: psum[0]

        with ExitStack() as st:
            cT, cTb = k.sb(st, [128, 8, NB1], F32, "cT")
            sT, sTb = k.sb(st, [128, 8, NB1], F32, "sT")
            E.ld(cT[:], cTb, I["cT"][:, :, :])
            E.act(sT[:], sTb, cT[:], cTb, AF.Silu)
            wbl = [k.sb(st, [128, 8, 512], F32, f"adaw{i}") for i in range(2)]
            brw = [k.sb(st, [NB1, 512], F32, f"brow{i}") for i in range(2)]
            rwt = [k.sb(st, [NB1, 512], F32, f"rowt{i}") for i in range(2)]
            abT, abTb = k.sb(st, [128, 2, 48], F32, "abT")
            E.ld(abT[:], abTb, I["ada_bT"].rearrange("l p c -> p l c"))
            it = 0
            for l in range(2):
                wv = I["ada_w"][l].rearrange("(k p) n -> p k n", p=128)
                for cb in range(12):
                    w_, wb_ = wbl[it % 2]
                    br_, brb_ = brw[it % 2]
                    rt_, rtb_ = rwt[it % 2]
                    it += 1
                    E.ld(w_[:], wb_, wv[:, :, cb * 512:(cb + 1) * 512])
                    E.ld(br_[:], brb_, I["ada_brow"][l, 0:1, cb * 512:(cb + 1) * 512].partition_broadcast(NB1))
                    for j in range(4):
                        ch = cb * 4 + j
                        p_, pb_ = bank()
                        for kk in range(8):
                            E.mm(p_[:, 0:NB1], pb_, w_[:, kk, j * 128:(j + 1) * 128], wb_, sT[:, kk, :], sTb, start=kk == 0, stop=kk == 7)
                        E.ts("dve", modT[l][0][:, ch, :], modT[l][1], p_[:, 0:NB1], pb_, abT[:, l, ch:ch + 1], None, ALU.add, xr=[abTb])
                    p_, pb_ = bank()
                    for kk in range(8):
                        E.mm(p_[0:NB1, :], pb_, sT[:, kk, :], sTb, w_[:, kk, :], wb_, start=kk == 0, stop=kk == 7)
                    E.tt("dve", rt_[:], rtb_, p_[0:NB1, :], pb_, br_[:], brb_, ALU.add)
                    E.dma("sp", modrows[l, :, cb * 512:(cb + 1) * 512], modrows_b, rt_[:], rtb_, own=rtb_, multi=True)
        k.barrier()

        for l in range(depth):
            last = l == 1
            mT, mTb = modT[l]
            for b in range(NB):
                def src_tile(ti):
                    if l == 0:
                        return (I["ctx"][b, ti * 128:(ti + 1) * 128, :] if ti < 2 else I["x"][b, (ti - 2) * 128:(ti - 1) * 128, :]), inb
                    return R[b, ti * 128:(ti + 1) * 128, :], Rb[b][ti]
                with ExitStack() as st:
                    wmi, wmib = k.sb(st, [128, 8, NIN], BF16, "wmi")
                    wv = I["w_mix_in"][l].rearrange("(k p) n -> p k n", p=128)
                    for h_ in range(4):
                        E.dma("pool", wmi[:, 2 * h_:2 * h_ + 2, :], wmib, wv[:, 2 * h_:2 * h_ + 2, :], inb, multi=True)
                    n1g, n1gb = k.sb(st, [128, 8], F32, "n1g")
                    E.ld(n1g[:], n1gb, I["n1gT"][l])
                    A1, A1b = k.sb(st, [128, 8, 2], F32, "A1")
                    for j, bi in enumerate((b, NB)):
                        k.op("dve", lambda e, j=j, bi=bi, A1=A1, mT=mT, n1g=n1g: e.scalar_tensor_tensor(out=A1[:, :, j], in0=mT[:, 8:16, bi], scalar=1.0, in1=n1g[:, :], op0=ALU.add, op1=ALU.mult), r=[mTb, n1gb], w=[A1b])
                    xts = [k.sb(st, [128, D], F32, f"xt{i}") for i in range(3)]
                    xns = [k.sb(st, [128, D], BF16, f"xn{i}") for i in range(2)]
                    junk, junkb = k.sb(st, [128, D], BF16, "junk")
                    st8 = [k.sb(st, [128, 4], F32, f"st{i}") for i in range(2)]
                    hTs = [k.sb(st, [128, 8, 512], BF16, f"hT{i}") for i in range(2)]
                    zts = [k.sb(st, [128, 512], F32, f"zt{i}") for i in range(3)]
                    nt = 0
                    nz = 0
                    for bi_, (s0, ntok) in enumerate(XBLK):
                        hT, hTb = hTs[bi_ % 2]
                        for tt in range(ntok // 128):
                            ti = (s0 + tt * 128) // 128
                            j = 0 if ti >= 2 else 1
                            bi = b if ti >= 2 else NB
                            xt, xtb = xts[nt % 3]
                            xn, xnb = xns[nt % 2]
                            s8, s8b = st8[nt % 2]
                            nt += 1
                            sap, sbuf_ = src_tile(ti)
                            E.ld(xt[:], xtb, sap, sbuf_)
                            E.act(junk[:], junkb, xt[:], xtb, AF.Square, accum=s8[:, 0:1], xw=[s8b])
                            E.act(s8[:, 1:2], s8b, s8[:, 0:1], s8b, AF.Sqrt, bias=EPS, scale=1.0 / D)
                            E.recip(s8[:, 2:3], s8b, s8[:, 1:2], s8b)
                            E.ts("dve", xn[:], xnb, xt[:], xtb, s8[:, 2:3], None, ALU.mult, xr=[s8b])
                            p_, pb_ = bank()
                            pv = p_[:].bitcast(BF16)
                            for kk in range(8):
                                E.tr(pv[:, kk * 128:(kk + 1) * 128], pb_, xn[:, kk * 128:(kk + 1) * 128], xnb, ident_b[:], ident_bb)
                            for kk in range(8):
                                o_ = hT[:, kk, tt * 128:(tt + 1) * 128]
                                i_ = pv[:, kk * 128:(kk + 1) * 128]
                                if kk % 2 == 0:
                                    E.act(o_, hTb, i_, pb_, AF.Identity, bias=mT[:, kk, bi:bi + 1], scale=A1[:, kk, j:j + 1], xr=[mTb, A1b])
                                else:
                                    E.ts("dve", o_, hTb, i_, pb_, A1[:, kk, j:j + 1], mT[:, kk, bi:bi + 1], ALU.mult, ALU.add, xr=[mTb, A1b])
                        for c0 in range(0, NIN, 128):
                            m = min(128, NIN - c0)
                            p_, pb_ = bank()
                            for kk in range(8):
                                E.mm(p_[0:m, 0:ntok], pb_, wmi[:, kk, c0:c0 + m], wmib, hT[:, kk, 0:ntok], hTb, start=kk == 0, stop=kk == 7)
                            zt, ztb = zts[nz % 3]
                            E.cp("act" if nz % 2 == 0 else "dve", zt[0:m, 0:ntok], ztb, p_[0:m, 0:ntok], pb_)
                            nz += 1
                            E.dma("sp", Zin[b, c0:c0 + m, s0:s0 + ntok], Zb[b], zt[0:m, 0:ntok], ztb, own=ztb, multi=True)
                k.barrier()
                mixers(nc, k, E, I, l, b, NB, last, Zin, Zb, Ycat, Yb, mT, mTb, bank, ident_b, ident_bb, ident_f, ident_fb, ones_b, ones_bb, inb)
                k.sec(None)
                k.barrier()
                with ExitStack() as st:
                    wmo, wmob = k.sb(st, [128, 8, D], BF16, "wmo")
                    wv = I["w_mix_out"][l].rearrange("(k p) n -> p k n", p=128)
                    for h_ in range(4):
                        E.dma("pool", wmo[:, 2 * h_:2 * h_ + 2, :], wmob, wv[:, 2 * h_:2 * h_ + 2, :], inb, multi=True)
                    g1t = [k.sb(st, [128, D], F32, f"g1_{i}") for i in range(2)]
                    E.ld(g1t[0][0][:], g1t[0][1], modrows[l, b:b + 1, 2 * D:3 * D].partition_broadcast(128), modrows_b)
                    E.ld(g1t[1][0][:], g1t[1][1], modrows[l, NB:NB1, 2 * D:3 * D].partition_broadcast(128), modrows_b)
                    ycs = [k.sb(st, [128, 8, 128], BF16, f"yc{i}") for i in range(2)]
                    xts = [k.sb(st, [128, D], F32, f"xr{i}") for i in range(2)]
                    xos = [k.sb(st, [128, D], F32, f"xo{i}") for i in range(2)]
                    yv = Ycat[b].rearrange("(k p) t -> p k t", p=128)
                    for n_, ti in enumerate(range(2 if last else 0, S // 128)):
                        yc, ycb = ycs[n_ % 2]
                        xt, xtb = xts[n_ % 2]
                        xo, xob = xos[n_ % 2]
                        g1, g1b = g1t[0] if ti >= 2 else g1t[1]
                        E.ld(yc[:], ycb, yv[:, :, ti * 128:(ti + 1) * 128], Yb[b])
                        sap, sbuf_ = src_tile(ti)
                        E.ld(xt[:], xtb, sap, sbuf_)
                        for hf in range(2):
                            p_, pb_ = bank()
                            for kk in range(8):
                                E.mm(p_[:, :], pb_, yc[:, kk, :], ycb, wmo[:, kk, hf * 512:(hf + 1) * 512], wmob, start=kk == 0, stop=kk == 7)
                            E.tt("dve", xo[:, hf * 512:(hf + 1) * 512], xob, p_[:, :], pb_, g1[:, hf * 512:(hf + 1) * 512], g1b, ALU.mult)
                        E.tt("pool", xo[:], xob, xo[:], xob, xt[:], xtb, ALU.add)
                        E.dma("sp", R[b, ti * 128:(ti + 1) * 128, :], Rb[b][ti], xo[:], xob, own=xob)
                k.barrier()
            moe(nc, k, E, I, l, NB, last, R, Rb, out, outb, modrows, modrows_b, H2, H2b, Xg, Xgb, Yg, Ygb, bank,
                ident_f, ident_fb, ident_b, ident_bb, ones_b, ones_bb, inb)
            k.sec(None)
            k.barrier()
        k.finish()
    return nc


IBLK = [(0, 256, 0)] + [(272 + 512 * j, 512, 256 + 512 * j) for j in range(4)]
TWO_PI = 2.0 * math.pi


def mixers(nc, k, E, I, l, b, NB, last, Zin, Zb, Ycat, Yb, mT, mTb, bank, ident_b, ident_bb, ident_f, ident_fb,
           ones_b, ones_bb, inb):
    Zv = Zin[b]
    Yv = Ycat[b]
    zb = Zb[b]
    yb = Yb[b]

    def yst(row0, nrow, s0, n, src, srcb):
        E.dma("sp", Yv[row0:row0 + nrow, s0:s0 + n], yb, src, srcb, own=srcb, multi=True)

    def castload(st, shape, name, src):
        t, tb = k.sb(st, shape, BF16, name)
        E.dma("pool", t[:], tb, src, inb)
        return t, tb

    def fload(st, shape, name, src):
        t, tb = k.sb(st, shape, F32, name)
        E.ld(t[:], tb, src)
        return t, tb

    k.sec('conv')
    with ExitStack() as st:
        U = [k.sb(st, [128, PADW], F32, f"cU{c}") for c in range(2)]
        acc = [k.sb(st, [128, NI], F32, f"cacc{c}") for c in range(2)]
        gt, gtb = k.sb(st, [128, NI], F32, "cgt")
        dw, dwb_ = fload(st, [128, 2, 31], "cdw", I["conv_dwT"][l])
        dwbias, dwbiasb = fload(st, [128, 2], "cdwb", I["conv_dwbT"][l])
        lng, lngb = fload(st, [128, 2], "clng", I["conv_lngT"][l])
        lnb, lnbb = fload(st, [128, 2], "clnb", I["conv_lnbT"][l])
        pw, pwb = castload(st, [128, 2, 256], "cpw", I["conv_pw"][l].rearrange("(c p) n -> p c n", p=128))
        onesf, onesfb = k.sb(st, [128, 128], F32, "onesf")
        E.memset("pool", onesf[:], onesfb, 1.0 / 256.0)
        for c in range(2):
            u, ub = U[c]
            a, ab = acc[c]
            for (c0, c1) in ((0, 16), (272, 288), (2336, PADW)):
                E.memset("pool", u[:, c0:c1], ub, 0.0)
            E.memset("pool", gt[:, 256:272], gtb, 0.0)
            k.dma("sp", lambda e, u=u, c=c: e.dma_start(out=u[:, 16:272], in_=Zv[c * 128:(c + 1) * 128, 0:LC]), r=[zb], w=[ub], multi=True)
            k.dma("sp", lambda e, u=u, c=c: e.dma_start(out=u[:, 288:2336], in_=Zv[c * 128:(c + 1) * 128, LC:S]), r=[zb], w=[ub], multi=True)
            k.dma("sp", lambda e, c=c: e.dma_start(out=gt[:, 0:256], in_=Zv[256 + c * 128:256 + (c + 1) * 128, 0:LC]), r=[zb], w=[gtb], multi=True)
            k.dma("sp", lambda e, c=c: e.dma_start(out=gt[:, 272:NI], in_=Zv[256 + c * 128:256 + (c + 1) * 128, LC:S]), r=[zb], w=[gtb], multi=True)
            E.act(gt[:], gtb, gt[:], gtb, AF.Sigmoid)
            E.tt("dve", u[:, 16:2336], ub, u[:, 16:2336], ub, gt[:], gtb, ALU.mult)
            E.ts("dve", a[:], ab, u[:, 1:1 + NI], ub, dw[:, c, 0:1], dwbias[:, c:c + 1], ALU.mult, ALU.add, xr=[dwb_, dwbiasb])
            for kk in range(1, 31):
                E.stt(a[:], ab, u[:, 1 + kk:1 + kk + NI], ub, dw[:, c, kk:kk + 1], a[:], ab, ALU.mult, ALU.add, xr=[dwb_])
        sq = [k.sb(st, [128, 512], F32, f"csq{c}") for c in range(2)]
        vs = [k.sb(st, [128, 512], BF16, f"cvs{c}") for c in range(2)]
        tm, tmb = k.sb(st, [128, 512], F32, "ctm")
        tv, tvb = k.sb(st, [128, 512], F32, "ctv")
        t1s = [k.sb(st, [128, 512], F32, f"ct1{c}") for c in range(2)]
        ots = [k.sb(st, [128, 512], BF16, f"cot{c}") for c in range(2)]
        for (i0, n, s0) in IBLK:
            pm, pmb = bank()
            pe2, pe2b = bank()
            for c in range(2):
                E.act(sq[c][0][:, 0:n], sq[c][1], acc[c][0][:, i0:i0 + n], acc[c][1], AF.Square)
            for c in range(2):
                E.mm(pm[:, 0:n], pmb, onesf[:], onesfb, acc[c][0][:, i0:i0 + n], acc[c][1], start=c == 0, stop=c == 1)
            for c in range(2):
                E.mm(pe2[:, 0:n], pe2b, onesf[:], onesfb, sq[c][0][:, 0:n], sq[c][1], start=c == 0, stop=c == 1)
            E.act(tm[:, 0:n], tmb, pm[:, 0:n], pmb, AF.Square)
            E.tt("dve", tv[:, 0:n], tvb, pe2[:, 0:n], pe2b, tm[:, 0:n], tmb, ALU.subtract)
            E.act(tv[:, 0:n], tvb, tv[:, 0:n], tvb, AF.Sqrt, bias=EPS)
            E.recip(tv[:, 0:n], tvb, tv[:, 0:n], tvb)
            for c in range(2):
                t1, t1b = t1s[c]
                E.tt("dve", t1[:, 0:n], t1b, acc[c][0][:, i0:i0 + n], acc[c][1], pm[:, 0:n], pmb, ALU.subtract)
                E.tt("pool", t1[:, 0:n], t1b, t1[:, 0:n], t1b, tv[:, 0:n], tvb, ALU.mult)
                E.act(vs[c][0][:, 0:n], vs[c][1], t1[:, 0:n], t1b, AF.Silu, bias=lnb[:, c:c + 1], scale=lng[:, c:c + 1], xr=[lnbb, lngb])
            for c2 in range(2):
                po, pob = bank()
                for c in range(2):
                    E.mm(po[:, 0:n], pob, pw[:, c, c2 * 128:(c2 + 1) * 128], pwb, vs[c][0][:, 0:n], vs[c][1], start=c == 0, stop=c == 1)
                ot, otb = ots[c2]
                E.cp("act", ot[:, 0:n], otb, po[:, 0:n], pob)
                yst(c2 * 128, 128, s0, n, ot[:, 0:n], otb)
    k.barrier()

    k.sec('pool')
    with ExitStack() as st:
        u, ub = k.sb(st, [128, PADW], F32, "pU")
        A, Ab = k.sb(st, [128, PADW], F32, "pA")
        Bt, Btb = k.sb(st, [128, PADW], F32, "pB")
        PL, PLb_ = k.sb(st, [128, NI], F32, "pPL")
        PLh, PLhb = k.sb(st, [128, NI], BF16, "pPLh")
        invw, invwb = fload(st, [128, 2], "pinvw", I["pool_invw"][:, :])
        corr, corrb = fload(st, [128, 2, 4, 8], "pcorr", I["pool_corr"][:, :, :, :])
        psc, pscb = fload(st, [128, 2], "ppsc", I["pool_scT"][l])
        pwbd, pwbdb = castload(st, [128, 2, 128], "ppw", I["pool_wbd"][l].rearrange("c p n -> p c n"))
        ots = [k.sb(st, [128, 512], BF16, f"pot{c}") for c in range(2)]
        for (c0, c1) in ((0, 16), (272, 288), (2336, PADW)):
            E.memset("pool", u[:, c0:c1], ub, 0.0)
        for c in range(2):
            r0 = OFF_POOL + c * 128
            k.dma("sp", lambda e, r0=r0: e.dma_start(out=u[:, 16:272], in_=Zv[r0:r0 + 128, 0:LC]), r=[zb], w=[ub], multi=True)
            k.dma("sp", lambda e, r0=r0: e.dma_start(out=u[:, 288:2336], in_=Zv[r0:r0 + 128, LC:S]), r=[zb], w=[ub], multi=True)
            W_ = PADW
            E.tt("dve", A[:, 1:W_], Ab, u[:, 0:W_ - 1], ub, u[:, 1:W_], ub, ALU.add)
            E.tt("pool", Bt[:, 2:W_ - 1], Btb, A[:, 1:W_ - 2], Ab, A[:, 3:W_], Ab, ALU.add)
            if c == 1:
                E.tt("dve", A[:, 4:W_ - 3], Ab, Bt[:, 2:W_ - 5], Btb, Bt[:, 6:W_ - 1], Btb, ALU.add)
                E.tt("pool", Bt[:, 8:W_ - 7], Btb, A[:, 4:W_ - 11], Ab, A[:, 12:W_ - 3], Ab, ALU.add)
            E.ts("dve", PL[0:64, :], PLb_, A[0:64, 16:2336], Ab, invw[0:64, c:c + 1], None, ALU.mult, xr=[invwb])
            E.ts("dve", PL[64:128, :], PLb_, Bt[64:128, 16:2336], Btb, invw[64:128, c:c + 1], None, ALU.mult, xr=[invwb])
            for r_, i0 in enumerate((0, 248, 272, 2312)):
                E.tt("dve", PL[:, i0:i0 + 8], PLb_, PL[:, i0:i0 + 8], PLb_, corr[:, c, r_, :], corrb, ALU.mult)
            E.tt("dve", PLh[:], PLhb, PL[:], PLb_, u[:, 16:2336], ub, ALU.subtract)
            for n_, (i0, n, s0) in enumerate(IBLK):
                po, pob = bank()
                E.mm(po[:, 0:n], pob, pwbd[:, c, :], pwbdb, PLh[:, i0:i0 + n], PLhb)
                ot, otb = ots[n_ % 2]
                E.ts("dve", ot[:, 0:n], otb, po[:, 0:n], pob, psc[:, c:c + 1], None, ALU.mult, xr=[pscb])
                yst(256 + c * 128, 128, s0, n, ot[:, 0:n], otb)
    k.barrier()

    k.sec('s5')
    with ExitStack() as st:
        are, areb = fload(st, [128, 2, 8], "sare", I["s5_are"][l].rearrange("d p s -> p d s"))
        aim, aimb = fload(st, [128, 2, 8], "saim", I["s5_aim"][l].rearrange("d p s -> p d s"))
        dt_, dtb = fload(st, [128, 2, 8], "sldt", I["s5_ldt"][l].rearrange("d p s -> p d s"))
        bre, breb = fload(st, [128, 2, 8, 32], "sbre", I["s5_bre"][l].rearrange("d s p c -> p d s c"))
        bim, bimb = fload(st, [128, 2, 8, 32], "sbim", I["s5_bim"][l].rearrange("d s p c -> p d s c"))
        tau, taub = fload(st, [128, 2, TS5], "stau", I["tau"][:, :, :])
        dsk, dskb = fload(st, [128, 2], "sd", I["s5_dT"][l])
        glub, glubb = fload(st, [128, 2], "sglub", I["s5_glubT"][l])
        gluw, gluwb = castload(st, [128, 2, 256], "sgluw", I["s5_gluw"][l].rearrange("(c p) n -> p c n", p=128))
        CB, CBb = k.sb(st, [128, 2, 8, 2, 128], BF16, "sCB")
        for d in range(2):
            E.dma("pool", CB[:, d, :, 0, :], CBb, I["s5_cre"][l, d].rearrange("s p n -> p s n"), inb, multi=True)
            E.dma("pool", CB[:, d, :, 1, :], CBb, I["s5_cim"][l, d].rearrange("s p n -> p s n"), inb, multi=True)
        for d in range(2):
            k.op("act", lambda e, d=d: e.mul(out=CB[:, d, :, 1, :], in_=CB[:, d, :, 1, :], constant=-1.0) if False else e.activation(out=CB[:, d, :, 1, :], in_=CB[:, d, :, 1, :], func=AF.Copy, scale=-1.0), r=[CBb], w=[CBb])
        LB, LBb = k.sb(st, [128, 2, 8, 2, 128], BF16, "sLB")
        E.memset("pool", LB[:], LBb, 0.0)
        mag, magb = k.sb(st, [128, 2, 8], F32, "smag")
        th, thb = k.sb(st, [128, 2, 8], F32, "sth")
        sc_ = {n_: k.sb(st, [128, 2, 8], F32, "s" + n_) for n_ in ("cs", "sn", "abr", "abi", "den", "fre", "fim", "nfim", "q1", "q2")}
        rt1, rt1b = k.sb(st, [128, TS5], F32, "rt1")
        rti, rtib = k.sb(st, [128, TS5], I32, "rti")

        def rr(x, xb, n, both=True):
            E.ts("dve", rt1[:, 0:n], rt1b, x, xb, 1.0 / TWO_PI, None, ALU.mult)
            E.cp("dve", rti[:, 0:n], rtib, rt1[:, 0:n], rt1b)
            E.cp("dve", rt1[:, 0:n], rt1b, rti[:, 0:n], rtib)
            E.stt(x, xb, rt1[:, 0:n], rt1b, -TWO_PI, x, xb, ALU.mult, ALU.add)
            E.ts("dve", rt1[:, 0:n], rt1b, x, xb, math.pi, -TWO_PI, ALU.is_gt, ALU.mult)
            E.tt("dve", x, xb, x, xb, rt1[:, 0:n], rt1b, ALU.add)
            E.ts("dve", rt1[:, 0:n], rt1b, x, xb, -math.pi, TWO_PI, ALU.is_lt, ALU.mult)
            E.tt("dve", x, xb, x, xb, rt1[:, 0:n], rt1b, ALU.add)

        def sincos(ang, angb, n, osin, osinb, ocos, ocosb):
            rr(ang, angb, n)
            E.act(osin, osinb, ang, angb, AF.Sin)
            E.ts("dve", ang, angb, ang, angb, math.pi / 2, None, ALU.add)
            rr(ang, angb, n)
            E.act(ocos, ocosb, ang, angb, AF.Sin)

        f2 = lambda t: t[:].rearrange("p d s -> p (d s)")
        V = {n_: (f2(t), tb) for n_, (t, tb) in sc_.items()}
        aref, aimf, dtf, magf, thf = f2(are), f2(aim), f2(dt_), f2(mag), f2(th)
        E.ts("dve", aref, areb, aref, areb, -1e-4, None, ALU.min)
        E.act(dtf, dtb, dtf, dtb, AF.Exp)
        E.tt("dve", magf, magb, aref, areb, dtf, dtb, ALU.mult)
        E.act(magf, magb, magf, magb, AF.Exp)
        E.tt("dve", thf, thb, aimf, aimb, dtf, dtb, ALU.mult)
        q1, q1b = V["q1"]
        q2, q2b = V["q2"]
        E.cp("dve", q1, q1b, thf, thb)
        sincos(q1, q1b, 16, V["sn"][0], V["sn"][1], V["cs"][0], V["cs"][1])
        E.tt("dve", V["abr"][0], V["abr"][1], magf, magb, V["cs"][0], V["cs"][1], ALU.mult)
        E.tt("dve", V["abi"][0], V["abi"][1], magf, magb, V["sn"][0], V["sn"][1], ALU.mult)
        E.tt("dve", q1, q1b, aref, areb, aref, areb, ALU.mult)
        E.tt("dve", q2, q2b, aimf, aimb, aimf, aimb, ALU.mult)
        E.tt("dve", V["den"][0], V["den"][1], q1, q1b, q2, q2b, ALU.add)
        E.recip(V["den"][0], V["den"][1], V["den"][0], V["den"][1])
        E.ts("dve", V["abr"][0], V["abr"][1], V["abr"][0], V["abr"][1], -1.0, None, ALU.add)
        E.tt("dve", q1, q1b, V["abr"][0], V["abr"][1], aref, areb, ALU.mult)
        E.tt("dve", q2, q2b, V["abi"][0], V["abi"][1], aimf, aimb, ALU.mult)
        E.tt("dve", q1, q1b, q1, q1b, q2, q2b, ALU.add)
        E.tt("dve", V["fre"][0], V["fre"][1], q1, q1b, V["den"][0], V["den"][1], ALU.mult)
        E.tt("dve", q1, q1b, V["abi"][0], V["abi"][1], aref, areb, ALU.mult)
        E.tt("dve", q2, q2b, V["abr"][0], V["abr"][1], aimf, aimb, ALU.mult)
        E.tt("dve", q1, q1b, q1, q1b, q2, q2b, ALU.subtract)
        E.tt("dve", V["fim"][0], V["fim"][1], q1, q1b, V["den"][0], V["den"][1], ALU.mult)
        E.ts("dve", V["nfim"][0], V["nfim"][1], V["fim"][0], V["fim"][1], -1.0, None, ALU.mult)
        fre, freb = sc_["fre"]
        fim, fimb = sc_["fim"]
        nfim, nfimb = sc_["nfim"]
        bbs = [k.sb(st, [128, 32], F32, f"sbb{i}") for i in range(2)]
        cosT, cosTb = k.sb(st, [128, 2, 8, TS5], F32, "scosT")
        sinT, sinTb = k.sb(st, [128, 2, 8, TS5], F32, "ssinT")
        ang, angb = k.sb(st, [128, TS5], F32, "sang")
        for d in range(2):
            for sc in range(8):
                bbr, bbrb = bbs[0]
                bbi, bbib = bbs[1]
                E.ts("dve", bbr[:], bbrb, bre[:, d, sc, :], breb, fre[:, d, sc:sc + 1], None, ALU.mult, xr=[freb])
                E.stt(bbr[:], bbrb, bim[:, d, sc, :], bimb, nfim[:, d, sc:sc + 1], bbr[:], bbrb, ALU.mult, ALU.add, xr=[nfimb])
                E.ts("dve", bbi[:], bbib, bim[:, d, sc, :], bimb, fre[:, d, sc:sc + 1], None, ALU.mult, xr=[freb])
                E.stt(bbi[:], bbib, bre[:, d, sc, :], breb, fim[:, d, sc:sc + 1], bbi[:], bbib, ALU.mult, ALU.add, xr=[fimb])
                r0 = (sc % 4) * 32
                for ri, (bb_, bbb_) in enumerate(((bbr, bbrb), (bbi, bbib))):
                    p_, pb_ = bank()
                    E.tr(p_[0:32, 0:128], pb_, bb_[:], bbb_, ident_f[:], ident_fb)
                    E.cp("act", LB[r0:r0 + 32, d, sc, ri, :], LBb, p_[0:32, 0:128], pb_)
                E.ts("dve", ang[:], angb, tau[:, d, :], taub, th[:, d, sc:sc + 1], None, ALU.mult, xr=[thb])
                sincos(ang[:], angb, TS5, sinT[:, d, sc, :], sinTb, cosT[:, d, sc, :], cosTb)
        Uf = [k.sb(st, [128, S], F32, f"sUf{c}") for c in range(2)]
        Ubf, Ubfb = k.sb(st, [128, 2, S], BF16, "sUb")
        yac = [k.sb(st, [128, S], F32, f"syac{c}") for c in range(2)]
        for c in range(2):
            r0 = OFF_SSM + c * 128
            E.ld(Uf[c][0][:], Uf[c][1], Zv[r0:r0 + 128, :], zb)
            E.cp("act", Ubf[:, c, :], Ubfb, Uf[c][0][:], Uf[c][1])
            E.ts("dve", yac[c][0][:], yac[c][1], Uf[c][0][:], Uf[c][1], dsk[:, c:c + 1], None, ALU.mult, xr=[dskb])
        carry, carryb = k.sb(st, [128, 2, 8], F32, "scarry")
        cbufs = [Buf(f"carry{i}") for i in range(8)]
        names = ("t1", "t2", "t3", "t4", "wre", "wim", "gre", "gim", "u1", "u2", "u3", "u4", "hre", "him")
        sets = [{n_: k.sb(st, [128, TS5], F32, f"s{n_}{i}") for n_ in names} for i in range(2)]
        hb, hbb = k.sb(st, [128, 8, 2, TS5], BF16, "shb")
        hbufs = [[Buf(f"hb{sc}_{ri}") for ri in range(2)] for sc in range(8)]
        MUL, ADD, SUB = ALU.mult, ALU.add, ALU.subtract
        for d in range(2):
            k.op("pool", lambda e: e.memset(carry[:], 0.0), w=cbufs)
            order = list(range(S // TS5)) if d == 0 else [1, 0] + list(range(S // TS5 - 1, 1, -1))
            for ch in order:
                cols = slice(ch * TS5, (ch + 1) * TS5)
                for sc in range(8):
                    c = sc // 4
                    T_ = sets[sc % 2]
                    pB, pBb = bank()
                    E.mm(pB[:, 0:TS5], pBb, LB[:, d, sc, 0, :], LBb, Ubf[:, c, cols], Ubfb)
                    E.mm(pB[:, TS5:2 * TS5], pBb, LB[:, d, sc, 1, :], LBb, Ubf[:, c, cols], Ubfb)
                    br, bi_ = pB[:, 0:TS5], pB[:, TS5:2 * TS5]
                    cs, sn = cosT[:, d, sc, :], sinT[:, d, sc, :]
                    g = lambda n_: (T_[n_][0][:], T_[n_][1])
                    E.tt("dve", *g("t1"), br, pBb, cs, cosTb, MUL)
                    E.tt("dve", *g("t2"), bi_, pBb, sn, sinTb, MUL)
                    E.tt("dve", *g("t3"), bi_, pBb, cs, cosTb, MUL)
                    E.tt("dve", *g("t4"), br, pBb, sn, sinTb, MUL)
                    E.tt("pool", *g("wre"), *g("t1"), *g("t2"), ADD)
                    E.tt("pool", *g("wim"), *g("t3"), *g("t4"), SUB)
                    mg = mag[:, d, sc:sc + 1].to_broadcast([128, TS5])
                    for ri, (gn, wn) in enumerate((("gre", "wre"), ("gim", "wim"))):
                        go, gob = T_[gn]
                        wi, wib = T_[wn]
                        if d == 0:
                            E.scan(go[:], gob, mg, magb, wi[:], wib, carry[:, ri, sc:sc + 1], cbufs[sc])
                        else:
                            E.scan(go[:, ::-1], gob, mg, magb, wi[:, ::-1], wib, carry[:, ri, sc:sc + 1], cbufs[sc])
                    E.tt("pool", *g("u1"), cs, cosTb, *g("gre"), MUL)
                    E.tt("pool", *g("u2"), sn, sinTb, *g("gim"), MUL)
                    E.tt("pool", *g("hre"), *g("u1"), *g("u2"), SUB)
                    E.tt("pool", *g("u3"), sn, sinTb, *g("gre"), MUL)
                    E.tt("pool", *g("u4"), cs, cosTb, *g("gim"), MUL)
                    E.tt("pool", *g("him"), *g("u3"), *g("u4"), ADD)
                    lc = TS5 - 1 if d == 0 else 0
                    E.cp("pool", carry[:, 0, sc:sc + 1], cbufs[sc], T_["hre"][0][:, lc:lc + 1], T_["hre"][1])
                    E.cp("pool", carry[:, 1, sc:sc + 1], cbufs[sc], T_["him"][0][:, lc:lc + 1], T_["him"][1])
                    E.cp("act", hb[:, sc, 0, :], hbufs[sc][0], *g("hre"))
                    E.cp("act", hb[:, sc, 1, :], hbufs[sc][1], *g("him"))
                for c in range(2):
                    po, pob = bank()
                    n_ = 0
                    for sc in range(4 * c, 4 * c + 4):
                        for ri in range(2):
                            E.mm(po[:, 0:TS5], pob, CB[:, d, sc, ri, :], CBb, hb[:, sc, ri, :], hbufs[sc][ri], start=n_ == 0, stop=n_ == 7)
                            n_ += 1
                    E.tt("dve", yac[c][0][:, cols], yac[c][1], yac[c][0][:, cols], yac[c][1], po[:, 0:TS5], pob, ADD)
        ta = [k.sb(st, [128, 512], F32, f"sta{c}") for c in range(2)]
        tb_ = [k.sb(st, [128, 512], F32, f"stb{c}") for c in range(2)]
        zf = [k.sb(st, [128, 512], F32, f"szf{c}") for c in range(2)]
        zbh = [k.sb(st, [128, 512], BF16, f"szb{c}") for c in range(2)]
        ots = [k.sb(st, [128, 512], BF16, f"sot{c}") for c in range(2)]
        for (s0, n) in XBLK:
            for c in range(2):
                y, yb_ = yac[c][0][:, s0:s0 + n], yac[c][1]
                a_, ab_ = ta[c][0][:, 0:n], ta[c][1]
                b_, bb_ = tb_[c][0][:, 0:n], tb_[c][1]
                E.act(a_, ab_, y, yb_, AF.Square)
                E.ts("dve", b_, bb_, a_, ab_, 0.044715, 1.0, MUL, ADD)
                E.tt("dve", b_, bb_, b_, bb_, y, yb_, MUL)
                E.act(a_, ab_, b_, bb_, AF.Sigmoid, scale=1.5957691216057308)
                E.tt("pool", zf[c][0][:, 0:n], zf[c][1], y, yb_, a_, ab_, MUL)
                E.cp("act", zbh[c][0][:, 0:n], zbh[c][1], zf[c][0][:, 0:n], zf[c][1])
            for c2 in range(2):
                po, pob = bank()
                for c in range(2):
                    E.mm(po[:, 0:n], pob, gluw[:, c, c2 * 128:(c2 + 1) * 128], gluwb, zbh[c][0][:, 0:n], zbh[c][1], start=c == 0, stop=c == 1)
                a_, ab_ = ta[c2][0][:, 0:n], ta[c2][1]
                E.act(a_, ab_, po[:, 0:n], pob, AF.Sigmoid, bias=glub[:, c2:c2 + 1], xr=[glubb])
                ot, otb = ots[c2]
                E.tt("dve", ot[:, 0:n], otb, zf[c2][0][:, 0:n], zf[c2][1], a_, ab_, MUL)
                yst(512 + c2 * 128, 128, s0, n, ot[:, 0:n], otb)
    k.barrier()
    k.sec('mla')
    mla(nc, k, E, I, l, b, NB, last, Zv, zb, yst, bank, ones_b, ones_bb, inb, castload, fload)


def mla(nc, k, E, I, l, b, NB, last, Zv, zb, yst, bank, ones_b, ones_bb, inb, castload, fload):
    MUL, ADD = ALU.mult, ALU.add
    with ExitStack() as st:
        qag, qagb = fload(st, [128, 2], "mqag", I["qagT"][l])
        kvag, kvagb = fload(st, [128, 1], "mkvag", I["kvagT"][l])
        qg, qgb = fload(st, [96, 1], "mqg", I["qgT"][l])
        kg, kgb = fload(st, [96, 1], "mkg", I["kgT"][l])
        kgpe, kgpeb = fload(st, [32, 1], "mkgpe", I["kgpeT"][l])
        wqb, wqbb = k.sb(st, [128, 2, 384], BF16, "mwqb")
        E.dma("pool", wqb[:, 0, :], wqbb, I["wq_b"][l, 0:128, :], inb, multi=True)
        E.dma("pool", wqb[0:64, 1, :], wqbb, I["wq_b"][l, 128:192, :], inb, multi=True)
        wkvb, wkvbb = castload(st, [128, 512], "mwkvb", I["wkv_b"][l])
        prot, protb = k.sb(st, [96, 96], BF16, "mprot")
        E.ld(prot[:], protb, I["prot"][:, :])
        cqn, cqnb = k.sb(st, [128, 2, S], BF16, "mcqn")
        ckvn, ckvnb = k.sb(st, [128, S], BF16, "mckvn")
        kpef, kpefb = k.sb(st, [32, S], F32, "mkpef")
        kpe2, kpe2b = k.sb(st, [32, S], BF16, "mkpe2")
        z0s = [k.sb(st, [128, 512], F32, f"mz{i}") for i in range(3)]
        sqs = [k.sb(st, [128, 512], BF16, f"msq{i}") for i in range(2)]
        rs, rsb = k.sb(st, [128, 512], F32, "mrs")

        def rstd_from(pm, pmb, m, n, scale):
            E.act(rs[0:m, 0:n], rsb, pm[0:m, 0:n], pmb, AF.Sqrt, bias=EPS, scale=scale)
            E.recip(rs[0:m, 0:n], rsb, rs[0:m, 0:n], rsb)

        E.ld(kpef[:], kpefb, Zv[OFF_KPE:OFF_KPE + 32, :], zb)
        E.act(kpe2[:], kpe2b, kpef[:], kpefb, AF.Square)
        for (s0, n) in XBLK:
            z0, z0b = z0s[0]
            z1, z1b = z0s[1]
            z2, z2b = z0s[2]
            E.ld(z0[:, 0:n], z0b, Zv[OFF_Q:OFF_Q + 128, s0:s0 + n], zb)
            E.ld(z1[0:64, 0:n], z1b, Zv[OFF_Q + 128:OFF_Q + 192, s0:s0 + n], zb)
            E.ld(z2[:, 0:n], z2b, Zv[OFF_KV:OFF_KV + 128, s0:s0 + n], zb)
            E.act(sqs[0][0][:, 0:n], sqs[0][1], z0[:, 0:n], z0b, AF.Square)
            E.act(sqs[1][0][0:64, 0:n], sqs[1][1], z1[0:64, 0:n], z1b, AF.Square)
            pm, pmb = bank()
            E.mm(pm[:, 0:n], pmb, ones_b[:, :], ones_bb, sqs[0][0][:, 0:n], sqs[0][1], start=True, stop=False)
            E.mm(pm[:, 0:n], pmb, ones_b[0:64, :], ones_bb, sqs[1][0][0:64, 0:n], sqs[1][1], start=False, stop=True)
            rstd_from(pm, pmb, 128, n, 1.0 / 192)
            E.stt(cqn[:, 0, s0:s0 + n], cqnb, z0[:, 0:n], z0b, qag[:, 0:1], rs[:, 0:n], rsb, MUL, MUL, xr=[qagb])
            E.stt(cqn[0:64, 1, s0:s0 + n], cqnb, z1[0:64, 0:n], z1b, qag[0:64, 1:2], rs[0:64, 0:n], rsb, MUL, MUL, xr=[qagb])
            E.act(sqs[0][0][:, 0:n], sqs[0][1], z2[:, 0:n], z2b, AF.Square)
            pm, pmb = bank()
            E.mm(pm[:, 0:n], pmb, ones_b[:, :], ones_bb, sqs[0][0][:, 0:n], sqs[0][1])
            rstd_from(pm, pmb, 128, n, 1.0 / 128)
            E.stt(ckvn[:, s0:s0 + n], ckvnb, z2[:, 0:n], z2b, kvag[:, 0:1], rs[:, 0:n], rsb, MUL, MUL, xr=[kvagb])
        Kt, Ktb = k.sb(st, [96, S], BF16, "mKt")
        Qt, Qtb = k.sb(st, [96, S], BF16, "mQt")
        Vh, Vhb = k.sb(st, [128, S // 128, 128], BF16, "mVh")
        E.memset("pool", Vh[:, :, 64:128], Vhb, 1.0)
        knf, knfb = k.sb(st, [96, 512], F32, "mknf")
        knb, knbb = k.sb(st, [96, 512], BF16, "mknb")
        cosb, cosbb = k.sb(st, [96, 512], F32, "mcos")
        sinb, sinbb = k.sb(st, [96, 512], F32, "msin")
        r1, r1b = k.sb(st, [96, 512], F32, "mr1")
        r2, r2b = k.sb(st, [96, 512], F32, "mr2")
        pts = [k.sb(st, [128, 512], BF16, f"mpt{i}") for i in range(3)]
        rd, rdb = k.sb(st, [64, 512], F32, "mrd")
        ots = [k.sb(st, [64, 512], BF16, f"mot{i}") for i in range(2)]
        acc0 = psum0 = None
        SC = 96 ** -0.5

        def norm_rope(pq, pqb, gain_parts, s0, n, dst, dstb):
            isx = s0 >= LC
            for (o0, m, src, srcb, gap, gb) in gain_parts:
                E.stt(knf[o0:o0 + m, 0:n], knfb, src, srcb, gap, rs[0:m, 0:n], rsb, MUL, MUL, xr=[gb])
            if not isx:
                E.cp("act", dst[:, s0:s0 + n], dstb, knf[:, 0:n], knfb)
                return
            xs = s0 - LC
            E.cp("act", knb[:, 0:n], knbb, knf[:, 0:n], knfb)
            pk, pkb = bank()
            E.mm(pk[0:96, 0:n], pkb, prot[:, :], protb, knb[:, 0:n], knbb)
            E.ld(cosb[:, 0:n], cosbb, I["ropecos"][:, xs:xs + n])
            E.ld(sinb[:, 0:n], sinbb, I["ropesin"][:, xs:xs + n])
            E.tt("pool", r1[:, 0:n], r1b, knf[:, 0:n], knfb, cosb[:, 0:n], cosbb, MUL)
            E.tt("dve", r2[:, 0:n], r2b, pk[0:96, 0:n], pkb, sinb[:, 0:n], sinbb, MUL)
            E.tt("dve", dst[:, s0:s0 + n], dstb, r1[:, 0:n], r1b, r2[:, 0:n], r2b, ADD)

        for h in range(4):
            for (s0, n) in XBLK:
                pn, pnb = bank()
                E.mm(pn[0:64, 0:n], pnb, wkvb[:, h * 128:h * 128 + 64], wkvbb, ckvn[:, s0:s0 + n], ckvnb)
                E.act(sqs[0][0][0:64, 0:n], sqs[0][1], pn[0:64, 0:n], pnb, AF.Square)
                pm, pmb = bank()
                E.mm(pm[0:96, 0:n], pmb, ones_b[0:64, 0:96], ones_bb, sqs[0][0][0:64, 0:n], sqs[0][1], start=True, stop=False)
                E.mm(pm[0:96, 0:n], pmb, ones_b[0:32, 0:96], ones_bb, kpe2[0:32, s0:s0 + n], kpe2b, start=False, stop=True)
                rstd_from(pm, pmb, 96, n, 1.0 / 96)
                norm_rope(None, None, [(0, 64, pn[0:64, 0:n], pnb, kg[0:64, 0:1], kgb),
                                       (64, 32, kpef[0:32, s0:s0 + n], kpefb, kgpe[0:32, 0:1], kgpeb)], s0, n, Kt, Ktb)
                if last and s0 < LC:
                    continue
                pq, pqb = bank()
                E.mm(pq[0:96, 0:n], pqb, wqb[:, 0, h * 96:(h + 1) * 96], wqbb, cqn[:, 0, s0:s0 + n], cqnb, start=True, stop=False)
                E.mm(pq[0:96, 0:n], pqb, wqb[0:64, 1, h * 96:(h + 1) * 96], wqbb, cqn[0:64, 1, s0:s0 + n], cqnb, start=False, stop=True)
                E.act(sqs[1][0][0:96, 0:n], sqs[1][1], pq[0:96, 0:n], pqb, AF.Square)
                pm, pmb = bank()
                E.mm(pm[0:96, 0:n], pmb, ones_b[0:96, 0:96], ones_bb, sqs[1][0][0:96, 0:n], sqs[1][1])
                rstd_from(pm, pmb, 96, n, 1.0 / 96)
                norm_rope(None, None, [(0, 96, pq[0:96, 0:n], pqb, qg[0:96, 0:1], qgb)], s0, n, Qt, Qtb)
            for kc in range(S // 128):
                pv, pvb = bank()
                E.mm(pv[:, 0:64], pvb, ckvn[:, kc * 128:(kc + 1) * 128], ckvnb, wkvb[:, h * 128 + 64:h * 128 + 128], wkvbb)
                E.cp("act" if kc % 2 else "dve", Vh[:, kc, 0:64], Vhb, pv[:, 0:64], pvb)

            def attend(q0, nq, kcs, n_):
                po, pob = bank.acc()
                for i, kc in enumerate(kcs):
                    psc, pscb = bank()
                    E.mm(psc[:, 0:nq], pscb, Kt[:, kc * 128:(kc + 1) * 128], Ktb, Qt[:, q0:q0 + nq], Qtb)
                    pt, ptb = pts[i % 3]
                    E.act(pt[:, 0:nq], ptb, psc[:, 0:nq], pscb, AF.Exp, scale=SC)
                    E.mm(po[:, 0:nq], pob, Vh[:, kc, :], Vhb, pt[:, 0:nq], ptb, start=i == 0, stop=i == len(kcs) - 1)
                E.recip(rd[:, 0:nq], rdb, po[64:128, 0:nq], pob)
                ot, otb = ots[n_ % 2]
                E.tt("dve", ot[:, 0:nq], otb, po[0:64, 0:nq], pob, rd[:, 0:nq], rdb, MUL)
                yst(768 + h * 64, 64, q0, nq, ot[:, 0:nq], otb)

            for j in range(4):
                attend(LC + 512 * j, 512, list(range(S // 128)), j)
            if not last:
                attend(0, LC, [0, 1], 0)


def moe(nc, k, E, I, l, NB, last, R, Rb, out, outb, modrows, modrows_b, H2, H2b, Xg, Xgb, Yg, Ygb, bank,
        ident_f, ident_fb, ident_b, ident_bb, ones_b, ones_bb, inb):
    NB1 = NB + 1
    MUL, ADD = ALU.mult, ALU.add
    tiles = [(b, ti) for b in range(NB) for ti in range(2 if last else 0, S // 128)]
    NT = len(tiles)
    NTOK = NT * 128
    NBLK = -(-NTOK * 4 // MB) + NE
    with ExitStack() as top:
        Wall, Wallb = k.sb(top, [128, NT, 4], F32, "Wall")
        IDX, IDXb = k.sb(top, [128, NT, 4], F32, "IDXall")
        Dall, Dallb = k.sb(top, [128, NT, NE], F32, "Dall")
        dest, destb = k.sb(top, [128, NT, 4], I32, "dest")
        base, baseb = k.sb(top, [128, NE], F32, "base")
        iota, iotab = k.sb(top, [128, NE], F32, "iota")
        bexi, bexib = k.sb(top, [128, NBLK], I32, "bexi")
        pstart, pstartb = k.sb(top, [128, NE], F32, "pstart")
        widx, widxb = k.sb(top, [128, NBLK, 8], I32, "widx")
        bidx, bidxb = k.sb(top, [128, NBLK], I32, "bidx")
        oidx, oidxb = k.sb(top, [128, NBLK], I32, "oidx")
        pofs, pofsb = k.sb(top, [128, 9], F32, "pofs")
        E.ld(pofs[:], pofsb, I["pofs"][l])
        cst, cstb = k.sb(top, [128, 4], F32, "cst")
        for ci, cv in enumerate((7.0, 1024.0, 128.0, -7.0)):
            E.memset("pool", cst[:, ci:ci + 1], cstb, cv)
        idxt = [k.sb(top, [128, 1], I32, f"idxt{i}") for i in range(12)]
        ni = [0]

        def stage_idx(src_ap, srcb, eng="dve"):
            t, tb = idxt[ni[0] % 12]
            ni[0] += 1
            E.cp(eng, t[:], tb, src_ap, srcb)
            return t, tb
        E.ld(iota[:], iotab, I["iota32"][:, :])
        E.memset("pool", base[:], baseb, 0.0)
        k.sec('moeA')
        with ExitStack() as st:
            n2g, n2gb = k.sb(st, [128, D], F32, "n2g")
            E.ld(n2g[:], n2gb, I["n2g"][l, 0:1, :].partition_broadcast(128))
            A2, S2 = [], []
            for bi in range(NB1):
                a_, ab_ = k.sb(st, [128, D], F32, f"A2_{bi}")
                s_, sb_ = k.sb(st, [128, D], F32, f"S2_{bi}")
                E.ld(a_[:], ab_, modrows[l, bi:bi + 1, 4 * D:5 * D].partition_broadcast(128), modrows_b)
                E.ld(s_[:], sb_, modrows[l, bi:bi + 1, 3 * D:4 * D].partition_broadcast(128), modrows_b)
                E.stt(a_[:], ab_, a_[:], ab_, 1.0, n2g[:], n2gb, ADD, MUL)
                A2.append((a_, ab_))
                S2.append((s_, sb_))
            rw, rwb = k.sb(st, [128, 8, NE], F32, "rw")
            E.ld(rw[:], rwb, I["router_w"][l].rearrange("(k p) e -> p k e", p=128))
            rbt, rbtb = k.sb(st, [128, NE], F32, "rb")
            E.ld(rbt[:], rbtb, I["router_b"][l, 0:1, :].partition_broadcast(128))
            ustr, ustrb = k.sb(st, [128, 128], BF16, "ustr")
            E.ld(ustr[:], ustrb, I["ustrict"][:, :])
            xts = [k.sb(st, [128, D], F32, f"mxt{i}") for i in range(2)]
            h2s = [k.sb(st, [128, D], F32, f"mh2{i}") for i in range(2)]
            h2bs = [k.sb(st, [128, D], BF16, f"mh2b{i}") for i in range(2)]
            junk, junkb = k.sb(st, [128, D], BF16, "mjunk")
            h2T, h2Tb = k.sb(st, [128, 8, 128], F32, "mh2T")
            st8 = [k.sb(st, [128, 4], F32, f"mst{i}") for i in range(2)]
            lg, lgb = k.sb(st, [128, NE], F32, "mlg")
            v8, v8b = k.sb(st, [128, 8], F32, "mv8")
            i8, i8b = k.sb(st, [128, 8], U32, "mi8")
            ex, exb = k.sb(st, [128, 8], F32, "mex")
            msk, mskb = k.sb(st, [128, NE], F32, "mmsk")
            mskh, mskhb = k.sb(st, [128, NE], BF16, "mmskh")
            for i, (b, ti) in enumerate(tiles):
                bi = b if ti >= 2 else NB
                xt, xtb = xts[i % 2]
                h2, h2b_ = h2s[i % 2]
                hb_, hbb_ = h2bs[i % 2]
                s8, s8b = st8[i % 2]
                E.ld(xt[:], xtb, R[b, ti * 128:(ti + 1) * 128, :], Rb[b][ti])
                E.act(junk[:], junkb, xt[:], xtb, AF.Square, accum=s8[:, 0:1], xw=[s8b])
                E.act(s8[:, 1:2], s8b, s8[:, 0:1], s8b, AF.Sqrt, bias=EPS, scale=1.0 / D)
                E.recip(s8[:, 2:3], s8b, s8[:, 1:2], s8b)
                E.stt(h2[:], h2b_, xt[:], xtb, s8[:, 2:3], A2[bi][0][:], A2[bi][1], MUL, MUL, xr=[s8b])
                E.tt("pool", h2[:], h2b_, h2[:], h2b_, S2[bi][0][:], S2[bi][1], ADD)
                E.cp("act", hb_[:], hbb_, h2[:], h2b_)
                E.dma("sp", H2[i * 128:(i + 1) * 128, :], H2b[i], hb_[:], hbb_, own=hbb_)
                for hf in range(2):
                    p_, pb_ = bank()
                    for q_ in range(4):
                        kk = hf * 4 + q_
                        E.tr(p_[:, q_ * 128:(q_ + 1) * 128], pb_, h2[:, kk * 128:(kk + 1) * 128], h2b_, ident_f[:], ident_fb)
                    E.cp("act" if hf else "dve", h2T[:, hf * 4:hf * 4 + 4, :], h2Tb, p_[:, :].rearrange("p (a b) -> p a b", a=4), pb_)
                pl, plb = bank()
                for kk in range(8):
                    E.mm(pl[:, 0:NE], plb, h2T[:, kk, :], h2Tb, rw[:, kk, :], rwb, start=kk == 0, stop=kk == 7)
                E.tt("dve", lg[:], lgb, pl[:, 0:NE], plb, rbt[:], rbtb, ADD)
                k.op("dve", lambda e: e.max(out=v8[:], in_=lg[:]), r=[lgb], w=[v8b])
                k.op("dve", lambda e: e.max_index(out=i8[:], in_max=v8[:], in_values=lg[:]), r=[lgb, v8b], w=[i8b])
                E.cp("dve", IDX[:, i, :], IDXb, i8[:, 0:4], i8b)
                E.ts("dve", ex[:, 4:5], exb, v8[:, 0:1], v8b, -1.0, None, MUL)
                E.act(ex[:, 0:4], exb, v8[:, 0:4], v8b, AF.Exp, bias=ex[:, 4:5], accum=ex[:, 5:6], xr=[exb])
                E.recip(ex[:, 6:7], exb, ex[:, 5:6], exb)
                E.ts("dve", Wall[:, i, :], Wallb, ex[:, 0:4], exb, ex[:, 6:7], None, MUL)
                E.ts("dve", msk[:], mskb, iota[:], iotab, IDX[:, i, 0:1], None, ALU.is_equal, xr=[IDXb])
                for k4 in range(1, 4):
                    E.stt(msk[:], mskb, iota[:], iotab, IDX[:, i, k4:k4 + 1], msk[:], mskb, ALU.is_equal, ADD, xr=[IDXb])
                E.cp("dve", mskh[:], mskhb, msk[:], mskb)
                pp, ppb = bank()
                E.mm(pp[:, 0:NE], ppb, ustr[:], ustrb, mskh[:], mskhb)
                E.tt("dve", Dall[:, i, :], Dallb, pp[:, 0:NE], ppb, base[:], baseb, ADD)
                pc, pcb = bank()
                E.mm(pc[:, 0:NE], pcb, ones_b[:], ones_bb, mskh[:], mskhb)
                E.tt("dve", base[:], baseb, base[:], baseb, pc[:, 0:NE], pcb, ADD)
        k.barrier()
        k.sec(None)
        with ExitStack() as st:
            nbk, nbkb = k.sb(st, [128, NE], F32, "nbk")
            pend, pendb = k.sb(st, [128, NE], F32, "pend")
            one32, one32b = k.sb(st, [128, NE], F32, "one32")
            blki, blkib = k.sb(st, [128, NBLK], F32, "blki")
            bexf, bexfb = k.sb(st, [128, NBLK], F32, "bexf")
            E.ld(blki[:], blkib, I["blkiota"][:, 0:NBLK])
            E.memset("pool", one32[:], one32b, 1.0)
            E.memset("pool", nbk[:], nbkb, 0.0)
            E.memset("pool", bexf[:], bexfb, 0.0)
            for j in range(-(-NTOK // MB)):
                E.stt(nbk[:], nbkb, base[:], baseb, float(MB * j), nbk[:], nbkb, ALU.is_gt, ADD)
            E.ts("dve", nbk[:], nbkb, nbk[:], nbkb, float(MB), None, MUL)
            E.scan(pend[:], pendb, one32[:], one32b, nbk[:], nbkb, 0.0, None)
            E.tt("dve", pstart[:], pstartb, pend[:], pendb, nbk[:], nbkb, ALU.subtract)
            for e_ in range(NE):
                E.stt(bexf[:], bexfb, blki[:], blkib, pend[:, e_:e_ + 1], bexf[:], bexfb, ALU.is_ge, ADD, xr=[pendb])
            E.ts("dve", bexf[:], bexfb, bexf[:], bexfb, float(NE - 1), None, ALU.min)
            E.cp("dve", bexi[:], bexib, bexf[:], bexfb)
            for kk in range(8):
                E.ts("dve", widx[:, :, kk], widxb, bexf[:], bexfb, cst[:, 1:2], pofs[:, kk:kk + 1], MUL, ADD, xr=[pofsb, cstb])
            E.ts("dve", bidx[:], bidxb, bexf[:], bexfb, cst[:, 2:3], pofs[:, 8:9], MUL, ADD, xr=[pofsb, cstb])
            E.ts("dve", oidx[:], oidxb, bexf[:], bexfb, float(l * NE), None, ADD)
        k.barrier()
        k.sec('moeB')
        with ExitStack() as st:
            Dp, Dpb = k.sb(st, [128, NE], F32, "Dp")
            oh, ohb = k.sb(st, [128, NE], F32, "oh")
            jk, jkb = k.sb(st, [128, NE], F32, "jk")
            dfs = [k.sb(st, [128, 4], F32, f"df{i}") for i in range(2)]
            hts = [k.sb(st, [128, D], BF16, f"ht{i}") for i in range(3)]
            for i in range(NT):
                df, dfb = dfs[i % 2]
                ht, htb = hts[i % 3]
                E.ld(ht[:], htb, H2[i * 128:(i + 1) * 128, :], H2b[i])
                E.tt("dve", Dp[:], Dpb, Dall[:, i, :], Dallb, pstart[:], pstartb, ADD)
                for k4 in range(4):
                    E.ts("dve", oh[:], ohb, iota[:], iotab, IDX[:, i, k4:k4 + 1], None, ALU.is_equal, xr=[IDXb])
                    E.tt("dve", jk[:], jkb, oh[:], ohb, Dp[:], Dpb, MUL)
                    k.op("dve", lambda e, k4=k4, df=df: e.tensor_reduce(out=df[:, k4:k4 + 1], in_=jk[:], axis=mybir.AxisListType.X, op=ADD), r=[jkb], w=[dfb])
                E.cp("dve", dest[:, i, :], destb, df[:], dfb)
                for k4 in range(4):
                    it_, itb_ = stage_idx(dest[:, i, k4:k4 + 1], destb)
                    k.dma("pool", lambda e, it_=it_, ht=ht: e.indirect_dma_start(out=Xg[:, :], out_offset=bass.IndirectOffsetOnAxis(ap=it_[:, 0:1], axis=0), in_=ht[:], in_offset=None),
                          r=[htb, itb_], w=[Xgb], own=htb, multi=True)
        if l == 0:
            dmp = _DUMP[0]
            dmp("dest", dest[:], destb, [128, NT, 4], I32)
            dmp("Wall", Wall[:], Wallb, [128, NT, 4], F32)
            dmp("IDX", IDX[:], IDXb, [128, NT, 4], F32)
            dmp("Dall", Dall[:], Dallb, [128, NT, NE], F32)
            dmp("bexi", bexi[:], bexib, [128, NBLK], I32)
            dmp("widx", widx[:], widxb, [128, NBLK, 8], I32)
            dmp("pstart", pstart[:], pstartb, [128, NE], F32)
            dmp("base", base[:], baseb, [128, NE], F32)
        k.barrier()
        k.sec('moeC')
        with ExitStack() as st:
            wins = [k.sb(st, [128, 8, 2 * D], BF16, f"win{i}") for i in range(2)]
            wouts = [k.sb(st, [128, 8, D], BF16, f"wout{i}") for i in range(2)]
            xrs = [k.sb(st, [128, 4, D], BF16, f"xr{i}") for i in range(2)]
            xT, xTb = k.sb(st, [128, 8, 512], BF16, "xT")
            aT, aTb = k.sb(st, [128, 8, 512], BF16, "aT")
            bins = [k.sb(st, [128, 16], F32, f"bin{i}") for i in range(2)]
            bouts = [k.sb(st, [128, D], F32, f"bout{i}") for i in range(2)]
            ysbs = [k.sb(st, [128, D], F32, f"ysb{i}") for i in range(2)]
            tgs = [k.sb(st, [128, 512], F32, f"tg{i}") for i in range(2)]
            tss = [k.sb(st, [128, 512], F32, f"tsg{i}") for i in range(2)]
            tus = [k.sb(st, [128, 512], F32, f"tu{i}") for i in range(2)]
            ny = 0
            WIN = I["exp_w_in"].rearrange("l e k n -> (l e k) n")
            WOUT = I["exp_w_out"].rearrange("l e k n -> (l e k) n")
            BIN = I["exp_b_inT"].rearrange("l e p c -> (l e p) c")
            BOUT = I["exp_b_out"].rearrange("l e o d -> (l e o) d")
            for j in range(NBLK):
                win, winb = wins[j % 2]
                wout, woutb = wouts[j % 2]
                bin_, binb = bins[j % 2]
                bout, boutb = bouts[j % 2]

                for kk in range(8):
                    it_, itb_ = stage_idx(widx[:, j, kk:kk + 1], widxb)
                    k.dma("pool", lambda e, kk=kk, win=win, it_=it_: e.indirect_dma_start(out=win[:, kk, :], out_offset=None, in_=WIN[:, :], in_offset=bass.IndirectOffsetOnAxis(ap=it_[:, 0:1], axis=0)),
                          r=[itb_, inb], w=[winb], multi=True)
                    k.dma("pool", lambda e, kk=kk, wout=wout, it_=it_: e.indirect_dma_start(out=wout[:, kk, :], out_offset=None, in_=WOUT[:, :], in_offset=bass.IndirectOffsetOnAxis(ap=it_[:, 0:1], axis=0)),
                          r=[itb_, inb], w=[woutb], multi=True)
                it_, itb_ = stage_idx(bidx[:, j:j + 1], bidxb)
                k.dma("pool", lambda e, bin_=bin_, it_=it_: e.indirect_dma_start(out=bin_[:], out_offset=None, in_=BIN[:, :], in_offset=bass.IndirectOffsetOnAxis(ap=it_[:, 0:1], axis=0)),
                      r=[itb_, inb], w=[binb])
                it_, itb_ = stage_idx(oidx[:, j:j + 1], oidxb)
                k.dma("pool", lambda e, bout=bout, it_=it_: e.indirect_dma_start(out=bout[:], out_offset=None, in_=BOUT[:, :], in_offset=bass.IndirectOffsetOnAxis(ap=it_[:, 0:1], axis=0)),
                      r=[itb_, inb], w=[boutb])
                for hl in range(MB // 512):
                    r0 = j * MB + hl * 512
                    xr_, xrb = xrs[(j * (MB // 512) + hl) % 2]
                    E.ld(xr_[:], xrb, Xg[r0:r0 + 512, :].rearrange("(t p) d -> p t d", p=128), Xgb)
                    for kk in range(8):
                        p_, pb_ = bank()
                        pv = p_[:].bitcast(BF16)
                        for tt in range(4):
                            E.tr(pv[:, tt * 128:(tt + 1) * 128], pb_, xr_[:, tt, kk * 128:(kk + 1) * 128], xrb, ident_b[:], ident_bb)
                        E.cp("act" if kk % 2 else "dve", xT[:, kk, :], xTb, pv[:, 0:512], pb_)
                    for fc in range(8):
                        tg, tgb = tgs[fc % 2]
                        tsg, tsgb = tss[fc % 2]
                        tu, tub = tus[fc % 2]
                        pg, pgb = bank()
                        for kk in range(8):
                            E.mm(pg[:, :], pgb, win[:, kk, fc * 128:(fc + 1) * 128], winb, xT[:, kk, :], xTb, start=kk == 0, stop=kk == 7)
                        pu, pub = bank()
                        for kk in range(8):
                            E.mm(pu[:, :], pub, win[:, kk, D + fc * 128:D + (fc + 1) * 128], winb, xT[:, kk, :], xTb, start=kk == 0, stop=kk == 7)
                        E.ts("dve", tg[:], tgb, pg[:, :], pgb, bin_[:, fc:fc + 1], cst[:, 0:1], ADD, ALU.min, xr=[binb, cstb])
                        E.act(tsg[:], tsgb, tg[:], tgb, AF.Silu, scale=1.702)
                        E.ts("dve", tu[:], tub, pu[:, :], pub, bin_[:, 8 + fc:9 + fc], cst[:, 0:1], ADD, ALU.min, xr=[binb, cstb])
                        E.ts("dve", tu[:], tub, tu[:], tub, -7.0, 1.0, ALU.max, ADD)
                        E.stt(aT[:, fc, :], aTb, tu[:], tub, 1.0 / 1.702, tsg[:], tsgb, MUL, MUL)
                    for tt in range(4):
                        ysb, ysbb = ysbs[ny % 2]
                        ny += 1
                        for hf in range(2):
                            py, pyb = bank()
                            for kk in range(8):
                                E.mm(py[:, :], pyb, aT[:, kk, tt * 128:(tt + 1) * 128], aTb, wout[:, kk, hf * 512:(hf + 1) * 512], woutb, start=kk == 0, stop=kk == 7)
                            E.tt("dve", ysb[:, hf * 512:(hf + 1) * 512], ysbb, py[:, :], pyb, bout[:, hf * 512:(hf + 1) * 512], boutb, ADD)
                        for hf in range(2):
                            E.dma("sp", Yg[hf][r0 + tt * 128:r0 + (tt + 1) * 128, :], Ygb, ysb[:, hf * 512:(hf + 1) * 512], ysbb, own=ysbb, multi=True)
        k.barrier()
        k.sec('moeD')
        with ExitStack() as st:
            G2 = []
            for bi in range(NB1):
                g_, gb_ = k.sb(st, [128, D], F32, f"G2_{bi}")
                E.ld(g_[:], gb_, modrows[l, bi:bi + 1, 5 * D:6 * D].partition_broadcast(128), modrows_b)
                G2.append((g_, gb_))
            xts = [k.sb(st, [128, D], F32, f"dxt{i}") for i in range(2)]
            yks = [[k.sb(st, [128, D], F32, f"dy{i}_{q_}") for q_ in range(4)] for i in range(2)]
            ms = [k.sb(st, [128, D], F32, f"dm{i}") for i in range(2)]
            for i, (b, ti) in enumerate(tiles):
                bi = b if ti >= 2 else NB
                xt, xtb = xts[i % 2]
                m_, mb_ = ms[i % 2]
                E.ld(xt[:], xtb, R[b, ti * 128:(ti + 1) * 128, :], Rb[b][ti])
                for k4 in range(4):
                    y_, yb_ = yks[i % 2][k4]
                    it_, itb_ = stage_idx(dest[:, i, k4:k4 + 1], destb)
                    for hf in range(2):
                        k.dma("pool", lambda e, it_=it_, y_=y_, hf=hf: e.indirect_dma_start(out=y_[:, hf * 512:(hf + 1) * 512], out_offset=None, in_=Yg[hf][:, :], in_offset=bass.IndirectOffsetOnAxis(ap=it_[:, 0:1], axis=0)),
                              r=[Ygb, itb_], w=[yb_], multi=True)
                y_, yb_ = yks[i % 2][0]
                E.ts("dve", m_[:], mb_, y_[:], yb_, Wall[:, i, 0:1], None, MUL, xr=[Wallb])
                for k4 in range(1, 4):
                    y_, yb_ = yks[i % 2][k4]
                    E.stt(m_[:], mb_, y_[:], yb_, Wall[:, i, k4:k4 + 1], m_[:], mb_, MUL, ADD, xr=[Wallb])
                E.tt("pool", m_[:], mb_, m_[:], mb_, G2[bi][0][:], G2[bi][1], MUL)
                E.tt("dve", m_[:], mb_, m_[:], mb_, xt[:], xtb, ADD)
                if last:
                    E.dma("sp", out[b, (ti - 2) * 128:(ti - 1) * 128, :], outb[b][ti - 2], m_[:], mb_, own=mb_)
                else:
                    E.dma("sp", R[b, ti * 128:(ti + 1) * 128, :], Rb[b][ti], m_[:], mb_, own=mb_)


_NC_CACHE = {}


def _core_inputs(inp, shared, b0, NB):
    m = dict(shared)
    m["x"] = np.ascontiguousarray(inp["x"][b0:b0 + NB])
    m["ctx"] = np.ascontiguousarray(inp["ctx"][b0:b0 + NB])
    call = np.concatenate([inp["c"][b0:b0 + NB], inp["c_ctx"][None, :]], axis=0)
    m["cT"] = np.ascontiguousarray(np.transpose(call.reshape(NB + 1, 8, 128), (2, 1, 0)))
    return m


def run(inp, NB, n_cores, dbg=(), depth=2):
    inp = {k_: np.asarray(v) for k_, v in inp.items()}
    shared = _prep_shared(inp)
    nblk0 = -(-NB * S * 4 // MB) + NE
    pofs = np.zeros((2, 128, 9), np.float32)
    for l_ in range(2):
        for kk in range(8):
            pofs[l_, :, kk] = l_ * NE * 1024 + kk * 128 + np.arange(128)
        pofs[l_, :, 8] = l_ * NE * 128 + np.arange(128)
    shared["pofs"] = pofs
    shared["blkiota"] = np.broadcast_to((np.arange(nblk0, dtype=np.float32) * MB), (128, nblk0)).copy()
    in_maps = [_core_inputs(inp, shared, c * NB, NB) for c in range(n_cores)]
    specs = {k_: (v.shape, v.dtype) for k_, v in in_maps[0].items()}
    key = (NB, tuple(dbg), depth)
    if key not in _NC_CACHE:
        _NC_CACHE[key] = build(NB, specs, dbg, depth)
    nc = _NC_CACHE[key]
    res = run_bass_kernel_spmd(nc, in_maps, core_ids=list(range(n_cores)))
    return res


def kernel(**inputs):
    n_cores = 8
    NB = inputs["x"].shape[0] // n_cores
    res = run(inputs, NB, n_cores)
    return np.concatenate([r["out"] for r in res.results], axis=0).astype(np.float32)
```

```python
import math
from contextlib import ExitStack
import numpy as np
import ml_dtypes
import concourse.bass as bass
import concourse.mybir as mybir
from concourse.bass_utils import run_bass_kernel_spmd

F32 = mybir.dt.float32
BF16 = mybir.dt.bfloat16
I32 = mybir.dt.int32
U32 = mybir.dt.uint32
ALU = mybir.AluOpType
AF = mybir.ActivationFunctionType

D = 1024
LC = 256
LX = 2048
S = LC + LX
NIN = 1376
OFF_POOL, OFF_Q, OFF_SSM, OFF_KV, OFF_KPE = 512, 768, 960, 1216, 1344
NE = 32
EPS = 1e-6
TS5 = 128
PADW = 2352
NI = 2320
MB = 1536


SKIP = set()


class Buf:
    __slots__ = ("name", "w", "r", "dsem", "dcnt")

    def __init__(self, name):
        self.name = name
        self.w = {}
        self.r = {}
        self.dsem = None
        self.dcnt = 0


class KB:
    ENG = ("pe", "act", "dve", "pool", "sp")

    def __init__(self, nc, stack):
        self.nc = nc
        self.stack = stack
        self.prog = {e: [] for e in self.ENG}
        self.esem = {e: stack.enter_context(nc.semaphore("s_" + e)) for e in self.ENG}
        self.ecnt = {e: 0 for e in self.ENG}
        self.known = {e: {} for e in self.ENG}
        self.dbufs = []
        self.sempool = []
        self.mute = False
        self.nsem = 0
        self.n = 0

    def sb(self, st, shape, dt, name):
        self.n += 1
        nm = f"{name}_{self.n}"
        return st.enter_context(self.nc.sbuf_tensor(nm, list(shape), dt)), Buf(nm)

    def ps(self, st, shape, dt, name):
        self.n += 1
        nm = f"{name}_{self.n}"
        return st.enter_context(self.nc.psum_tensor(nm, list(shape), dt)), Buf(nm)

    def _need(self, e, ev):
        sem, val, who = ev
        kn = self.known[e]
        if kn.get(id(sem), 0) >= val:
            return
        kn[id(sem)] = val
        self.prog[e].append(("wait", sem, val))

    def _deps(self, e, r, w, skip_who=None):
        for b in r:
            for ev in b.w.values():
                self._need(e, ev)
        for b in w:
            for ev in b.w.values():
                if skip_who is not None and ev[2] == skip_who:
                    continue
                self._need(e, ev)
            for ev in b.r.values():
                self._need(e, ev)

    def _record(self, ev, r, w, multi):
        key = id(ev[0])
        for b in r:
            b.r[key] = ev
        for b in w:
            if multi:
                b.w[key] = ev
            else:
                b.w = {key: ev}
            b.r = {}

    def sec(self, name):
        self.mute = name is not None and name in SKIP

    def op(self, e, fn, r=(), w=()):
        if self.mute:
            return
        self._deps(e, r, w, skip_who="pe" if e == "pe" else None)
        self.ecnt[e] += 1
        ev = (self.esem[e], self.ecnt[e], e)
        self.prog[e].append(("ins", fn, self.esem[e], 1))
        self._record(ev, r, w, multi=False)

    def dma(self, q, fn, r=(), w=(), own=None, multi=False):
        if self.mute:
            return
        b0 = own if own is not None else w[0]
        if b0.dsem is None:
            if self.sempool:
                b0.dsem, b0.dcnt = self.sempool.pop()
            else:
                self.nsem += 1
                b0.dsem = self.stack.enter_context(self.nc.semaphore(f"dsem{self.nsem}"))
                b0.dcnt = 0
            self.dbufs.append(b0)
        self._deps(q, r, w, skip_who="dma" if multi else None)
        b0.dcnt += 16
        ev = (b0.dsem, b0.dcnt, "dma")
        self.prog[q].append(("ins", fn, b0.dsem, 16))
        self._record(ev, r, w, multi=multi)

    def raw(self, e, fn, r=()):
        self._deps(e, r, [])
        self.prog[e].append(("raw", fn))

    def barrier(self):
        for e in self.ENG:
            for e2 in self.ENG:
                if e2 != e and self.ecnt[e2] > 0:
                    self._need(e, (self.esem[e2], self.ecnt[e2], e2))
            for b in self.dbufs:
                if b.dcnt > 0:
                    self._need(e, (b.dsem, b.dcnt, "dma"))
        for b in self.dbufs:
            self.sempool.append((b.dsem, b.dcnt))
            b.dsem = None
        self.dbufs = []

    def finish(self):
        self.barrier()
        nc = self.nc
        with nc.Block() as block:
            def mk(ename):
                def body(eng):
                    for it in self.prog[ename]:
                        if it[0] == "wait":
                            eng.wait_ge(it[1], it[2])
                        elif it[0] == "raw":
                            it[1](eng)
                        else:
                            it[1](eng).then_inc(it[2], it[3])
                return body
            block.tensor(mk("pe"))
            block.scalar(mk("act"))
            block.vector(mk("dve"))
            block.gpsimd(mk("pool"))
            block.sync(mk("sp"))


class Emit:
    def __init__(self, k):
        self.k = k
        self.q = 0

    def mm(self, out, ob, lhsT, lb, rhs, rb, start=True, stop=True):
        self.k.op("pe", lambda e: e.matmul(out=out, lhsT=lhsT, rhs=rhs, start=start, stop=stop), r=[lb, rb], w=[ob])

    def tr(self, out, ob, in_, ib, ident, idb):
        self.k.op("pe", lambda e: e.transpose(out=out, in_=in_, identity=ident), r=[ib, idb], w=[ob])

    def act(self, out, ob, in_, ib, func, bias=None, scale=None, accum=None, xr=(), xw=()):
        kw = {}
        if bias is not None:
            kw["bias"] = bias
        if scale is not None:
            kw["scale"] = scale
        if accum is not None:
            kw["accum_out"] = accum
        self.k.op("act", lambda e: e.activation(out=out, in_=in_, func=func, **kw), r=[ib, *xr], w=[ob, *xw])

    def tt(self, eng, out, ob, a, ab, b, bb, op):
        self.k.op(eng, lambda e: e.tensor_tensor(out=out, in0=a, in1=b, op=op), r=[ab, bb], w=[ob])

    def ts(self, eng, out, ob, a, ab, s1, s2, op0, op1=None, xr=(), accum=None, xw=()):
        kw = {}
        if op1 is not None:
            kw["op1"] = op1
        if accum is not None:
            kw["accum_out"] = accum
        self.k.op(eng, lambda e: e.tensor_scalar(out=out, in0=a, scalar1=s1, scalar2=s2, op0=op0, **kw), r=[ab, *xr], w=[ob, *xw])

    def stt(self, out, ob, a, ab, scalar, b, bb, op0, op1, xr=()):
        self.k.op("dve", lambda e: e.scalar_tensor_tensor(out=out, in0=a, scalar=scalar, in1=b, op0=op0, op1=op1), r=[ab, bb, *xr], w=[ob])

    def cp(self, eng, out, ob, in_, ib):
        if eng == "act":
            self.k.op("act", lambda e: e.copy(out=out, in_=in_), r=[ib], w=[ob])
        else:
            self.k.op(eng, lambda e: e.tensor_copy(out=out, in_=in_), r=[ib], w=[ob])

    def memset(self, eng, ap, ob, val):
        self.k.op(eng, lambda e: e.memset(ap, val), w=[ob])

    def recip(self, out, ob, in_, ib):
        self.k.op("dve", lambda e: e.reciprocal(out=out, in_=in_), r=[ib], w=[ob])

    def scan(self, out, ob, d0, d0b, d1, d1b, init, initb):
        r = [d0b, d1b] + ([initb] if initb is not None else [])
        self.k.op("dve", lambda e: e.tensor_tensor_scan(out=out, data0=d0, data1=d1, initial=init, op0=ALU.mult, op1=ALU.add), r=r, w=[ob])

    def dma(self, q, out, ob, in_, ib, own=None, multi=False, **kw):
        r = [ib] if isinstance(ib, Buf) else list(ib)
        self.k.dma(q, lambda e: e.dma_start(out=out, in_=in_, **kw), r=r, w=[ob], own=own, multi=multi)

    def ld(self, out, ob, in_, ib=()):
        self.q ^= 1
        r = [ib] if isinstance(ib, Buf) else list(ib)
        self.k.dma("sp" if self.q else "act", lambda e: e.dma_start(out=out, in_=in_), r=r, w=[ob])


def _pk(v, nchunk):
    sh = v.shape[:-1]
    return np.ascontiguousarray(np.swapaxes(v.reshape(*sh, nchunk, 128), -1, -2))


def _consts():
    c = {}
    c["ident_f"] = np.eye(128, dtype=np.float32)
    c["ident_b"] = np.eye(128).astype(ml_dtypes.bfloat16)
    c["ustrict"] = np.triu(np.ones((128, 128), np.float32), 1).astype(ml_dtypes.bfloat16)
    c["iota32"] = np.broadcast_to(np.arange(NE, dtype=np.float32), (128, NE)).copy()
    t = np.arange(LX)
    row = (t // 64).astype(np.float32)
    col = (t % 64).astype(np.float32)
    inv = (10000.0 ** (-np.arange(0, 16, 2, dtype=np.float32) / 16)).astype(np.float32)
    ang_r = (row[:, None] * inv).astype(np.float32)
    ang_c = (col[:, None] * inv).astype(np.float32)
    cosT = np.ones((96, LX), np.float32)
    sinT = np.zeros((96, LX), np.float32)
    cosT[64:72] = np.cos(ang_r).T
    cosT[72:80] = np.cos(ang_r).T
    sinT[64:72] = np.sin(ang_r).T
    sinT[72:80] = np.sin(ang_r).T
    cosT[80:88] = np.cos(ang_c).T
    cosT[88:96] = np.cos(ang_c).T
    sinT[80:88] = np.sin(ang_c).T
    sinT[88:96] = np.sin(ang_c).T
    c["ropecos"] = cosT
    c["ropesin"] = sinT
    pm = np.zeros((96, 96), np.float32)
    for base in (64, 80):
        for j in range(8):
            pm[base + j, base + 8 + j] = -1.0
            pm[base + 8 + j, base + j] = 1.0
    c["prot"] = np.ascontiguousarray(pm.T).astype(ml_dtypes.bfloat16)
    wins = (2, 4, 8, 16)
    invw = np.zeros((128, 2), np.float32)
    corr = np.ones((128, 2, 4, 8), np.float32)
    for g, win in enumerate(wins):
        cch, half = g // 2, g % 2
        ps = slice(half * 64, half * 64 + 64)
        invw[ps, cch] = 1.0 / win
        for ri, L in ((0, LC), (2, LX)):
            for j in range(8):
                tt = j
                lo, hi = max(tt - win // 2, 0), min(tt + win - 1 - win // 2, L - 1)
                corr[ps, cch, ri, j] = win / float(hi - lo + 1)
                tt = L - 8 + j
                lo, hi = max(tt - win // 2, 0), min(tt + win - 1 - win // 2, L - 1)
                corr[ps, cch, ri + 1, j] = win / float(hi - lo + 1)
    c["pool_invw"] = invw
    c["pool_corr"] = corr
    tau = np.zeros((128, 2, TS5), np.float32)
    tau[:, 0, :] = np.arange(1, TS5 + 1, dtype=np.float32)
    tau[:, 1, :] = np.arange(TS5, 0, -1).astype(np.float32)
    c["tau"] = tau
    return c


def _prep_shared(inp):
    L = 2
    o = {}
    o["ada_w"] = inp["ada_w"]
    o["ada_bT"] = _pk(inp["ada_b"], 48)
    o["ada_brow"] = inp["ada_b"].reshape(L, 1, 6 * D)
    o["n1gT"] = _pk(inp["norm1_g"], 8)
    o["n2g"] = inp["norm2_g"].reshape(L, 1, D)
    o["w_mix_in"] = inp["w_mix_in"]
    o["w_mix_out"] = inp["w_mix_out"]
    o["conv_dwT"] = np.ascontiguousarray(np.transpose(inp["conv_dw"].reshape(L, 31, 2, 128), (0, 3, 2, 1)))
    o["conv_dwbT"] = _pk(inp["conv_dw_b"], 2)
    o["conv_lngT"] = _pk(inp["conv_ln_g"], 2)
    o["conv_lnbT"] = _pk(inp["conv_ln_b"], 2)
    o["conv_pw"] = inp["conv_pw"]
    pw = inp["pool_w"]
    pbd = np.zeros((L, 2, 128, 128), np.float32)
    for g in range(4):
        cch, half = g // 2, g % 2
        pbd[:, cch, half * 64:half * 64 + 64, half * 64:half * 64 + 64] = pw[:, g]
    o["pool_wbd"] = pbd
    o["pool_scT"] = _pk(inp["pool_scale"], 2)

    def st_layout(a):
        return np.ascontiguousarray(np.transpose(a.reshape(L, 2, 8, 2, 64), (0, 1, 3, 4, 2)).reshape(L, 2, 128, 8))
    o["s5_are"] = st_layout(inp["ssm_a_re"])
    o["s5_aim"] = st_layout(inp["ssm_a_im"])
    o["s5_ldt"] = st_layout(np.broadcast_to(inp["ssm_log_dt"][..., None], (L, 2, 16, 64)))
    bbd_re = np.zeros((L, 2, 8, 128, 32), np.float32)
    bbd_im = np.zeros((L, 2, 8, 128, 32), np.float32)
    cbd_re = np.zeros((L, 2, 8, 128, 128), np.float32)
    cbd_im = np.zeros((L, 2, 8, 128, 128), np.float32)
    for g in range(16):
        sc, gi = g // 2, g % 2
        bbd_re[:, :, sc, gi * 64:gi * 64 + 64, gi * 16:gi * 16 + 16] = inp["ssm_b_re"][:, :, g]
        bbd_im[:, :, sc, gi * 64:gi * 64 + 64, gi * 16:gi * 16 + 16] = inp["ssm_b_im"][:, :, g]
        c0 = (sc % 4) * 32 + gi * 16
        cbd_re[:, :, sc, gi * 64:gi * 64 + 64, c0:c0 + 16] = np.swapaxes(inp["ssm_c_re"][:, :, g], -1, -2)
        cbd_im[:, :, sc, gi * 64:gi * 64 + 64, c0:c0 + 16] = np.swapaxes(inp["ssm_c_im"][:, :, g], -1, -2)
    o["s5_bre"], o["s5_bim"], o["s5_cre"], o["s5_cim"] = bbd_re, bbd_im, cbd_re, cbd_im
    o["s5_dT"] = _pk(inp["ssm_d"], 2)
    o["s5_gluw"] = inp["ssm_glu_w"]
    o["s5_glubT"] = _pk(inp["ssm_glu_b"], 2)
    qag = np.zeros((L, 256), np.float32)
    qag[:, :192] = inp["mla_q_a_g"]
    o["qagT"] = _pk(qag, 2)
    o["wq_b"] = inp["mla_wq_b"]
    o["kvagT"] = inp["mla_kv_a_g"].reshape(L, 128, 1)
    o["wkv_b"] = inp["mla_wkv_b"]
    o["qgT"] = inp["mla_q_g"].reshape(L, 96, 1)
    o["kgT"] = inp["mla_k_g"].reshape(L, 96, 1)
    o["kgpeT"] = np.ascontiguousarray(inp["mla_k_g"][:, 64:96]).reshape(L, 32, 1)
    o["router_w"] = inp["router_w"]
    o["router_b"] = inp["router_b"].reshape(L, 1, NE)
    o["exp_w_in"] = inp["exp_w_in"]
    o["exp_b_inT"] = _pk(inp["exp_b_in"], 16)
    o["exp_w_out"] = inp["exp_w_out"]
    o["exp_b_out"] = inp["exp_b_out"].reshape(L, NE, 1, D)
    o.update(_consts())
    return {k_: np.ascontiguousarray(v) for k_, v in o.items()}


XBLK = [(0, 256)] + [(256 + 512 * j, 512) for j in range(4)]


def pcol(s0):
    return 16 + s0 if s0 < LC else 32 + s0


_DUMP = [None]


def build(NB, specs, dbg=(), depth=2):
    NB1 = NB + 1
    nc = bass.Bass("TRN2", target_bir_lowering=False)
    I = {}
    for name, (shape, dt) in specs.items():
        bdt = {np.dtype(np.float32): F32, np.dtype(ml_dtypes.bfloat16): BF16, np.dtype(np.int32): I32}[np.dtype(dt)]
        I[name] = nc.dram_tensor(name, list(shape), bdt, kind="ExternalInput").ap()

    def scr(name, shape, dt=F32):
        kind = "ExternalOutput" if name in dbg else "Internal"
        return nc.dram_tensor(name, list(shape), dt, kind=kind).ap()

    out = nc.dram_tensor("out", [NB, LX, D], F32, kind="ExternalOutput").ap()
    R = scr("R", [NB, S, D])
    modrows = scr("modrows", [2, NB1, 6 * D])
    Zin = scr("Zin", [NB, NIN, S])
    Ycat = scr("Ycat", [NB, D, S], BF16)
    NTOK0 = NB * S
    NBLK0 = -(-NTOK0 * 4 // MB) + NE
    H2 = scr("H2", [NTOK0, D], BF16)
    Xg = scr("Xg", [NBLK0 * MB, D], BF16)
    Yg = [scr(f"Yg{h_}", [NBLK0 * MB, D // 2], F32) for h_ in range(2)]
    Rb = [[Buf(f"R{b}_{t}") for t in range(S // 128)] for b in range(NB)]
    outb = [[Buf(f"o{b}_{t}") for t in range(LX // 128)] for b in range(NB)]
    modrows_b = Buf("modrows")
    Zb = [Buf(f"Z{b}") for b in range(NB)]
    Yb = [Buf(f"Yc{b}") for b in range(NB)]
    H2b = [Buf(f"H2_{t}") for t in range(NTOK0 // 128)]
    Xgb = Buf("Xg")
    Ygb = Buf("Yg")
    inb = Buf("inputs")

    with ExitStack() as top:
        k = KB(nc, top)
        E = Emit(k)

        def dump(name, ap, buf, shape, dt):
            if ("dbg_" + name) not in dbg:
                return
            t_ = nc.dram_tensor("dbg_" + name, list(shape), dt, kind="ExternalOutput").ap()
            E.dma("sp", t_, Buf("dbg_" + name), ap, buf, own=buf)
        _DUMP[0] = dump
        ident_b, ident_bb = k.sb(top, [128, 128], BF16, "identb")
        ident_f, ident_fb = k.sb(top, [128, 128], F32, "identf")
        ones_b, ones_bb = k.sb(top, [128, 128], BF16, "onesb")
        E.ld(ident_b[:], ident_bb, I["ident_b"][:, :])
        E.ld(ident_f[:], ident_fb, I["ident_f"][:, :])
        E.memset("pool", ones_b[:], ones_bb, 1.0)
        modT = [k.sb(top, [128, 48, NB1], F32, f"modT{l}") for l in range(2)]
        psum = [k.ps(top, [128, 512], F32, f"bank{i}") for i in range(8)]
        pi = [0]

        def bank():
            pi[0] = pi[0] % 7 + 1
            return psum[pi[0]]
        bank.acc = lambda: psum[0]

        with ExitStack() as st:
            cT, cTb = k.sb(st, [128, 8, NB1], F32, "cT")
            sT, sTb = k.sb(st, [128, 8, NB1], F32, "sT")
            E.ld(cT[:], cTb, I["cT"][:, :, :])
            E.act(sT[:], sTb, cT[:], cTb, AF.Silu)
            wbl = [k.sb(st, [128, 8, 512], F32, f"adaw{i}") for i in range(2)]
            brw = [k.sb(st, [NB1, 512], F32, f"brow{i}") for i in range(2)]
            rwt = [k.sb(st, [NB1, 512], F32, f"rowt{i}") for i in range(2)]
            abT, abTb = k.sb(st, [128, 2, 48], F32, "abT")
            E.ld(abT[:], abTb, I["ada_bT"].rearrange("l p c -> p l c"))
            it = 0
            for l in range(2):
                wv = I["ada_w"][l].rearrange("(k p) n -> p k n", p=128)
                for cb in range(12):
                    w_, wb_ = wbl[it % 2]
                    br_, brb_ = brw[it % 2]
                    rt_, rtb_ = rwt[it % 2]
                    it += 1
                    E.ld(w_[:], wb_, wv[:, :, cb * 512:(cb + 1) * 512])
                    E.ld(br_[:], brb_, I["ada_brow"][l, 0:1, cb * 512:(cb + 1) * 512].partition_broadcast(NB1))
                    for j in range(4):
                        ch = cb * 4 + j
                        p_, pb_ = bank()
                        for kk in range(8):
                            E.mm(p_[:, 0:NB1], pb_, w_[:, kk, j * 128:(j + 1) * 128], wb_, sT[:, kk, :], sTb, start=kk == 0, stop=kk == 7)
                        E.ts("dve", modT[l][0][:, ch, :], modT[l][1], p_[:, 0:NB1], pb_, abT[:, l, ch:ch + 1], None, ALU.add, xr=[abTb])
                    p_, pb_ = bank()
                    for kk in range(8):
                        E.mm(p_[0:NB1, :], pb_, sT[:, kk, :], sTb, w_[:, kk, :], wb_, start=kk == 0, stop=kk == 7)
                    E.tt("dve", rt_[:], rtb_, p_[0:NB1, :], pb_, br_[:], brb_, ALU.add)
                    E.dma("sp", modrows[l, :, cb * 512:(cb + 1) * 512], modrows_b, rt_[:], rtb_, own=rtb_, multi=True)
        k.barrier()

        for l in range(depth):
            last = l == 1
            mT, mTb = modT[l]
            for b in range(NB):
                def src_tile(ti):
                    if l == 0:
                        return (I["ctx"][b, ti * 128:(ti + 1) * 128, :] if ti < 2 else I["x"][b, (ti - 2) * 128:(ti - 1) * 128, :]), inb
                    return R[b, ti * 128:(ti + 1) * 128, :], Rb[b][ti]
                with ExitStack() as st:
                    wmi, wmib = k.sb(st, [128, 8, NIN], BF16, "wmi")
                    wv = I["w_mix_in"][l].rearrange("(k p) n -> p k n", p=128)
                    for h_ in range(4):
                        E.dma("pool", wmi[:, 2 * h_:2 * h_ + 2, :], wmib, wv[:, 2 * h_:2 * h_ + 2, :], inb, multi=True)
                    n1g, n1gb = k.sb(st, [128, 8], F32, "n1g")
                    E.ld(n1g[:], n1gb, I["n1gT"][l])
                    A1, A1b = k.sb(st, [128, 8, 2], F32, "A1")
                    for j, bi in enumerate((b, NB)):
                        k.op("dve", lambda e, j=j, bi=bi, A1=A1, mT=mT, n1g=n1g: e.scalar_tensor_tensor(out=A1[:, :, j], in0=mT[:, 8:16, bi], scalar=1.0, in1=n1g[:, :], op0=ALU.add, op1=ALU.mult), r=[mTb, n1gb], w=[A1b])
                    xts = [k.sb(st, [128, D], F32, f"xt{i}") for i in range(3)]
                    xns = [k.sb(st, [128, D], BF16, f"xn{i}") for i in range(2)]
                    junk, junkb = k.sb(st, [128, D], BF16, "junk")
                    st8 = [k.sb(st, [128, 4], F32, f"st{i}") for i in range(2)]
                    hTs = [k.sb(st, [128, 8, 512], BF16, f"hT{i}") for i in range(2)]
                    zts = [k.sb(st, [128, 512], F32, f"zt{i}") for i in range(3)]
                    nt = 0
                    nz = 0
                    for bi_, (s0, ntok) in enumerate(XBLK):
                        hT, hTb = hTs[bi_ % 2]
                        for tt in range(ntok // 128):
                            ti = (s0 + tt * 128) // 128
                            j = 0 if ti >= 2 else 1
                            bi = b if ti >= 2 else NB
                            xt, xtb = xts[nt % 3]
                            xn, xnb = xns[nt % 2]
                            s8, s8b = st8[nt % 2]
                            nt += 1
                            sap, sbuf_ = src_tile(ti)
                            E.ld(xt[:], xtb, sap, sbuf_)
                            E.act(junk[:], junkb, xt[:], xtb, AF.Square, accum=s8[:, 0:1], xw=[s8b])
                            E.act(s8[:, 1:2], s8b, s8[:, 0:1], s8b, AF.Sqrt, bias=EPS, scale=1.0 / D)
                            E.recip(s8[:, 2:3], s8b, s8[:, 1:2], s8b)
                            E.ts("dve", xn[:], xnb, xt[:], xtb, s8[:, 2:3], None, ALU.mult, xr=[s8b])
                            p_, pb_ = bank()
                            pv = p_[:].bitcast(BF16)
                            for kk in range(8):
                                E.tr(pv[:, kk * 128:(kk + 1) * 128], pb_, xn[:, kk * 128:(kk + 1) * 128], xnb, ident_b[:], ident_bb)
                            for kk in range(8):
                                o_ = hT[:, kk, tt * 128:(tt + 1) * 128]
                                i_ = pv[:, kk * 128:(kk + 1) * 128]
                                if kk % 2 == 0:
                                    E.act(o_, hTb, i_, pb_, AF.Identity, bias=mT[:, kk, bi:bi + 1], scale=A1[:, kk, j:j + 1], xr=[mTb, A1b])
                                else:
                                    E.ts("dve", o_, hTb, i_, pb_, A1[:, kk, j:j + 1], mT[:, kk, bi:bi + 1], ALU.mult, ALU.add, xr=[mTb, A1b])
                        for c0 in range(0, NIN, 128):
                            m = min(128, NIN - c0)
                            p_, pb_ = bank()
                            for kk in range(8):
                                E.mm(p_[0:m, 0:ntok], pb_, wmi[:, kk, c0:c0 + m], wmib, hT[:, kk, 0:ntok], hTb, start=kk == 0, stop=kk == 7)
                            zt, ztb = zts[nz % 3]
                            E.cp("act" if nz % 2 == 0 else "dve", zt[0:m, 0:ntok], ztb, p_[0:m, 0:ntok], pb_)
                            nz += 1
                            E.dma("sp", Zin[b, c0:c0 + m, s0:s0 + ntok], Zb[b], zt[0:m, 0:ntok], ztb, own=ztb, multi=True)
                k.barrier()
                mixers(nc, k, E, I, l, b, NB, last, Zin, Zb, Ycat, Yb, mT, mTb, bank, ident_b, ident_bb, ident_f, ident_fb, ones_b, ones_bb, inb)
                k.sec(None)
                k.barrier()
                with ExitStack() as st:
                    wmo, wmob = k.sb(st, [128, 8, D], BF16, "wmo")
                    wv = I["w_mix_out"][l].rearrange("(k p) n -> p k n", p=128)
                    for h_ in range(4):
                        E.dma("pool", wmo[:, 2 * h_:2 * h_ + 2, :], wmob, wv[:, 2 * h_:2 * h_ + 2, :], inb, multi=True)
                    g1t = [k.sb(st, [128, D], F32, f"g1_{i}") for i in range(2)]
                    E.ld(g1t[0][0][:], g1t[0][1], modrows[l, b:b + 1, 2 * D:3 * D].partition_broadcast(128), modrows_b)
                    E.ld(g1t[1][0][:], g1t[1][1], modrows[l, NB:NB1, 2 * D:3 * D].partition_broadcast(128), modrows_b)
                    ycs = [k.sb(st, [128, 8, 128], BF16, f"yc{i}") for i in range(2)]
                    xts = [k.sb(st, [128, D], F32, f"xr{i}") for i in range(2)]
                    xos = [k.sb(st, [128, D], F32, f"xo{i}") for i in range(2)]
                    yv = Ycat[b].rearrange("(k p) t -> p k t", p=128)
                    for n_, ti in enumerate(range(2 if last else 0, S // 128)):
                        yc, ycb = ycs[n_ % 2]
                        xt, xtb = xts[n_ % 2]
                        xo, xob = xos[n_ % 2]
                        g1, g1b = g1t[0] if ti >= 2 else g1t[1]
                        E.ld(yc[:], ycb, yv[:, :, ti * 128:(ti + 1) * 128], Yb[b])
                        sap, sbuf_ = src_tile(ti)
                        E.ld(xt[:], xtb, sap, sbuf_)
                        for hf in range(2):
                            p_, pb_ = bank()
                            for kk in range(8):
                                E.mm(p_[:, :], pb_, yc[:, kk, :], ycb, wmo[:, kk, hf * 512:(hf + 1) * 512], wmob, start=kk == 0, stop=kk == 7)
                            E.tt("dve", xo[:, hf * 512:(hf + 1) * 512], xob, p_[:, :], pb_, g1[:, hf * 512:(hf + 1) * 512], g1b, ALU.mult)
                        E.tt("pool", xo[:], xob, xo[:], xob, xt[:], xtb, ALU.add)
                        E.dma("sp", R[b, ti * 128:(ti + 1) * 128, :], Rb[b][ti], xo[:], xob, own=xob)
                k.barrier()
            moe(nc, k, E, I, l, NB, last, R, Rb, out, outb, modrows, modrows_b, H2, H2b, Xg, Xgb, Yg, Ygb, bank,
                ident_f, ident_fb, ident_b, ident_bb, ones_b, ones_bb, inb)
            k.sec(None)
            k.barrier()
        k.finish()
    return nc


IBLK = [(0, 256, 0)] + [(272 + 512 * j, 512, 256 + 512 * j) for j in range(4)]
TWO_PI = 2.0 * math.pi


def mixers(nc, k, E, I, l, b, NB, last, Zin, Zb, Ycat, Yb, mT, mTb, bank, ident_b, ident_bb, ident_f, ident_fb,
           ones_b, ones_bb, inb):
    Zv = Zin[b]
    Yv = Ycat[b]
    zb = Zb[b]
    yb = Yb[b]

    def yst(row0, nrow, s0, n, src, srcb):
        E.dma("sp", Yv[row0:row0 + nrow, s0:s0 + n], yb, src, srcb, own=srcb, multi=True)

    def castload(st, shape, name, src):
        t, tb = k.sb(st, shape, BF16, name)
        E.dma("pool", t[:], tb, src, inb)
        return t, tb

    def fload(st, shape, name, src):
        t, tb = k.sb(st, shape, F32, name)
        E.ld(t[:], tb, src)
        return t, tb

    k.sec('conv')
    with ExitStack() as st:
        U = [k.sb(st, [128, PADW], F32, f"cU{c}") for c in range(2)]
        acc = [k.sb(st, [128, NI], F32, f"cacc{c}") for c in range(2)]
        gt, gtb = k.sb(st, [128, NI], F32, "cgt")
        dw, dwb_ = fload(st, [128, 2, 31], "cdw", I["conv_dwT"][l])
        dwbias, dwbiasb = fload(st, [128, 2], "cdwb", I["conv_dwbT"][l])
        lng, lngb = fload(st, [128, 2], "clng", I["conv_lngT"][l])
        lnb, lnbb = fload(st, [128, 2], "clnb", I["conv_lnbT"][l])
        pw, pwb = castload(st, [128, 2, 256], "cpw", I["conv_pw"][l].rearrange("(c p) n -> p c n", p=128))
        onesf, onesfb = k.sb(st, [128, 128], F32, "onesf")
        E.memset("pool", onesf[:], onesfb, 1.0 / 256.0)
        for c in range(2):
            u, ub = U[c]
            a, ab = acc[c]
            for (c0, c1) in ((0, 16), (272, 288), (2336, PADW)):
                E.memset("pool", u[:, c0:c1], ub, 0.0)
            E.memset("pool", gt[:, 256:272], gtb, 0.0)
            k.dma("sp", lambda e, u=u, c=c: e.dma_start(out=u[:, 16:272], in_=Zv[c * 128:(c + 1) * 128, 0:LC]), r=[zb], w=[ub], multi=True)
            k.dma("sp", lambda e, u=u, c=c: e.dma_start(out=u[:, 288:2336], in_=Zv[c * 128:(c + 1) * 128, LC:S]), r=[zb], w=[ub], multi=True)
            k.dma("sp", lambda e, c=c: e.dma_start(out=gt[:, 0:256], in_=Zv[256 + c * 128:256 + (c + 1) * 128, 0:LC]), r=[zb], w=[gtb], multi=True)
            k.dma("sp", lambda e, c=c: e.dma_start(out=gt[:, 272:NI], in_=Zv[256 + c * 128:256 + (c + 1) * 128, LC:S]), r=[zb], w=[gtb], multi=True)
            E.act(gt[:], gtb, gt[:], gtb, AF.Sigmoid)
            E.tt("dve", u[:, 16:2336], ub, u[:, 16:2336], ub, gt[:], gtb, ALU.mult)
            E.ts("dve", a[:], ab, u[:, 1:1 + NI], ub, dw[:, c, 0:1], dwbias[:, c:c + 1], ALU.mult, ALU.add, xr=[dwb_, dwbiasb])
            for kk in range(1, 31):
                E.stt(a[:], ab, u[:, 1 + kk:1 + kk + NI], ub, dw[:, c, kk:kk + 1], a[:], ab, ALU.mult, ALU.add, xr=[dwb_])
        sq = [k.sb(st, [128, 512], F32, f"csq{c}") for c in range(2)]
        vs = [k.sb(st, [128, 512], BF16, f"cvs{c}") for c in range(2)]
        tm, tmb = k.sb(st, [128, 512], F32, "ctm")
        tv, tvb = k.sb(st, [128, 512], F32, "ctv")
        t1s = [k.sb(st, [128, 512], F32, f"ct1{c}") for c in range(2)]
        ots = [k.sb(st, [128, 512], BF16, f"cot{c}") for c in range(2)]
        for (i0, n, s0) in IBLK:
            pm, pmb = bank()
            pe2, pe2b = bank()
            for c in range(2):
                E.act(sq[c][0][:, 0:n], sq[c][1], acc[c][0][:, i0:i0 + n], acc[c][1], AF.Square)
            for c in range(2):
                E.mm(pm[:, 0:n], pmb, onesf[:], onesfb, acc[c][0][:, i0:i0 + n], acc[c][1], start=c == 0, stop=c == 1)
            for c in range(2):
                E.mm(pe2[:, 0:n], pe2b, onesf[:], onesfb, sq[c][0][:, 0:n], sq[c][1], start=c == 0, stop=c == 1)
            E.act(tm[:, 0:n], tmb, pm[:, 0:n], pmb, AF.Square)
            E.tt("dve", tv[:, 0:n], tvb, pe2[:, 0:n], pe2b, tm[:, 0:n], tmb, ALU.subtract)
            E.act(tv[:, 0:n], tvb, tv[:, 0:n], tvb, AF.Sqrt, bias=EPS)
            E.recip(tv[:, 0:n], tvb, tv[:, 0:n], tvb)
            for c in range(2):
                t1, t1b = t1s[c]
                E.tt("dve", t1[:, 0:n], t1b, acc[c][0][:, i0:i0 + n], acc[c][1], pm[:, 0:n], pmb, ALU.subtract)
                E.tt("pool", t1[:, 0:n], t1b, t1[:, 0:n], t1b, tv[:, 0:n], tvb, ALU.mult)
                E.act(vs[c][0][:, 0:n], vs[c][1], t1[:, 0:n], t1b, AF.Silu, bias=lnb[:, c:c + 1], scale=lng[:, c:c + 1], xr=[lnbb, lngb])
            for c2 in range(2):
                po, pob = bank()
                for c in range(2):
                    E.mm(po[:, 0:n], pob, pw[:, c, c2 * 128:(c2 + 1) * 128], pwb, vs[c][0][:, 0:n], vs[c][1], start=c == 0, stop=c == 1)
                ot, otb = ots[c2]
                E.cp("act", ot[:, 0:n], otb, po[:, 0:n], pob)
                yst(c2 * 128, 128, s0, n, ot[:, 0:n], otb)
    k.barrier()

    k.sec('pool')
    with ExitStack() as st:
        u, ub = k.sb(st, [128, PADW], F32, "pU")
        A, Ab = k.sb(st, [128, PADW], F32, "pA")
        Bt, Btb = k.sb(st, [128, PADW], F32, "pB")
        PL, PLb_ = k.sb(st, [128, NI], F32, "pPL")
        PLh, PLhb = k.sb(st, [128, NI], BF16, "pPLh")
        invw, invwb = fload(st, [128, 2], "pinvw", I["pool_invw"][:, :])
        corr, corrb = fload(st, [128, 2, 4, 8], "pcorr", I["pool_corr"][:, :, :, :])
        psc, pscb = fload(st, [128, 2], "ppsc", I["pool_scT"][l])
        pwbd, pwbdb = castload(st, [128, 2, 128], "ppw", I["pool_wbd"][l].rearrange("c p n -> p c n"))
        ots = [k.sb(st, [128, 512], BF16, f"pot{c}") for c in range(2)]
        for (c0, c1) in ((0, 16), (272, 288), (2336, PADW)):
            E.memset("pool", u[:, c0:c1], ub, 0.0)
        for c in range(2):
            r0 = OFF_POOL + c * 128
            k.dma("sp", lambda e, r0=r0: e.dma_start(out=u[:, 16:272], in_=Zv[r0:r0 + 128, 0:LC]), r=[zb], w=[ub], multi=True)
            k.dma("sp", lambda e, r0=r0: e.dma_start(out=u[:, 288:2336], in_=Zv[r0:r0 + 128, LC:S]), r=[zb], w=[ub], multi=True)
            W_ = PADW
            E.tt("dve", A[:, 1:W_], Ab, u[:, 0:W_ - 1], ub, u[:, 1:W_], ub, ALU.add)
            E.tt("pool", Bt[:, 2:W_ - 1], Btb, A[:, 1:W_ - 2], Ab, A[:, 3:W_], Ab, ALU.add)
            if c == 1:
                E.tt("dve", A[:, 4:W_ - 3], Ab, Bt[:, 2:W_ - 5], Btb, Bt[:, 6:W_ - 1], Btb, ALU.add)
                E.tt("pool", Bt[:, 8:W_ - 7], Btb, A[:, 4:W_ - 11], Ab, A[:, 12:W_ - 3], Ab, ALU.add)
            E.ts("dve", PL[0:64, :], PLb_, A[0:64, 16:2336], Ab, invw[0:64, c:c + 1], None, ALU.mult, xr=[invwb])
            E.ts("dve", PL[64:128, :], PLb_, Bt[64:128, 16:2336], Btb, invw[64:128, c:c + 1], None, ALU.mult, xr=[invwb])
            for r_, i0 in enumerate((0, 248, 272, 2312)):
                E.tt("dve", PL[:, i0:i0 + 8], PLb_, PL[:, i0:i0 + 8], PLb_, corr[:, c, r_, :], corrb, ALU.mult)
            E.tt("dve", PLh[:], PLhb, PL[:], PLb_, u[:, 16:2336], ub, ALU.subtract)
            for n_, (i0, n, s0) in enumerate(IBLK):
                po, pob = bank()
                E.mm(po[:, 0:n], pob, pwbd[:, c, :], pwbdb, PLh[:, i0:i0 + n], PLhb)
                ot, otb = ots[n_ % 2]
                E.ts("dve", ot[:, 0:n], otb, po[:, 0:n], pob, psc[:, c:c + 1], None, ALU.mult, xr=[pscb])
                yst(256 + c * 128, 128, s0, n, ot[:, 0:n], otb)
    k.barrier()

    k.sec('s5')
    with ExitStack() as st:
        are, areb = fload(st, [128, 2, 8], "sare", I["s5_are"][l].rearrange("d p s -> p d s"))
        aim, aimb = fload(st, [128, 2, 8], "saim", I["s5_aim"][l].rearrange("d p s -> p d s"))
        dt_, dtb = fload(st, [128, 2, 8], "sldt", I["s5_ldt"][l].rearrange("d p s -> p d s"))
        bre, breb = fload(st, [128, 2, 8, 32], "sbre", I["s5_bre"][l].rearrange("d s p c -> p d s c"))
        bim, bimb = fload(st, [128, 2, 8, 32], "sbim", I["s5_bim"][l].rearrange("d s p c -> p d s c"))
        tau, taub = fload(st, [128, 2, TS5], "stau", I["tau"][:, :, :])
        dsk, dskb = fload(st, [128, 2], "sd", I["s5_dT"][l])
        glub, glubb = fload(st, [128, 2], "sglub", I["s5_glubT"][l])
        gluw, gluwb = castload(st, [128, 2, 256], "sgluw", I["s5_gluw"][l].rearrange("(c p) n -> p c n", p=128))
        CB, CBb = k.sb(st, [128, 2, 8, 2, 128], BF16, "sCB")
        for d in range(2):
            E.dma("pool", CB[:, d, :, 0, :], CBb, I["s5_cre"][l, d].rearrange("s p n -> p s n"), inb, multi=True)
            E.dma("pool", CB[:, d, :, 1, :], CBb, I["s5_cim"][l, d].rearrange("s p n -> p s n"), inb, multi=True)
        for d in range(2):
            k.op("act", lambda e, d=d: e.mul(out=CB[:, d, :, 1, :], in_=CB[:, d, :, 1, :], constant=-1.0) if False else e.activation(out=CB[:, d, :, 1, :], in_=CB[:, d, :, 1, :], func=AF.Copy, scale=-1.0), r=[CBb], w=[CBb])
        LB, LBb = k.sb(st, [128, 2, 8, 2, 128], BF16, "sLB")
        E.memset("pool", LB[:], LBb, 0.0)
        mag, magb = k.sb(st, [128, 2, 8], F32, "smag")
        th, thb = k.sb(st, [128, 2, 8], F32, "sth")
        sc_ = {n_: k.sb(st, [128, 2, 8], F32, "s" + n_) for n_ in ("cs", "sn", "abr", "abi", "den", "fre", "fim", "nfim", "q1", "q2")}
        rt1, rt1b = k.sb(st, [128, TS5], F32, "rt1")
        rti, rtib = k.sb(st, [128, TS5], I32, "rti")

        def rr(x, xb, n, both=True):
            E.ts("dve", rt1[:, 0:n], rt1b, x, xb, 1.0 / TWO_PI, None, ALU.mult)
            E.cp("dve", rti[:, 0:n], rtib, rt1[:, 0:n], rt1b)
            E.cp("dve", rt1[:, 0:n], rt1b, rti[:, 0:n], rtib)
            E.stt(x, xb, rt1[:, 0:n], rt1b, -TWO_PI, x, xb, ALU.mult, ALU.add)
            E.ts("dve", rt1[:, 0:n], rt1b, x, xb, math.pi, -TWO_PI, ALU.is_gt, ALU.mult)
            E.tt("dve", x, xb, x, xb, rt1[:, 0:n], rt1b, ALU.add)
            E.ts("dve", rt1[:, 0:n], rt1b, x, xb, -math.pi, TWO_PI, ALU.is_lt, ALU.mult)
            E.tt("dve", x, xb, x, xb, rt1[:, 0:n], rt1b, ALU.add)

        def sincos(ang, angb, n, osin, osinb, ocos, ocosb):
            rr(ang, angb, n)
            E.act(osin, osinb, ang, angb, AF.Sin)
            E.ts("dve", ang, angb, ang, angb, math.pi / 2, None, ALU.add)
            rr(ang, angb, n)
            E.act(ocos, ocosb, ang, angb, AF.Sin)

        f2 = lambda t: t[:].rearrange("p d s -> p (d s)")
        V = {n_: (f2(t), tb) for n_, (t, tb) in sc_.items()}
        aref, aimf, dtf, magf, thf = f2(are), f2(aim), f2(dt_), f2(mag), f2(th)
        E.ts("dve", aref, areb, aref, areb, -1e-4, None, ALU.min)
        E.act(dtf, dtb, dtf, dtb, AF.Exp)
        E.tt("dve", magf, magb, aref, areb, dtf, dtb, ALU.mult)
        E.act(magf, magb, magf, magb, AF.Exp)
        E.tt("dve", thf, thb, aimf, aimb, dtf, dtb, ALU.mult)
        q1, q1b = V["q1"]
        q2, q2b = V["q2"]
        E.cp("dve", q1, q1b, thf, thb)
        sincos(q1, q1b, 16, V["sn"][0], V["sn"][1], V["cs"][0], V["cs"][1])
        E.tt("dve", V["abr"][0], V["abr"][1], magf, magb, V["cs"][0], V["cs"][1], ALU.mult)
        E.tt("dve", V["abi"][0], V["abi"][1], magf, magb, V["sn"][0], V["sn"][1], ALU.mult)
        E.tt("dve", q1, q1b, aref, areb, aref, areb, ALU.mult)
        E.tt("dve", q2, q2b, aimf, aimb, aimf, aimb, ALU.mult)
        E.tt("dve", V["den"][0], V["den"][1], q1, q1b, q2, q2b, ALU.add)
        E.recip(V["den"][0], V["den"][1], V["den"][0], V["den"][1])
        E.ts("dve", V["abr"][0], V["abr"][1], V["abr"][0], V["abr"][1], -1.0, None, ALU.add)
        E.tt("dve", q1, q1b, V["abr"][0], V["abr"][1], aref, areb, ALU.mult)
        E.tt("dve", q2, q2b, V["abi"][0], V["abi"][1], aimf, aimb, ALU.mult)
        E.tt("dve", q1, q1b, q1, q1b, q2, q2b, ALU.add)
        E.tt("dve", V["fre"][0], V["fre"][1], q1, q1b, V["den"][0], V["den"][1], ALU.mult)
        E.tt("dve", q1, q1b, V["abi"][0], V["abi"][1], aref, areb, ALU.mult)
        E.tt("dve", q2, q2b, V["abr"][0], V["abr"][1], aimf, aimb, ALU.mult)
        E.tt("dve", q1, q1b, q1, q1b, q2, q2b, ALU.subtract)
        E.tt("dve", V["fim"][0], V["fim"][1], q1, q1b, V["den"][0], V["den"][1], ALU.mult)
        E.ts("dve", V["nfim"][0], V["nfim"][1], V["fim"][0], V["fim"][1], -1.0, None, ALU.mult)
        fre, freb = sc_["fre"]
        fim, fimb = sc_["fim"]
        nfim, nfimb = sc_["nfim"]
        bbs = [k.sb(st, [128, 32], F32, f"sbb{i}") for i in range(2)]
        cosT, cosTb = k.sb(st, [128, 2, 8, TS5], F32, "scosT")
        sinT, sinTb = k.sb(st, [128, 2, 8, TS5], F32, "ssinT")
        ang, angb = k.sb(st, [128, TS5], F32, "sang")
        for d in range(2):
            for sc in range(8):
                bbr, bbrb = bbs[0]
                bbi, bbib = bbs[1]
                E.ts("dve", bbr[:], bbrb, bre[:, d, sc, :], breb, fre[:, d, sc:sc + 1], None, ALU.mult, xr=[freb])
                E.stt(bbr[:], bbrb, bim[:, d, sc, :], bimb, nfim[:, d, sc:sc + 1], bbr[:], bbrb, ALU.mult, ALU.add, xr=[nfimb])
                E.ts("dve", bbi[:], bbib, bim[:, d, sc, :], bimb, fre[:, d, sc:sc + 1], None, ALU.mult, xr=[freb])
                E.stt(bbi[:], bbib, bre[:, d, sc, :], breb, fim[:, d, sc:sc + 1], bbi[:], bbib, ALU.mult, ALU.add, xr=[fimb])
                r0 = (sc % 4) * 32
                for ri, (bb_, bbb_) in enumerate(((bbr, bbrb), (bbi, bbib))):
                    p_, pb_ = bank()
                    E.tr(p_[0:32, 0:128], pb_, bb_[:], bbb_, ident_f[:], ident_fb)
                    E.cp("act", LB[r0:r0 + 32, d, sc, ri, :], LBb, p_[0:32, 0:128], pb_)
                E.ts("dve", ang[:], angb, tau[:, d, :], taub, th[:, d, sc:sc + 1], None, ALU.mult, xr=[thb])
                sincos(ang[:], angb, TS5, sinT[:, d, sc, :], sinTb, cosT[:, d, sc, :], cosTb)
        Uf = [k.sb(st, [128, S], F32, f"sUf{c}") for c in range(2)]
        Ubf, Ubfb = k.sb(st, [128, 2, S], BF16, "sUb")
        yac = [k.sb(st, [128, S], F32, f"syac{c}") for c in range(2)]
        for c in range(2):
            r0 = OFF_SSM + c * 128
            E.ld(Uf[c][0][:], Uf[c][1], Zv[r0:r0 + 128, :], zb)
            E.cp("act", Ubf[:, c, :], Ubfb, Uf[c][0][:], Uf[c][1])
            E.ts("dve", yac[c][0][:], yac[c][1], Uf[c][0][:], Uf[c][1], dsk[:, c:c + 1], None, ALU.mult, xr=[dskb])
        carry, carryb = k.sb(st, [128, 2, 8], F32, "scarry")
        cbufs = [Buf(f"carry{i}") for i in range(8)]
        names = ("t1", "t2", "t3", "t4", "wre", "wim", "gre", "gim", "u1", "u2", "u3", "u4", "hre", "him")
        sets = [{n_: k.sb(st, [128, TS5], F32, f"s{n_}{i}") for n_ in names} for i in range(2)]
        hb, hbb = k.sb(st, [128, 8, 2, TS5], BF16, "shb")
        hbufs = [[Buf(f"hb{sc}_{ri}") for ri in range(2)] for sc in range(8)]
        MUL, ADD, SUB = ALU.mult, ALU.add, ALU.subtract
        for d in range(2):
            k.op("pool", lambda e: e.memset(carry[:], 0.0), w=cbufs)
            order = list(range(S // TS5)) if d == 0 else [1, 0] + list(range(S // TS5 - 1, 1, -1))
            for ch in order:
                cols = slice(ch * TS5, (ch + 1) * TS5)
                for sc in range(8):
                    c = sc // 4
                    T_ = sets[sc % 2]
                    pB, pBb = bank()
                    E.mm(pB[:, 0:TS5], pBb, LB[:, d, sc, 0, :], LBb, Ubf[:, c, cols], Ubfb)
                    E.mm(pB[:, TS5:2 * TS5], pBb, LB[:, d, sc, 1, :], LBb, Ubf[:, c, cols], Ubfb)
                    br, bi_ = pB[:, 0:TS5], pB[:, TS5:2 * TS5]
                    cs, sn = cosT[:, d, sc, :], sinT[:, d, sc, :]
                    g = lambda n_: (T_[n_][0][:], T_[n_][1])
                    E.tt("dve", *g("t1"), br, pBb, cs, cosTb, MUL)
                    E.tt("dve", *g("t2"), bi_, pBb, sn, sinTb, MUL)
                    E.tt("dve", *g("t3"), bi_, pBb, cs, cosTb, MUL)
                    E.tt("dve", *g("t4"), br, pBb, sn, sinTb, MUL)
                    E.tt("pool", *g("wre"), *g("t1"), *g("t2"), ADD)
                    E.tt("pool", *g("wim"), *g("t3"), *g("t4"), SUB)
                    mg = mag[:, d, sc:sc + 1].to_broadcast([128, TS5])
                    for ri, (gn, wn) in enumerate((("gre", "wre"), ("gim", "wim"))):
                        go, gob = T_[gn]
                        wi, wib = T_[wn]
                        if d == 0:
                            E.scan(go[:], gob, mg, magb, wi[:], wib, carry[:, ri, sc:sc + 1], cbufs[sc])
                        else:
                            E.scan(go[:, ::-1], gob, mg, magb, wi[:, ::-1], wib, carry[:, ri, sc:sc + 1], cbufs[sc])
                    E.tt("pool", *g("u1"), cs, cosTb, *g("gre"), MUL)
                    E.tt("pool", *g("u2"), sn, sinTb, *g("gim"), MUL)
                    E.tt("pool", *g("hre"), *g("u1"), *g("u2"), SUB)
                    E.tt("pool", *g("u3"), sn, sinTb, *g("gre"), MUL)
                    E.tt("pool", *g("u4"), cs, cosTb, *g("gim"), MUL)
                    E.tt("pool", *g("him"), *g("u3"), *g("u4"), ADD)
                    lc = TS5 - 1 if d == 0 else 0
                    E.cp("pool", carry[:, 0, sc:sc + 1], cbufs[sc], T_["hre"][0][:, lc:lc + 1], T_["hre"][1])
                    E.cp("pool", carry[:, 1, sc:sc + 1], cbufs[sc], T_["him"][0][:, lc:lc + 1], T_["him"][1])
                    E.cp("act", hb[:, sc, 0, :], hbufs[sc][0], *g("hre"))
                    E.cp("act", hb[:, sc, 1, :], hbufs[sc][1], *g("him"))
                for c in range(2):
                    po, pob = bank()
                    n_ = 0
                    for sc in range(4 * c, 4 * c + 4):
                        for ri in range(2):
                            E.mm(po[:, 0:TS5], pob, CB[:, d, sc, ri, :], CBb, hb[:, sc, ri, :], hbufs[sc][ri], start=n_ == 0, stop=n_ == 7)
                            n_ += 1
                    E.tt("dve", yac[c][0][:, cols], yac[c][1], yac[c][0][:, cols], yac[c][1], po[:, 0:TS5], pob, ADD)
        ta = [k.sb(st, [128, 512], F32, f"sta{c}") for c in range(2)]
        tb_ = [k.sb(st, [128, 512], F32, f"stb{c}") for c in range(2)]
        zf = [k.sb(st, [128, 512], F32, f"szf{c}") for c in range(2)]
        zbh = [k.sb(st, [128, 512], BF16, f"szb{c}") for c in range(2)]
        ots = [k.sb(st, [128, 512], BF16, f"sot{c}") for c in range(2)]
        for (s0, n) in XBLK:
            for c in range(2):
                y, yb_ = yac[c][0][:, s0:s0 + n], yac[c][1]
                a_, ab_ = ta[c][0][:, 0:n], ta[c][1]
                b_, bb_ = tb_[c][0][:, 0:n], tb_[c][1]
                E.act(a_, ab_, y, yb_, AF.Square)
                E.ts("dve", b_, bb_, a_, ab_, 0.044715, 1.0, MUL, ADD)
                E.tt("dve", b_, bb_, b_, bb_, y, yb_, MUL)
                E.act(a_, ab_, b_, bb_, AF.Sigmoid, scale=1.5957691216057308)
                E.tt("pool", zf[c][0][:, 0:n], zf[c][1], y, yb_, a_, ab_, MUL)
                E.cp("act", zbh[c][0][:, 0:n], zbh[c][1], zf[c][0][:, 0:n], zf[c][1])
            for c2 in range(2):
                po, pob = bank()
                for c in range(2):
                    E.mm(po[:, 0:n], pob, gluw[:, c, c2 * 128:(c2 + 1) * 128], gluwb, zbh[c][0][:, 0:n], zbh[c][1], start=c == 0, stop=c == 1)
                a_, ab_ = ta[c2][0][:, 0:n], ta[c2][1]
                E.act(a_, ab_, po[:, 0:n], pob, AF.Sigmoid, bias=glub[:, c2:c2 + 1], xr=[glubb])
                ot, otb = ots[c2]
                E.tt("dve", ot[:, 0:n], otb, zf[c2][0][:, 0:n], zf[c2][1], a_, ab_, MUL)
                yst(512 + c2 * 128, 128, s0, n, ot[:, 0:n], otb)
    k.barrier()
    k.sec('mla')
    mla(nc, k, E, I, l, b, NB, last, Zv, zb, yst, bank, ones_b, ones_bb, inb, castload, fload)


def mla(nc, k, E, I, l, b, NB, last, Zv, zb, yst, bank, ones_b, ones_bb, inb, castload, fload):
    MUL, ADD = ALU.mult, ALU.add
    with ExitStack() as st:
        qag, qagb = fload(st, [128, 2], "mqag", I["qagT"][l])
        kvag, kvagb = fload(st, [128, 1], "mkvag", I["kvagT"][l])
        qg, qgb = fload(st, [96, 1], "mqg", I["qgT"][l])
        kg, kgb = fload(st, [96, 1], "mkg", I["kgT"][l])
        kgpe, kgpeb = fload(st, [32, 1], "mkgpe", I["kgpeT"][l])
        wqb, wqbb = k.sb(st, [128, 2, 384], BF16, "mwqb")
        E.dma("pool", wqb[:, 0, :], wqbb, I["wq_b"][l, 0:128, :], inb, multi=True)
        E.dma("pool", wqb[0:64, 1, :], wqbb, I["wq_b"][l, 128:192, :], inb, multi=True)
        wkvb, wkvbb = castload(st, [128, 512], "mwkvb", I["wkv_b"][l])
        prot, protb = k.sb(st, [96, 96], BF16, "mprot")
        E.ld(prot[:], protb, I["prot"][:, :])
        cqn, cqnb = k.sb(st, [128, 2, S], BF16, "mcqn")
        ckvn, ckvnb = k.sb(st, [128, S], BF16, "mckvn")
        kpef, kpefb = k.sb(st, [32, S], F32, "mkpef")
        kpe2, kpe2b = k.sb(st, [32, S], BF16, "mkpe2")
        z0s = [k.sb(st, [128, 512], F32, f"mz{i}") for i in range(3)]
        sqs = [k.sb(st, [128, 512], BF16, f"msq{i}") for i in range(2)]
        rs, rsb = k.sb(st, [128, 512], F32, "mrs")

        def rstd_from(pm, pmb, m, n, scale):
            E.act(rs[0:m, 0:n], rsb, pm[0:m, 0:n], pmb, AF.Sqrt, bias=EPS, scale=scale)
            E.recip(rs[0:m, 0:n], rsb, rs[0:m, 0:n], rsb)

        E.ld(kpef[:], kpefb, Zv[OFF_KPE:OFF_KPE + 32, :], zb)
        E.act(kpe2[:], kpe2b, kpef[:], kpefb, AF.Square)
        for (s0, n) in XBLK:
            z0, z0b = z0s[0]
            z1, z1b = z0s[1]
            z2, z2b = z0s[2]
            E.ld(z0[:, 0:n], z0b, Zv[OFF_Q:OFF_Q + 128, s0:s0 + n], zb)
            E.ld(z1[0:64, 0:n], z1b, Zv[OFF_Q + 128:OFF_Q + 192, s0:s0 + n], zb)
            E.ld(z2[:, 0:n], z2b, Zv[OFF_KV:OFF_KV + 128, s0:s0 + n], zb)
            E.act(sqs[0][0][:, 0:n], sqs[0][1], z0[:, 0:n], z0b, AF.Square)
            E.act(sqs[1][0][0:64, 0:n], sqs[1][1], z1[0:64, 0:n], z1b, AF.Square)
            pm, pmb = bank()
            E.mm(pm[:, 0:n], pmb, ones_b[:, :], ones_bb, sqs[0][0][:, 0:n], sqs[0][1], start=True, stop=False)
            E.mm(pm[:, 0:n], pmb, ones_b[0:64, :], ones_bb, sqs[1][0][0:64, 0:n], sqs[1][1], start=False, stop=True)
            rstd_from(pm, pmb, 128, n, 1.0 / 192)
            E.stt(cqn[:, 0, s0:s0 + n], cqnb, z0[:, 0:n], z0b, qag[:, 0:1], rs[:, 0:n], rsb, MUL, MUL, xr=[qagb])
            E.stt(cqn[0:64, 1, s0:s0 + n], cqnb, z1[0:64, 0:n], z1b, qag[0:64, 1:2], rs[0:64, 0:n], rsb, MUL, MUL, xr=[qagb])
            E.act(sqs[0][0][:, 0:n], sqs[0][1], z2[:, 0:n], z2b, AF.Square)
            pm, pmb = bank()
            E.mm(pm[:, 0:n], pmb, ones_b[:, :], ones_bb, sqs[0][0][:, 0:n], sqs[0][1])
            rstd_from(pm, pmb, 128, n, 1.0 / 128)
            E.stt(ckvn[:, s0:s0 + n], ckvnb, z2[:, 0:n], z2b, kvag[:, 0:1], rs[:, 0:n], rsb, MUL, MUL, xr=[kvagb])
        Kt, Ktb = k.sb(st, [96, S], BF16, "mKt")
        Qt, Qtb = k.sb(st, [96, S], BF16, "mQt")
        Vh, Vhb = k.sb(st, [128, S // 128, 128], BF16, "mVh")
        E.memset("pool", Vh[:, :, 64:128], Vhb, 1.0)
        knf, knfb = k.sb(st, [96, 512], F32, "mknf")
        knb, knbb = k.sb(st, [96, 512], BF16, "mknb")
        cosb, cosbb = k.sb(st, [96, 512], F32, "mcos")
        sinb, sinbb = k.sb(st, [96, 512], F32, "msin")
        r1, r1b = k.sb(st, [96, 512], F32, "mr1")
        r2, r2b = k.sb(st, [96, 512], F32, "mr2")
        pts = [k.sb(st, [128, 512], BF16, f"mpt{i}") for i in range(3)]
        rd, rdb = k.sb(st, [64, 512], F32, "mrd")
        ots = [k.sb(st, [64, 512], BF16, f"mot{i}") for i in range(2)]
        acc0 = psum0 = None
        SC = 96 ** -0.5

        def norm_rope(pq, pqb, gain_parts, s0, n, dst, dstb):
            isx = s0 >= LC
            for (o0, m, src, srcb, gap, gb) in gain_parts:
                E.stt(knf[o0:o0 + m, 0:n], knfb, src, srcb, gap, rs[0:m, 0:n], rsb, MUL, MUL, xr=[gb])
            if not isx:
                E.cp("act", dst[:, s0:s0 + n], dstb, knf[:, 0:n], knfb)
                return
            xs = s0 - LC
            E.cp("act", knb[:, 0:n], knbb, knf[:, 0:n], knfb)
            pk, pkb = bank()
            E.mm(pk[0:96, 0:n], pkb, prot[:, :], protb, knb[:, 0:n], knbb)
            E.ld(cosb[:, 0:n], cosbb, I["ropecos"][:, xs:xs + n])
            E.ld(sinb[:, 0:n], sinbb, I["ropesin"][:, xs:xs + n])
            E.tt("pool", r1[:, 0:n], r1b, knf[:, 0:n], knfb, cosb[:, 0:n], cosbb, MUL)
            E.tt("dve", r2[:, 0:n], r2b, pk[0:96, 0:n], pkb, sinb[:, 0:n], sinbb, MUL)
            E.tt("dve", dst[:, s0:s0 + n], dstb, r1[:, 0:n], r1b, r2[:, 0:n], r2b, ADD)

        for h in range(4):
            for (s0, n) in XBLK:
                pn, pnb = bank()
                E.mm(pn[0:64, 0:n], pnb, wkvb[:, h * 128:h * 128 + 64], wkvbb, ckvn[:, s0:s0 + n], ckvnb)
                E.act(sqs[0][0][0:64, 0:n], sqs[0][1], pn[0:64, 0:n], pnb, AF.Square)
                pm, pmb = bank()
                E.mm(pm[0:96, 0:n], pmb, ones_b[0:64, 0:96], ones_bb, sqs[0][0][0:64, 0:n], sqs[0][1], start=True, stop=False)
                E.mm(pm[0:96, 0:n], pmb, ones_b[0:32, 0:96], ones_bb, kpe2[0:32, s0:s0 + n], kpe2b, start=False, stop=True)
                rstd_from(pm, pmb, 96, n, 1.0 / 96)
                norm_rope(None, None, [(0, 64, pn[0:64, 0:n], pnb, kg[0:64, 0:1], kgb),
                                       (64, 32, kpef[0:32, s0:s0 + n], kpefb, kgpe[0:32, 0:1], kgpeb)], s0, n, Kt, Ktb)
                if last and s0 < LC:
                    continue
                pq, pqb = bank()
                E.mm(pq[0:96, 0:n], pqb, wqb[:, 0, h * 96:(h + 1) * 96], wqbb, cqn[:, 0, s0:s0 + n], cqnb, start=True, stop=False)
                E.mm(pq[0:96, 0:n], pqb, wqb[0:64, 1, h * 96:(h + 1) * 96], wqbb, cqn[0:64, 1, s0:s0 + n], cqnb, start=False, stop=True)
                E.act(sqs[1][0][0:96, 0:n], sqs[1][1], pq[0:96, 0:n], pqb, AF.Square)
                pm, pmb = bank()
                E.mm(pm[0:96, 0:n], pmb, ones_b[0:96, 0:96], ones_bb, sqs[1][0][0:96, 0:n], sqs[1][1])
                rstd_from(pm, pmb, 96, n, 1.0 / 96)
                norm_rope(None, None, [(0, 96, pq[0:96, 0:n], pqb, qg[0:96, 0:1], qgb)], s0, n, Qt, Qtb)
            for kc in range(S // 128):
                pv, pvb = bank()
                E.mm(pv[:, 0:64], pvb, ckvn[:, kc * 128:(kc + 1) * 128], ckvnb, wkvb[:, h * 128 + 64:h * 128 + 128], wkvbb)
                E.cp("act" if kc % 2 else "dve", Vh[:, kc, 0:64], Vhb, pv[:, 0:64], pvb)

            def attend(q0, nq, kcs, n_):
                po, pob = bank.acc()
                for i, kc in enumerate(kcs):
                    psc, pscb = bank()
                    E.mm(psc[:, 0:nq], pscb, Kt[:, kc * 128:(kc + 1) * 128], Ktb, Qt[:, q0:q0 + nq], Qtb)
                    pt, ptb = pts[i % 3]
                    E.act(pt[:, 0:nq], ptb, psc[:, 0:nq], pscb, AF.Exp, scale=SC)
                    E.mm(po[:, 0:nq], pob, Vh[:, kc, :], Vhb, pt[:, 0:nq], ptb, start=i == 0, stop=i == len(kcs) - 1)
                E.recip(rd[:, 0:nq], rdb, po[64:128, 0:nq], pob)
                ot, otb = ots[n_ % 2]
                E.tt("dve", ot[:, 0:nq], otb, po[0:64, 0:nq], pob, rd[:, 0:nq], rdb, MUL)
                yst(768 + h * 64, 64, q0, nq, ot[:, 0:nq], otb)

            for j in range(4):
                attend(LC + 512 * j, 512, list(range(S // 128)), j)
            if not last:
                attend(0, LC, [0, 1], 0)


def moe(nc, k, E, I, l, NB, last, R, Rb, out, outb, modrows, modrows_b, H2, H2b, Xg, Xgb, Yg, Ygb, bank,
        ident_f, ident_fb, ident_b, ident_bb, ones_b, ones_bb, inb):
    NB1 = NB + 1
    MUL, ADD = ALU.mult, ALU.add
    tiles = [(b, ti) for b in range(NB) for ti in range(2 if last else 0, S // 128)]
    NT = len(tiles)
    NTOK = NT * 128
    NBLK = -(-NTOK * 4 // MB) + NE
    with ExitStack() as top:
        Wall, Wallb = k.sb(top, [128, NT, 4], F32, "Wall")
        IDX, IDXb = k.sb(top, [128, NT, 4], F32, "IDXall")
        Dall, Dallb = k.sb(top, [128, NT, NE], F32, "Dall")
        dest, destb = k.sb(top, [128, NT, 4], I32, "dest")
        base, baseb = k.sb(top, [128, NE], F32, "base")
        iota, iotab = k.sb(top, [128, NE], F32, "iota")
        bexi, bexib = k.sb(top, [128, NBLK], I32, "bexi")
        pstart, pstartb = k.sb(top, [128, NE], F32, "pstart")
        widx, widxb = k.sb(top, [128, NBLK, 8], I32, "widx")
        bidx, bidxb = k.sb(top, [128, NBLK], I32, "bidx")
        oidx, oidxb = k.sb(top, [128, NBLK], I32, "oidx")
        pofs, pofsb = k.sb(top, [128, 9], F32, "pofs")
        E.ld(pofs[:], pofsb, I["pofs"][l])
        cst, cstb = k.sb(top, [128, 4], F32, "cst")
        for ci, cv in enumerate((7.0, 1024.0, 128.0, -7.0)):
            E.memset("pool", cst[:, ci:ci + 1], cstb, cv)
        idxt = [k.sb(top, [128, 1], I32, f"idxt{i}") for i in range(12)]
        ni = [0]

        def stage_idx(src_ap, srcb, eng="dve"):
            t, tb = idxt[ni[0] % 12]
            ni[0] += 1
            E.cp(eng, t[:], tb, src_ap, srcb)
            return t, tb
        E.ld(iota[:], iotab, I["iota32"][:, :])
        E.memset("pool", base[:], baseb, 0.0)
        k.sec('moeA')
        with ExitStack() as st:
            n2g, n2gb = k.sb(st, [128, D], F32, "n2g")
            E.ld(n2g[:], n2gb, I["n2g"][l, 0:1, :].partition_broadcast(128))
            A2, S2 = [], []
            for bi in range(NB1):
                a_, ab_ = k.sb(st, [128, D], F32, f"A2_{bi}")
                s_, sb_ = k.sb(st, [128, D], F32, f"S2_{bi}")
                E.ld(a_[:], ab_, modrows[l, bi:bi + 1, 4 * D:5 * D].partition_broadcast(128), modrows_b)
                E.ld(s_[:], sb_, modrows[l, bi:bi + 1, 3 * D:4 * D].partition_broadcast(128), modrows_b)
                E.stt(a_[:], ab_, a_[:], ab_, 1.0, n2g[:], n2gb, ADD, MUL)
                A2.append((a_, ab_))
                S2.append((s_, sb_))
            rw, rwb = k.sb(st, [128, 8, NE], F32, "rw")
            E.ld(rw[:], rwb, I["router_w"][l].rearrange("(k p) e -> p k e", p=128))
            rbt, rbtb = k.sb(st, [128, NE], F32, "rb")
            E.ld(rbt[:], rbtb, I["router_b"][l, 0:1, :].partition_broadcast(128))
            ustr, ustrb = k.sb(st, [128, 128], BF16, "ustr")
            E.ld(ustr[:], ustrb, I["ustrict"][:, :])
            xts = [k.sb(st, [128, D], F32, f"mxt{i}") for i in range(2)]
            h2s = [k.sb(st, [128, D], F32, f"mh2{i}") for i in range(2)]
            h2bs = [k.sb(st, [128, D], BF16, f"mh2b{i}") for i in range(2)]
            junk, junkb = k.sb(st, [128, D], BF16, "mjunk")
            h2T, h2Tb = k.sb(st, [128, 8, 128], F32, "mh2T")
            st8 = [k.sb(st, [128, 4], F32, f"mst{i}") for i in range(2)]
            lg, lgb = k.sb(st, [128, NE], F32, "mlg")
            v8, v8b = k.sb(st, [128, 8], F32, "mv8")
            i8, i8b = k.sb(st, [128, 8], U32, "mi8")
            ex, exb = k.sb(st, [128, 8], F32, "mex")
            msk, mskb = k.sb(st, [128, NE], F32, "mmsk")
            mskh, mskhb = k.sb(st, [128, NE], BF16, "mmskh")
            for i, (b, ti) in enumerate(tiles):
                bi = b if ti >= 2 else NB
                xt, xtb = xts[i % 2]
                h2, h2b_ = h2s[i % 2]
                hb_, hbb_ = h2bs[i % 2]
                s8, s8b = st8[i % 2]
                E.ld(xt[:], xtb, R[b, ti * 128:(ti + 1) * 128, :], Rb[b][ti])
                E.act(junk[:], junkb, xt[:], xtb, AF.Square, accum=s8[:, 0:1], xw=[s8b])
                E.act(s8[:, 1:2], s8b, s8[:, 0:1], s8b, AF.Sqrt, bias=EPS, scale=1.0 / D)
                E.recip(s8[:, 2:3], s8b, s8[:, 1:2], s8b)
                E.stt(h2[:], h2b_, xt[:], xtb, s8[:, 2:3], A2[bi][0][:], A2[bi][1], MUL, MUL, xr=[s8b])
                E.tt("pool", h2[:], h2b_, h2[:], h2b_, S2[bi][0][:], S2[bi][1], ADD)
                E.cp("act", hb_[:], hbb_, h2[:], h2b_)
                E.dma("sp", H2[i * 128:(i + 1) * 128, :], H2b[i], hb_[:], hbb_, own=hbb_)
                for hf in range(2):
                    p_, pb_ = bank()
                    for q_ in range(4):
                        kk = hf * 4 + q_
                        E.tr(p_[:, q_ * 128:(q_ + 1) * 128], pb_, h2[:, kk * 128:(kk + 1) * 128], h2b_, ident_f[:], ident_fb)
                    E.cp("act" if hf else "dve", h2T[:, hf * 4:hf * 4 + 4, :], h2Tb, p_[:, :].rearrange("p (a b) -> p a b", a=4), pb_)
                pl, plb = bank()
                for kk in range(8):
                    E.mm(pl[:, 0:NE], plb, h2T[:, kk, :], h2Tb, rw[:, kk, :], rwb, start=kk == 0, stop=kk == 7)
                E.tt("dve", lg[:], lgb, pl[:, 0:NE], plb, rbt[:], rbtb, ADD)
                k.op("dve", lambda e: e.max(out=v8[:], in_=lg[:]), r=[lgb], w=[v8b])
                k.op("dve", lambda e: e.max_index(out=i8[:], in_max=v8[:], in_values=lg[:]), r=[lgb, v8b], w=[i8b])
                E.cp("dve", IDX[:, i, :], IDXb, i8[:, 0:4], i8b)
                E.ts("dve", ex[:, 4:5], exb, v8[:, 0:1], v8b, -1.0, None, MUL)
                E.act(ex[:, 0:4], exb, v8[:, 0:4], v8b, AF.Exp, bias=ex[:, 4:5], accum=ex[:, 5:6], xr=[exb])
                E.recip(ex[:, 6:7], exb, ex[:, 5:6], exb)
                E.ts("dve", Wall[:, i, :], Wallb, ex[:, 0:4], exb, ex[:, 6:7], None, MUL)
                E.ts("dve", msk[:], mskb, iota[:], iotab, IDX[:, i, 0:1], None, ALU.is_equal, xr=[IDXb])
                for k4 in range(1, 4):
                    E.stt(msk[:], mskb, iota[:], iotab, IDX[:, i, k4:k4 + 1], msk[:], mskb, ALU.is_equal, ADD, xr=[IDXb])
                E.cp("dve", mskh[:], mskhb, msk[:], mskb)
                pp, ppb = bank()
                E.mm(pp[:, 0:NE], ppb, ustr[:], ustrb, mskh[:], mskhb)
                E.tt("dve", Dall[:, i, :], Dallb, pp[:, 0:NE], ppb, base[:], baseb, ADD)
                pc, pcb = bank()
                E.mm(pc[:, 0:NE], pcb, ones_b[:], ones_bb, mskh[:], mskhb)
                E.tt("dve", base[:], baseb, base[:], baseb, pc[:, 0:NE], pcb, ADD)
        k.barrier()
        k.sec(None)
        with ExitStack() as st:
            nbk, nbkb = k.sb(st, [128, NE], F32, "nbk")
            pend, pendb = k.sb(st, [128, NE], F32, "pend")
            one32, one32b = k.sb(st, [128, NE], F32, "one32")
            blki, blkib = k.sb(st, [128, NBLK], F32, "blki")
            bexf, bexfb = k.sb(st, [128, NBLK], F32, "bexf")
            E.ld(blki[:], blkib, I["blkiota"][:, 0:NBLK])
            E.memset("pool", one32[:], one32b, 1.0)
            E.memset("pool", nbk[:], nbkb, 0.0)
            E.memset("pool", bexf[:], bexfb, 0.0)
            for j in range(-(-NTOK // MB)):
                E.stt(nbk[:], nbkb, base[:], baseb, float(MB * j), nbk[:], nbkb, ALU.is_gt, ADD)
            E.ts("dve", nbk[:], nbkb, nbk[:], nbkb, float(MB), None, MUL)
            E.scan(pend[:], pendb, one32[:], one32b, nbk[:], nbkb, 0.0, None)
            E.tt("dve", pstart[:], pstartb, pend[:], pendb, nbk[:], nbkb, ALU.subtract)
            for e_ in range(NE):
                E.stt(bexf[:], bexfb, blki[:], blkib, pend[:, e_:e_ + 1], bexf[:], bexfb, ALU.is_ge, ADD, xr=[pendb])
            E.ts("dve", bexf[:], bexfb, bexf[:], bexfb, float(NE - 1), None, ALU.min)
            E.cp("dve", bexi[:], bexib, bexf[:], bexfb)
            for kk in range(8):
                E.ts("dve", widx[:, :, kk], widxb, bexf[:], bexfb, cst[:, 1:2], pofs[:, kk:kk + 1], MUL, ADD, xr=[pofsb, cstb])
            E.ts("dve", bidx[:], bidxb, bexf[:], bexfb, cst[:, 2:3], pofs[:, 8:9], MUL, ADD, xr=[pofsb, cstb])
            E.ts("dve", oidx[:], oidxb, bexf[:], bexfb, float(l * NE), None, ADD)
        k.barrier()
        k.sec('moeB')
        with ExitStack() as st:
            Dp, Dpb = k.sb(st, [128, NE], F32, "Dp")
            oh, ohb = k.sb(st, [128, NE], F32, "oh")
            jk, jkb = k.sb(st, [128, NE], F32, "jk")
            dfs = [k.sb(st, [128, 4], F32, f"df{i}") for i in range(2)]
            hts = [k.sb(st, [128, D], BF16, f"ht{i}") for i in range(3)]
            for i in range(NT):
                df, dfb = dfs[i % 2]
                ht, htb = hts[i % 3]
                E.ld(ht[:], htb, H2[i * 128:(i + 1) * 128, :], H2b[i])
                E.tt("dve", Dp[:], Dpb, Dall[:, i, :], Dallb, pstart[:], pstartb, ADD)
                for k4 in range(4):
                    E.ts("dve", oh[:], ohb, iota[:], iotab, IDX[:, i, k4:k4 + 1], None, ALU.is_equal, xr=[IDXb])
                    E.tt("dve", jk[:], jkb, oh[:], ohb, Dp[:], Dpb, MUL)
                    k.op("dve", lambda e, k4=k4, df=df: e.tensor_reduce(out=df[:, k4:k4 + 1], in_=jk[:], axis=mybir.AxisListType.X, op=ADD), r=[jkb], w=[dfb])
                E.cp("dve", dest[:, i, :], destb, df[:], dfb)
                for k4 in range(4):
                    it_, itb_ = stage_idx(dest[:, i, k4:k4 + 1], destb)
                    k.dma("pool", lambda e, it_=it_, ht=ht: e.indirect_dma_start(out=Xg[:, :], out_offset=bass.IndirectOffsetOnAxis(ap=it_[:, 0:1], axis=0), in_=ht[:], in_offset=None),
                          r=[htb, itb_], w=[Xgb], own=htb, multi=True)
        if l == 0:
            dmp = _DUMP[0]
            dmp("dest", dest[:], destb, [128, NT, 4], I32)
            dmp("Wall", Wall[:], Wallb, [128, NT, 4], F32)
            dmp("IDX", IDX[:], IDXb, [128, NT, 4], F32)
            dmp("Dall", Dall[:], Dallb, [128, NT, NE], F32)
            dmp("bexi", bexi[:], bexib, [128, NBLK], I32)
            dmp("widx", widx[:], widxb, [128, NBLK, 8], I32)
            dmp("pstart", pstart[:], pstartb, [128, NE], F32)
            dmp("base", base[:], baseb, [128, NE], F32)
        k.barrier()
        k.sec('moeC')
        with ExitStack() as st:
            wins = [k.sb(st, [128, 8, 2 * D], BF16, f"win{i}") for i in range(2)]
            wouts = [k.sb(st, [128, 8, D], BF16, f"wout{i}") for i in range(2)]
            xrs = [k.sb(st, [128, 4, D], BF16, f"xr{i}") for i in range(2)]
            xTs = [k.sb(st, [128, 8, 512], BF16, f"xT{i}") for i in range(2)]
            aTs = [k.sb(st, [128, 8, 512], BF16, f"aT{i}") for i in range(2)]
            bins = [k.sb(st, [128, 16], F32, f"bin{i}") for i in range(2)]
            bouts = [k.sb(st, [128, D], F32, f"bout{i}") for i in range(2)]
            ysbs = [k.sb(st, [128, D], F32, f"ysb{i}") for i in range(2)]
            tgs = [k.sb(st, [128, 512], F32, f"tg{i}") for i in range(2)]
            tss = [k.sb(st, [128, 512], F32, f"tsg{i}") for i in range(2)]
            tus = [k.sb(st, [128, 512], F32, f"tu{i}") for i in range(2)]
            ny = 0
            WIN = I["exp_w_in"].rearrange("l e k n -> (l e k) n")
            WOUT = I["exp_w_out"].rearrange("l e k n -> (l e k) n")
            BIN = I["exp_b_inT"].rearrange("l e p c -> (l e p) c")
            BOUT = I["exp_b_out"].rearrange("l e o d -> (l e o) d")
            for j in range(NBLK):
                win, winb = wins[j % 2]
                wout, woutb = wouts[j % 2]
                bin_, binb = bins[j % 2]
                bout, boutb = bouts[j % 2]

                for kk in range(8):
                    it_, itb_ = stage_idx(widx[:, j, kk:kk + 1], widxb)
                    k.dma("pool", lambda e, kk=kk, win=win, it_=it_: e.indirect_dma_start(out=win[:, kk, :], out_offset=None, in_=WIN[:, :], in_offset=bass.IndirectOffsetOnAxis(ap=it_[:, 0:1], axis=0)),
                          r=[itb_, inb], w=[winb], multi=True)
                    k.dma("pool", lambda e, kk=kk, wout=wout, it_=it_: e.indirect_dma_start(out=wout[:, kk, :], out_offset=None, in_=WOUT[:, :], in_offset=bass.IndirectOffsetOnAxis(ap=it_[:, 0:1], axis=0)),
                          r=[itb_, inb], w=[woutb], multi=True)
                it_, itb_ = stage_idx(bidx[:, j:j + 1], bidxb)
                k.dma("pool", lambda e, bin_=bin_, it_=it_: e.indirect_dma_start(out=bin_[:], out_offset=None, in_=BIN[:, :], in_offset=bass.IndirectOffsetOnAxis(ap=it_[:, 0:1], axis=0)),
                      r=[itb_, inb], w=[binb])
                it_, itb_ = stage_idx(oidx[:, j:j + 1], oidxb)
                k.dma("pool", lambda e, bout=bout, it_=it_: e.indirect_dma_start(out=bout[:], out_offset=None, in_=BOUT[:, :], in_offset=bass.IndirectOffsetOnAxis(ap=it_[:, 0:1], axis=0)),
                      r=[itb_, inb], w=[boutb])
                for hl in range(MB // 512):
                    r0 = j * MB + hl * 512
                    xr_, xrb = xrs[(j * (MB // 512) + hl) % 2]
                    xT, xTb = xTs[(j * (MB // 512) + hl) % 2]
                    aT, aTb = aTs[(j * (MB // 512) + hl) % 2]
                    E.ld(xr_[:], xrb, Xg[r0:r0 + 512, :].rearrange("(t p) d -> p t d", p=128), Xgb)
                    for kk in range(8):
                        p_, pb_ = bank()
                        pv = p_[:].bitcast(BF16)
                        for tt in range(4):
                            E.tr(pv[:, tt * 128:(tt + 1) * 128], pb_, xr_[:, tt, kk * 128:(kk + 1) * 128], xrb, ident_b[:], ident_bb)
                        E.cp("act" if kk % 2 else "dve", xT[:, kk, :], xTb, pv[:, 0:512], pb_)
                    for fc in range(8):
                        tg, tgb = tgs[fc % 2]
                        tsg, tsgb = tss[fc % 2]
                        tu, tub = tus[fc % 2]
                        pg, pgb = bank()
                        for kk in range(8):
                            E.mm(pg[:, :], pgb, win[:, kk, fc * 128:(fc + 1) * 128], winb, xT[:, kk, :], xTb, start=kk == 0, stop=kk == 7)
                        pu, pub = bank()
                        for kk in range(8):
                            E.mm(pu[:, :], pub, win[:, kk, D + fc * 128:D + (fc + 1) * 128], winb, xT[:, kk, :], xTb, start=kk == 0, stop=kk == 7)
                        E.ts("dve", tg[:], tgb, pg[:, :], pgb, bin_[:, fc:fc + 1], cst[:, 0:1], ADD, ALU.min, xr=[binb, cstb])
                        E.act(tsg[:], tsgb, tg[:], tgb, AF.Silu, scale=1.702)
                        E.ts("dve", tu[:], tub, pu[:, :], pub, bin_[:, 8 + fc:9 + fc], cst[:, 0:1], ADD, ALU.min, xr=[binb, cstb])
                        E.ts("dve", tu[:], tub, tu[:], tub, -7.0, 1.0, ALU.max, ADD)
                        E.stt(aT[:, fc, :], aTb, tu[:], tub, 1.0 / 1.702, tsg[:], tsgb, MUL, MUL)
                    for tt in range(4):
                        ysb, ysbb = ysbs[ny % 2]
                        ny += 1
                        for hf in range(2):
                            py, pyb = bank()
                            for kk in range(8):
                                E.mm(py[:, :], pyb, aT[:, kk, tt * 128:(tt + 1) * 128], aTb, wout[:, kk, hf * 512:(hf + 1) * 512], woutb, start=kk == 0, stop=kk == 7)
                            E.tt("dve", ysb[:, hf * 512:(hf + 1) * 512], ysbb, py[:, :], pyb, bout[:, hf * 512:(hf + 1) * 512], boutb, ADD)
                        for hf in range(2):
                            E.dma("sp", Yg[hf][r0 + tt * 128:r0 + (tt + 1) * 128, :], Ygb, ysb[:, hf * 512:(hf + 1) * 512], ysbb, own=ysbb, multi=True)
        k.barrier()
        k.sec('moeD')
        with ExitStack() as st:
            G2 = []
            for bi in range(NB1):
                g_, gb_ = k.sb(st, [128, D], F32, f"G2_{bi}")
                E.ld(g_[:], gb_, modrows[l, bi:bi + 1, 5 * D:6 * D].partition_broadcast(128), modrows_b)
                G2.append((g_, gb_))
            xts = [k.sb(st, [128, D], F32, f"dxt{i}") for i in range(2)]
            yks = [[k.sb(st, [128, D], F32, f"dy{i}_{q_}") for q_ in range(4)] for i in range(2)]
            ms = [k.sb(st, [128, D], F32, f"dm{i}") for i in range(2)]
            for i, (b, ti) in enumerate(tiles):
                bi = b if ti >= 2 else NB
                xt, xtb = xts[i % 2]
                m_, mb_ = ms[i % 2]
                E.ld(xt[:], xtb, R[b, ti * 128:(ti + 1) * 128, :], Rb[b][ti])
                for k4 in range(4):
                    y_, yb_ = yks[i % 2][k4]
                    it_, itb_ = stage_idx(dest[:, i, k4:k4 + 1], destb)
                    for hf in range(2):
                        k.dma("pool", lambda e, it_=it_, y_=y_, hf=hf: e.indirect_dma_start(out=y_[:, hf * 512:(hf + 1) * 512], out_offset=None, in_=Yg[hf][:, :], in_offset=bass.IndirectOffsetOnAxis(ap=it_[:, 0:1], axis=0)),
                              r=[Ygb, itb_], w=[yb_], multi=True)
                y_, yb_ = yks[i % 2][0]
                E.ts("dve", m_[:], mb_, y_[:], yb_, Wall[:, i, 0:1], None, MUL, xr=[Wallb])
                for k4 in range(1, 4):
                    y_, yb_ = yks[i % 2][k4]
                    E.stt(m_[:], mb_, y_[:], yb_, Wall[:, i, k4:k4 + 1], m_[:], mb_, MUL, ADD, xr=[Wallb])
                E.tt("pool", m_[:], mb_, m_[:], mb_, G2[bi][0][:], G2[bi][1], MUL)
                E.tt("dve", m_[:], mb_, m_[:], mb_, xt[:], xtb, ADD)
                if last:
                    E.dma("sp", out[b, (ti - 2) * 128:(ti - 1) * 128, :], outb[b][ti - 2], m_[:], mb_, own=mb_)
                else:
                    E.dma("sp", R[b, ti * 128:(ti + 1) * 128, :], Rb[b][ti], m_[:], mb_, own=mb_)


_NC_CACHE = {}


def _core_inputs(inp, shared, b0, NB):
    m = dict(shared)
    m["x"] = np.ascontiguousarray(inp["x"][b0:b0 + NB])
    m["ctx"] = np.ascontiguousarray(inp["ctx"][b0:b0 + NB])
    call = np.concatenate([inp["c"][b0:b0 + NB], inp["c_ctx"][None, :]], axis=0)
    m["cT"] = np.ascontiguousarray(np.transpose(call.reshape(NB + 1, 8, 128), (2, 1, 0)))
    return m


def run(inp, NB, n_cores, dbg=(), depth=2):
    inp = {k_: np.asarray(v) for k_, v in inp.items()}
    shared = _prep_shared(inp)
    nblk0 = -(-NB * S * 4 // MB) + NE
    pofs = np.zeros((2, 128, 9), np.float32)
    for l_ in range(2):
        for kk in range(8):
            pofs[l_, :, kk] = l_ * NE * 1024 + kk * 128 + np.arange(128)
        pofs[l_, :, 8] = l_ * NE * 128 + np.arange(128)
    shared["pofs"] = pofs
    shared["blkiota"] = np.broadcast_to((np.arange(nblk0, dtype=np.float32) * MB), (128, nblk0)).copy()
    in_maps = [_core_inputs(inp, shared, c * NB, NB) for c in range(n_cores)]
    specs = {k_: (v.shape, v.dtype) for k_, v in in_maps[0].items()}
    key = (NB, tuple(dbg), depth)
    if key not in _NC_CACHE:
        _NC_CACHE[key] = build(NB, specs, dbg, depth)
    nc = _NC_CACHE[key]
    res = run_bass_kernel_spmd(nc, in_maps, core_ids=list(range(n_cores)))
    return res


def kernel(**inputs):
    n_cores = 8
    NB = inputs["x"].shape[0] // n_cores
    res = run(inputs, NB, n_cores)
    return np.concatenate([r["out"] for r in res.results], axis=0).astype(np.float32)
```

```python
import math
from contextlib import ExitStack
import numpy as np
import ml_dtypes
import concourse.bass as bass
import concourse.mybir as mybir
from concourse.bass_utils import run_bass_kernel_spmd

F32 = mybir.dt.float32
BF16 = mybir.dt.bfloat16
I32 = mybir.dt.int32
U32 = mybir.dt.uint32
ALU = mybir.AluOpType
AF = mybir.ActivationFunctionType

D = 1024
LC = 256
LX = 2048
S = LC + LX
NIN = 1376
OFF_POOL, OFF_Q, OFF_SSM, OFF_KV, OFF_KPE = 512, 768, 960, 1216, 1344
NE = 32
EPS = 1e-6
TS5 = 256
PADW = 2352
NI = 2320
MB = 1536


SKIP = set()


class Buf:
    __slots__ = ("name", "w", "r", "dsem", "dcnt")

    def __init__(self, name):
        self.name = name
        self.w = {}
        self.r = {}
        self.dsem = None
        self.dcnt = 0


class KB:
    ENG = ("pe", "act", "dve", "pool", "sp")

    def __init__(self, nc, stack):
        self.nc = nc
        self.stack = stack
        self.prog = {e: [] for e in self.ENG}
        self.esem = {e: stack.enter_context(nc.semaphore("s_" + e)) for e in self.ENG}
        self.ecnt = {e: 0 for e in self.ENG}
        self.known = {e: {} for e in self.ENG}
        self.dbufs = []
        self.sempool = []
        self.mute = False
        self.nsem = 0
        self.n = 0

    def sb(self, st, shape, dt, name):
        self.n += 1
        nm = f"{name}_{self.n}"
        return st.enter_context(self.nc.sbuf_tensor(nm, list(shape), dt)), Buf(nm)

    def ps(self, st, shape, dt, name):
        self.n += 1
        nm = f"{name}_{self.n}"
        return st.enter_context(self.nc.psum_tensor(nm, list(shape), dt)), Buf(nm)

    def _need(self, e, ev):
        sem, val, who = ev
        kn = self.known[e]
        if kn.get(id(sem), 0) >= val:
            return
        kn[id(sem)] = val
        self.prog[e].append(("wait", sem, val))

    def _deps(self, e, r, w, skip_who=None):
        for b in r:
            for ev in b.w.values():
                self._need(e, ev)
        for b in w:
            for ev in b.w.values():
                if skip_who is not None and ev[2] == skip_who:
                    continue
                self._need(e, ev)
            for ev in b.r.values():
                self._need(e, ev)

    def _record(self, ev, r, w, multi):
        key = id(ev[0])
        for b in r:
            b.r[key] = ev
        for b in w:
            if multi:
                b.w[key] = ev
            else:
                b.w = {key: ev}
            b.r = {}

    def sec(self, name):
        self.mute = name is not None and name in SKIP

    def op(self, e, fn, r=(), w=()):
        if self.mute:
            return
        self._deps(e, r, w, skip_who="pe" if e == "pe" else None)
        self.ecnt[e] += 1
        ev = (self.esem[e], self.ecnt[e], e)
        self.prog[e].append(("ins", fn, self.esem[e], 1))
        self._record(ev, r, w, multi=False)

    def dma(self, q, fn, r=(), w=(), own=None, multi=False):
        if self.mute:
            return
        b0 = own if own is not None else w[0]
        if b0.dsem is None:
            if self.sempool:
                b0.dsem, b0.dcnt = self.sempool.pop()
            else:
                self.nsem += 1
                b0.dsem = self.stack.enter_context(self.nc.semaphore(f"dsem{self.nsem}"))
                b0.dcnt = 0
            self.dbufs.append(b0)
        self._deps(q, r, w, skip_who="dma" if multi else None)
        b0.dcnt += 16
        ev = (b0.dsem, b0.dcnt, "dma")
        self.prog[q].append(("ins", fn, b0.dsem, 16))
        self._record(ev, r, w, multi=multi)

    def raw(self, e, fn, r=()):
        self._deps(e, r, [])
        self.prog[e].append(("raw", fn))

    def barrier(self):
        for e in self.ENG:
            for e2 in self.ENG:
                if e2 != e and self.ecnt[e2] > 0:
                    self._need(e, (self.esem[e2], self.ecnt[e2], e2))
            for b in self.dbufs:
                if b.dcnt > 0:
                    self._need(e, (b.dsem, b.dcnt, "dma"))
        for b in self.dbufs:
            self.sempool.append((b.dsem, b.dcnt))
            b.dsem = None
        self.dbufs = []

    def finish(self):
        self.barrier()
        nc = self.nc
        with nc.Block() as block:
            def mk(ename):
                def body(eng):
                    for it in self.prog[ename]:
                        if it[0] == "wait":
                            eng.wait_ge(it[1], it[2])
                        elif it[0] == "raw":
                            it[1](eng)
                        else:
                            it[1](eng).then_inc(it[2], it[3])
                return body
            block.tensor(mk("pe"))
            block.scalar(mk("act"))
            block.vector(mk("dve"))
            block.gpsimd(mk("pool"))
            block.sync(mk("sp"))


class Emit:
    def __init__(self, k):
        self.k = k
        self.q = 0

    def mm(self, out, ob, lhsT, lb, rhs, rb, start=True, stop=True):
        self.k.op("pe", lambda e: e.matmul(out=out, lhsT=lhsT, rhs=rhs, start=start, stop=stop), r=[lb, rb], w=[ob])

    def tr(self, out, ob, in_, ib, ident, idb):
        self.k.op("pe", lambda e: e.transpose(out=out, in_=in_, identity=ident), r=[ib, idb], w=[ob])

    def act(self, out, ob, in_, ib, func, bias=None, scale=None, accum=None, xr=(), xw=()):
        kw = {}
        if bias is not None:
            kw["bias"] = bias
        if scale is not None:
            kw["scale"] = scale
        if accum is not None:
            kw["accum_out"] = accum
        self.k.op("act", lambda e: e.activation(out=out, in_=in_, func=func, **kw), r=[ib, *xr], w=[ob, *xw])

    def tt(self, eng, out, ob, a, ab, b, bb, op):
        self.k.op(eng, lambda e: e.tensor_tensor(out=out, in0=a, in1=b, op=op), r=[ab, bb], w=[ob])

    def ts(self, eng, out, ob, a, ab, s1, s2, op0, op1=None, xr=(), accum=None, xw=()):
        kw = {}
        if op1 is not None:
            kw["op1"] = op1
        if accum is not None:
            kw["accum_out"] = accum
        self.k.op(eng, lambda e: e.tensor_scalar(out=out, in0=a, scalar1=s1, scalar2=s2, op0=op0, **kw), r=[ab, *xr], w=[ob, *xw])

    def stt(self, out, ob, a, ab, scalar, b, bb, op0, op1, xr=()):
        self.k.op("dve", lambda e: e.scalar_tensor_tensor(out=out, in0=a, scalar=scalar, in1=b, op0=op0, op1=op1), r=[ab, bb, *xr], w=[ob])

    def cp(self, eng, out, ob, in_, ib):
        if eng == "act":
            self.k.op("act", lambda e: e.copy(out=out, in_=in_), r=[ib], w=[ob])
        else:
            self.k.op(eng, lambda e: e.tensor_copy(out=out, in_=in_), r=[ib], w=[ob])

    def memset(self, eng, ap, ob, val):
        self.k.op(eng, lambda e: e.memset(ap, val), w=[ob])

    def recip(self, out, ob, in_, ib):
        self.k.op("dve", lambda e: e.reciprocal(out=out, in_=in_), r=[ib], w=[ob])

    def scan(self, out, ob, d0, d0b, d1, d1b, init, initb):
        r = [d0b, d1b] + ([initb] if initb is not None else [])
        self.k.op("dve", lambda e: e.tensor_tensor_scan(out=out, data0=d0, data1=d1, initial=init, op0=ALU.mult, op1=ALU.add), r=r, w=[ob])

    def dma(self, q, out, ob, in_, ib, own=None, multi=False, **kw):
        r = [ib] if isinstance(ib, Buf) else list(ib)
        self.k.dma(q, lambda e: e.dma_start(out=out, in_=in_, **kw), r=r, w=[ob], own=own, multi=multi)

    def ld(self, out, ob, in_, ib=()):
        self.q ^= 1
        r = [ib] if isinstance(ib, Buf) else list(ib)
        self.k.dma("sp" if self.q else "act", lambda e: e.dma_start(out=out, in_=in_), r=r, w=[ob])


def _pk(v, nchunk):
    sh = v.shape[:-1]
    return np.ascontiguousarray(np.swapaxes(v.reshape(*sh, nchunk, 128), -1, -2))


def _consts():
    c = {}
    c["ident_f"] = np.eye(128, dtype=np.float32)
    c["ident_b"] = np.eye(128).astype(ml_dtypes.bfloat16)
    c["ustrict"] = np.triu(np.ones((128, 128), np.float32), 1).astype(ml_dtypes.bfloat16)
    c["iota32"] = np.broadcast_to(np.arange(NE, dtype=np.float32), (128, NE)).copy()
    t = np.arange(LX)
    row = (t // 64).astype(np.float32)
    col = (t % 64).astype(np.float32)
    inv = (10000.0 ** (-np.arange(0, 16, 2, dtype=np.float32) / 16)).astype(np.float32)
    ang_r = (row[:, None] * inv).astype(np.float32)
    ang_c = (col[:, None] * inv).astype(np.float32)
    cosT = np.ones((96, LX), np.float32)
    sinT = np.zeros((96, LX), np.float32)
    cosT[64:72] = np.cos(ang_r).T
    cosT[72:80] = np.cos(ang_r).T
    sinT[64:72] = np.sin(ang_r).T
    sinT[72:80] = np.sin(ang_r).T
    cosT[80:88] = np.cos(ang_c).T
    cosT[88:96] = np.cos(ang_c).T
    sinT[80:88] = np.sin(ang_c).T
    sinT[88:96] = np.sin(ang_c).T
    c["ropecos"] = cosT
    c["ropesin"] = sinT
    pm = np.zeros((96, 96), np.float32)
    for base in (64, 80):
        for j in range(8):
            pm[base + j, base + 8 + j] = -1.0
            pm[base + 8 + j, base + j] = 1.0
    c["prot"] = np.ascontiguousarray(pm.T).astype(ml_dtypes.bfloat16)
    wins = (2, 4, 8, 16)
    invw = np.zeros((128, 2), np.float32)
    corr = np.ones((128, 2, 4, 8), np.float32)
    for g, win in enumerate(wins):
        cch, half = g // 2, g % 2
        ps = slice(half * 64, half * 64 + 64)
        invw[ps, cch] = 1.0 / win
        for ri, L in ((0, LC), (2, LX)):
            for j in range(8):
                tt = j
                lo, hi = max(tt - win // 2, 0), min(tt + win - 1 - win // 2, L - 1)
                corr[ps, cch, ri, j] = win / float(hi - lo + 1)
                tt = L - 8 + j
                lo, hi = max(tt - win // 2, 0), min(tt + win - 1 - win // 2, L - 1)
                corr[ps, cch, ri + 1, j] = win / float(hi - lo + 1)
    c["pool_invw"] = invw
    c["pool_corr"] = corr
    tau = np.zeros((128, 2, TS5), np.float32)
    tau[:, 0, :] = np.arange(1, TS5 + 1, dtype=np.float32)
    tau[:, 1, :] = np.arange(TS5, 0, -1).astype(np.float32)
    c["tau"] = tau
    return c


def _prep_shared(inp):
    L = 2
    o = {}
    o["ada_w"] = inp["ada_w"]
    o["ada_bT"] = _pk(inp["ada_b"], 48)
    o["ada_brow"] = inp["ada_b"].reshape(L, 1, 6 * D)
    o["n1gT"] = _pk(inp["norm1_g"], 8)
    o["n2g"] = inp["norm2_g"].reshape(L, 1, D)
    o["w_mix_in"] = inp["w_mix_in"]
    o["w_mix_out"] = inp["w_mix_out"]
    o["conv_dwT"] = np.ascontiguousarray(np.transpose(inp["conv_dw"].reshape(L, 31, 2, 128), (0, 3, 2, 1)))
    o["conv_dwbT"] = _pk(inp["conv_dw_b"], 2)
    o["conv_lngT"] = _pk(inp["conv_ln_g"], 2)
    o["conv_lnbT"] = _pk(inp["conv_ln_b"], 2)
    o["conv_pw"] = inp["conv_pw"]
    pw = inp["pool_w"]
    pbd = np.zeros((L, 2, 128, 128), np.float32)
    for g in range(4):
        cch, half = g // 2, g % 2
        pbd[:, cch, half * 64:half * 64 + 64, half * 64:half * 64 + 64] = pw[:, g]
    o["pool_wbd"] = pbd
    o["pool_scT"] = _pk(inp["pool_scale"], 2)

    def st_layout(a):
        return np.ascontiguousarray(np.transpose(a.reshape(L, 2, 8, 2, 64), (0, 1, 3, 4, 2)).reshape(L, 2, 128, 8))
    o["s5_are"] = st_layout(inp["ssm_a_re"])
    o["s5_aim"] = st_layout(inp["ssm_a_im"])
    o["s5_ldt"] = st_layout(np.broadcast_to(inp["ssm_log_dt"][..., None], (L, 2, 16, 64)))
    bbd_re = np.zeros((L, 2, 8, 128, 32), np.float32)
    bbd_im = np.zeros((L, 2, 8, 128, 32), np.float32)
    cbd_re = np.zeros((L, 2, 8, 128, 128), np.float32)
    cbd_im = np.zeros((L, 2, 8, 128, 128), np.float32)
    for g in range(16):
        sc, gi = g // 2, g % 2
        bbd_re[:, :, sc, gi * 64:gi * 64 + 64, gi * 16:gi * 16 + 16] = inp["ssm_b_re"][:, :, g]
        bbd_im[:, :, sc, gi * 64:gi * 64 + 64, gi * 16:gi * 16 + 16] = inp["ssm_b_im"][:, :, g]
        c0 = (sc % 4) * 32 + gi * 16
        cbd_re[:, :, sc, gi * 64:gi * 64 + 64, c0:c0 + 16] = np.swapaxes(inp["ssm_c_re"][:, :, g], -1, -2)
        cbd_im[:, :, sc, gi * 64:gi * 64 + 64, c0:c0 + 16] = np.swapaxes(inp["ssm_c_im"][:, :, g], -1, -2)
    o["s5_bre"], o["s5_bim"], o["s5_cre"], o["s5_cim"] = bbd_re, bbd_im, cbd_re, cbd_im
    o["s5_dT"] = _pk(inp["ssm_d"], 2)
    o["s5_gluw"] = inp["ssm_glu_w"]
    o["s5_glubT"] = _pk(inp["ssm_glu_b"], 2)
    qag = np.zeros((L, 256), np.float32)
    qag[:, :192] = inp["mla_q_a_g"]
    o["qagT"] = _pk(qag, 2)
    o["wq_b"] = inp["mla_wq_b"]
    o["kvagT"] = inp["mla_kv_a_g"].reshape(L, 128, 1)
    o["wkv_b"] = inp["mla_wkv_b"]
    o["qgT"] = inp["mla_q_g"].reshape(L, 96, 1)
    o["kgT"] = inp["mla_k_g"].reshape(L, 96, 1)
    o["kgpeT"] = np.ascontiguousarray(inp["mla_k_g"][:, 64:96]).reshape(L, 32, 1)
    o["router_w"] = inp["router_w"]
    o["router_b"] = inp["router_b"].reshape(L, 1, NE)
    o["exp_w_in"] = inp["exp_w_in"]
    o["exp_b_inT"] = _pk(inp["exp_b_in"], 16)
    o["exp_w_out"] = inp["exp_w_out"]
    o["exp_b_out"] = inp["exp_b_out"].reshape(L, NE, 1, D)
    o.update(_consts())
    return {k_: np.ascontiguousarray(v) for k_, v in o.items()}


XBLK = [(0, 256)] + [(256 + 512 * j, 512) for j in range(4)]


def pcol(s0):
    return 16 + s0 if s0 < LC else 32 + s0


_DUMP = [None]


def build(NB, specs, dbg=(), depth=2):
    NB1 = NB + 1
    nc = bass.Bass("TRN2", target_bir_lowering=False)
    I = {}
    for name, (shape, dt) in specs.items():
        bdt = {np.dtype(np.float32): F32, np.dtype(ml_dtypes.bfloat16): BF16, np.dtype(np.int32): I32}[np.dtype(dt)]
        I[name] = nc.dram_tensor(name, list(shape), bdt, kind="ExternalInput").ap()

    def scr(name, shape, dt=F32):
        kind = "ExternalOutput" if name in dbg else "Internal"
        return nc.dram_tensor(name, list(shape), dt, kind=kind).ap()

    out = nc.dram_tensor("out", [NB, LX, D], F32, kind="ExternalOutput").ap()
    R = scr("R", [NB, S, D])
    modrows = scr("modrows", [2, NB1, 6 * D])
    Zin = scr("Zin", [NB, NIN, S])
    Ycat = scr("Ycat", [NB, D, S], BF16)
    NTOK0 = NB * S
    NBLK0 = -(-NTOK0 * 4 // MB) + NE
    H2 = scr("H2", [NTOK0, D], BF16)
    Xg = scr("Xg", [NBLK0 * MB, D], BF16)
    Yg = [scr(f"Yg{h_}", [NBLK0 * MB, D // 2], F32) for h_ in range(2)]
    Rb = [[Buf(f"R{b}_{t}") for t in range(S // 128)] for b in range(NB)]
    outb = [[Buf(f"o{b}_{t}") for t in range(LX // 128)] for b in range(NB)]
    modrows_b = Buf("modrows")
    Zb = [Buf(f"Z{b}") for b in range(NB)]
    Yb = [Buf(f"Yc{b}") for b in range(NB)]
    H2b = [Buf(f"H2_{t}") for t in range(NTOK0 // 128)]
    Xgb = Buf("Xg")
    Ygb = Buf("Yg")
    inb = Buf("inputs")

    with ExitStack() as top:
        k = KB(nc, top)
        E = Emit(k)

        def dump(name, ap, buf, shape, dt):
            if ("dbg_" + name) not in dbg:
                return
            t_ = nc.dram_tensor("dbg_" + name, list(shape), dt, kind="ExternalOutput").ap()
            E.dma("sp", t_, Buf("dbg_" + name), ap, buf, own=buf)
        _DUMP[0] = dump
        ident_b, ident_bb = k.sb(top, [128, 128], BF16, "identb")
        ident_f, ident_fb = k.sb(top, [128, 128], F32, "identf")
        ones_b, ones_bb = k.sb(top, [128, 128], BF16, "onesb")
        E.ld(ident_b[:], ident_bb, I["ident_b"][:, :])
        E.ld(ident_f[:], ident_fb, I["ident_f"][:, :])
        E.memset("pool", ones_b[:], ones_bb, 1.0)
        modT = [k.sb(top, [128, 48, NB1], F32, f"modT{l}") for l in range(2)]
        psum = [k.ps(top, [128, 512], F32, f"bank{i}") for i in range(8)]
        pi = [0]

        def bank():
            pi[0] = pi[0] % 7 + 1
            return psum[pi[0]]
        bank.acc = lambda: psum[0]

        with ExitStack() as st:
            cT, cTb = k.sb(st, [128, 8, NB1], F32, "cT")
            sT, sTb = k.sb(st, [128, 8, NB1], F32, "sT")
            E.ld(cT[:], cTb, I["cT"][:, :, :])
            E.act(sT[:], sTb, cT[:], cTb, AF.Silu)
            wbl = [k.sb(st, [128, 8, 512], F32, f"adaw{i}") for i in range(2)]
            brw = [k.sb(st, [NB1, 512], F32, f"brow{i}") for i in range(2)]
            rwt = [k.sb(st, [NB1, 512], F32, f"rowt{i}") for i in range(2)]
            abT, abTb = k.sb(st, [128, 2, 48], F32, "abT")
            E.ld(abT[:], abTb, I["ada_bT"].rearrange("l p c -> p l c"))
            it = 0
            for l in range(2):
                wv = I["ada_w"][l].rearrange("(k p) n -> p k n", p=128)
                for cb in range(12):
                    w_, wb_ = wbl[it % 2]
                    br_, brb_ = brw[it % 2]
                    rt_, rtb_ = rwt[it % 2]
                    it += 1
                    E.ld(w_[:], wb_, wv[:, :, cb * 512:(cb + 1) * 512])
                    E.ld(br_[:], brb_, I["ada_brow"][l, 0:1, cb * 512:(cb + 1) * 512].partition_broadcast(NB1))
                    for j in range(4):
                        ch = cb * 4 + j
                        p_, pb_ = bank()
                        for kk in range(8):
                            E.mm(p_[:, 0:NB1], pb_, w_[:, kk, j * 128:(j + 1) * 128], wb_, sT[:, kk, :], sTb, start=kk == 0, stop=kk == 7)
                        E.ts("dve", modT[l][0][:, ch, :], modT[l][1], p_[:, 0:NB1], pb_, abT[:, l, ch:ch + 1], None, ALU.add, xr=[abTb])
                    p_, pb_ = bank()
                    for kk in range(8):
                        E.mm(p_[0:NB1, :], pb_, sT[:, kk, :], sTb, w_[:, kk, :], wb_, start=kk == 0, stop=kk == 7)
                    E.tt("dve", rt_[:], rtb_, p_[0:NB1, :], pb_, br_[:], brb_, ALU.add)
                    E.dma("sp", modrows[l, :, cb * 512:(cb + 1) * 512], modrows_b, rt_[:], rtb_, own=rtb_, multi=True)
        k.barrier()

        for l in range(depth):
            last = l == 1
            mT, mTb = modT[l]
            for b in range(NB):
                def src_tile(ti):
                    if l == 0:
                        return (I["ctx"][b, ti * 128:(ti + 1) * 128, :] if ti < 2 else I["x"][b, (ti - 2) * 128:(ti - 1) * 128, :]), inb
                    return R[b, ti * 128:(ti + 1) * 128, :], Rb[b][ti]
                with ExitStack() as st:
                    wmi, wmib = k.sb(st, [128, 8, NIN], BF16, "wmi")
                    wv = I["w_mix_in"][l].rearrange("(k p) n -> p k n", p=128)
                    for h_ in range(4):
                        E.dma("pool", wmi[:, 2 * h_:2 * h_ + 2, :], wmib, wv[:, 2 * h_:2 * h_ + 2, :], inb, multi=True)
                    n1g, n1gb = k.sb(st, [128, 8], F32, "n1g")
                    E.ld(n1g[:], n1gb, I["n1gT"][l])
                    A1, A1b = k.sb(st, [128, 8, 2], F32, "A1")
                    for j, bi in enumerate((b, NB)):
                        k.op("dve", lambda e, j=j, bi=bi, A1=A1, mT=mT, n1g=n1g: e.scalar_tensor_tensor(out=A1[:, :, j], in0=mT[:, 8:16, bi], scalar=1.0, in1=n1g[:, :], op0=ALU.add, op1=ALU.mult), r=[mTb, n1gb], w=[A1b])
                    xts = [k.sb(st, [128, D], F32, f"xt{i}") for i in range(3)]
                    xns = [k.sb(st, [128, D], BF16, f"xn{i}") for i in range(2)]
                    junk, junkb = k.sb(st, [128, D], BF16, "junk")
                    st8 = [k.sb(st, [128, 4], F32, f"st{i}") for i in range(2)]
                    hTs = [k.sb(st, [128, 8, 512], BF16, f"hT{i}") for i in range(2)]
                    zts = [k.sb(st, [128, 512], F32, f"zt{i}") for i in range(3)]
                    nt = 0
                    nz = 0
                    for bi_, (s0, ntok) in enumerate(XBLK):
                        hT, hTb = hTs[bi_ % 2]
                        for tt in range(ntok // 128):
                            ti = (s0 + tt * 128) // 128
                            j = 0 if ti >= 2 else 1
                            bi = b if ti >= 2 else NB
                            xt, xtb = xts[nt % 3]
                            xn, xnb = xns[nt % 2]
                            s8, s8b = st8[nt % 2]
                            nt += 1
                            sap, sbuf_ = src_tile(ti)
                            E.ld(xt[:], xtb, sap, sbuf_)
                            E.act(junk[:], junkb, xt[:], xtb, AF.Square, accum=s8[:, 0:1], xw=[s8b])
                            E.act(s8[:, 1:2], s8b, s8[:, 0:1], s8b, AF.Sqrt, bias=EPS, scale=1.0 / D)
                            E.recip(s8[:, 2:3], s8b, s8[:, 1:2], s8b)
                            E.ts("dve", xn[:], xnb, xt[:], xtb, s8[:, 2:3], None, ALU.mult, xr=[s8b])
                            p_, pb_ = bank()
                            pv = p_[:].bitcast(BF16)
                            for kk in range(8):
                                E.tr(pv[:, kk * 128:(kk + 1) * 128], pb_, xn[:, kk * 128:(kk + 1) * 128], xnb, ident_b[:], ident_bb)
                            for kk in range(8):
                                o_ = hT[:, kk, tt * 128:(tt + 1) * 128]
                                i_ = pv[:, kk * 128:(kk + 1) * 128]
                                if kk % 2 == 0:
                                    E.act(o_, hTb, i_, pb_, AF.Identity, bias=mT[:, kk, bi:bi + 1], scale=A1[:, kk, j:j + 1], xr=[mTb, A1b])
                                else:
                                    E.ts("dve", o_, hTb, i_, pb_, A1[:, kk, j:j + 1], mT[:, kk, bi:bi + 1], ALU.mult, ALU.add, xr=[mTb, A1b])
                        for c0 in range(0, NIN, 128):
                            m = min(128, NIN - c0)
                            p_, pb_ = bank()
                            for kk in range(8):
                                E.mm(p_[0:m, 0:ntok], pb_, wmi[:, kk, c0:c0 + m], wmib, hT[:, kk, 0:ntok], hTb, start=kk == 0, stop=kk == 7)
                            zt, ztb = zts[nz % 3]
                            E.cp("act" if nz % 2 == 0 else "dve", zt[0:m, 0:ntok], ztb, p_[0:m, 0:ntok], pb_)
                            nz += 1
                            E.dma("sp", Zin[b, c0:c0 + m, s0:s0 + ntok], Zb[b], zt[0:m, 0:ntok], ztb, own=ztb, multi=True)
                k.barrier()
                mixers(nc, k, E, I, l, b, NB, last, Zin, Zb, Ycat, Yb, mT, mTb, bank, ident_b, ident_bb, ident_f, ident_fb, ones_b, ones_bb, inb)
                k.sec(None)
                k.barrier()
                with ExitStack() as st:
                    wmo, wmob = k.sb(st, [128, 8, D], BF16, "wmo")
                    wv = I["w_mix_out"][l].rearrange("(k p) n -> p k n", p=128)
                    for h_ in range(4):
                        E.dma("pool", wmo[:, 2 * h_:2 * h_ + 2, :], wmob, wv[:, 2 * h_:2 * h_ + 2, :], inb, multi=True)
                    g1t = [k.sb(st, [128, D], F32, f"g1_{i}") for i in range(2)]
                    E.ld(g1t[0][0][:], g1t[0][1], modrows[l, b:b + 1, 2 * D:3 * D].partition_broadcast(128), modrows_b)
                    E.ld(g1t[1][0][:], g1t[1][1], modrows[l, NB:NB1, 2 * D:3 * D].partition_broadcast(128), modrows_b)
                    ycs = [k.sb(st, [128, 8, 128], BF16, f"yc{i}") for i in range(2)]
                    xts = [k.sb(st, [128, D], F32, f"xr{i}") for i in range(2)]
                    xos = [k.sb(st, [128, D], F32, f"xo{i}") for i in range(2)]
                    yv = Ycat[b].rearrange("(k p) t -> p k t", p=128)
                    for n_, ti in enumerate(range(2 if last else 0, S // 128)):
                        yc, ycb = ycs[n_ % 2]
                        xt, xtb = xts[n_ % 2]
                        xo, xob = xos[n_ % 2]
                        g1, g1b = g1t[0] if ti >= 2 else g1t[1]
                        E.ld(yc[:], ycb, yv[:, :, ti * 128:(ti + 1) * 128], Yb[b])
                        sap, sbuf_ = src_tile(ti)
                        E.ld(xt[:], xtb, sap, sbuf_)
                        for hf in range(2):
                            p_, pb_ = bank()
                            for kk in range(8):
                                E.mm(p_[:, :], pb_, yc[:, kk, :], ycb, wmo[:, kk, hf * 512:(hf + 1) * 512], wmob, start=kk == 0, stop=kk == 7)
                            E.tt("dve", xo[:, hf * 512:(hf + 1) * 512], xob, p_[:, :], pb_, g1[:, hf * 512:(hf + 1) * 512], g1b, ALU.mult)
                        E.tt("pool", xo[:], xob, xo[:], xob, xt[:], xtb, ALU.add)
                        E.dma("sp", R[b, ti * 128:(ti + 1) * 128, :], Rb[b][ti], xo[:], xob, own=xob)
                k.barrier()
            moe(nc, k, E, I, l, NB, last, R, Rb, out, outb, modrows, modrows_b, H2, H2b, Xg, Xgb, Yg, Ygb, bank,
                ident_f, ident_fb, ident_b, ident_bb, ones_b, ones_bb, inb)
            k.sec(None)
            k.barrier()
        k.finish()
    return nc


IBLK = [(0, 256, 0)] + [(272 + 512 * j, 512, 256 + 512 * j) for j in range(4)]
TWO_PI = 2.0 * math.pi


def mixers(nc, k, E, I, l, b, NB, last, Zin, Zb, Ycat, Yb, mT, mTb, bank, ident_b, ident_bb, ident_f, ident_fb,
           ones_b, ones_bb, inb):
    Zv = Zin[b]
    Yv = Ycat[b]
    zb = Zb[b]
    yb = Yb[b]

    def yst(row0, nrow, s0, n, src, srcb):
        E.dma("sp", Yv[row0:row0 + nrow, s0:s0 + n], yb, src, srcb, own=srcb, multi=True)

    def castload(st, shape, name, src):
        t, tb = k.sb(st, shape, BF16, name)
        E.dma("pool", t[:], tb, src, inb)
        return t, tb

    def fload(st, shape, name, src):
        t, tb = k.sb(st, shape, F32, name)
        E.ld(t[:], tb, src)
        return t, tb

    k.sec('conv')
    with ExitStack() as st:
        U = [k.sb(st, [128, PADW], F32, f"cU{c}") for c in range(2)]
        acc = [k.sb(st, [128, NI], F32, f"cacc{c}") for c in range(2)]
        gt, gtb = k.sb(st, [128, NI], F32, "cgt")
        dw, dwb_ = fload(st, [128, 2, 31], "cdw", I["conv_dwT"][l])
        dwbias, dwbiasb = fload(st, [128, 2], "cdwb", I["conv_dwbT"][l])
        lng, lngb = fload(st, [128, 2], "clng", I["conv_lngT"][l])
        lnb, lnbb = fload(st, [128, 2], "clnb", I["conv_lnbT"][l])
        pw, pwb = castload(st, [128, 2, 256], "cpw", I["conv_pw"][l].rearrange("(c p) n -> p c n", p=128))
        onesf, onesfb = k.sb(st, [128, 128], F32, "onesf")
        E.memset("pool", onesf[:], onesfb, 1.0 / 256.0)
        for c in range(2):
            u, ub = U[c]
            a, ab = acc[c]
            for (c0, c1) in ((0, 16), (272, 288), (2336, PADW)):
                E.memset("pool", u[:, c0:c1], ub, 0.0)
            E.memset("pool", gt[:, 256:272], gtb, 0.0)
            k.dma("sp", lambda e, u=u, c=c: e.dma_start(out=u[:, 16:272], in_=Zv[c * 128:(c + 1) * 128, 0:LC]), r=[zb], w=[ub], multi=True)
            k.dma("sp", lambda e, u=u, c=c: e.dma_start(out=u[:, 288:2336], in_=Zv[c * 128:(c + 1) * 128, LC:S]), r=[zb], w=[ub], multi=True)
            k.dma("sp", lambda e, c=c: e.dma_start(out=gt[:, 0:256], in_=Zv[256 + c * 128:256 + (c + 1) * 128, 0:LC]), r=[zb], w=[gtb], multi=True)
            k.dma("sp", lambda e, c=c: e.dma_start(out=gt[:, 272:NI], in_=Zv[256 + c * 128:256 + (c + 1) * 128, LC:S]), r=[zb], w=[gtb], multi=True)
            E.act(gt[:], gtb, gt[:], gtb, AF.Sigmoid)
            E.tt("dve", u[:, 16:2336], ub, u[:, 16:2336], ub, gt[:], gtb, ALU.mult)
            E.ts("dve", a[:], ab, u[:, 1:1 + NI], ub, dw[:, c, 0:1], dwbias[:, c:c + 1], ALU.mult, ALU.add, xr=[dwb_, dwbiasb])
            for kk in range(1, 31):
                E.stt(a[:], ab, u[:, 1 + kk:1 + kk + NI], ub, dw[:, c, kk:kk + 1], a[:], ab, ALU.mult, ALU.add, xr=[dwb_])
        sq = [k.sb(st, [128, 512], F32, f"csq{c}") for c in range(2)]
        vs = [k.sb(st, [128, 512], BF16, f"cvs{c}") for c in range(2)]
        tm, tmb = k.sb(st, [128, 512], F32, "ctm")
        tv, tvb = k.sb(st, [128, 512], F32, "ctv")
        t1s = [k.sb(st, [128, 512], F32, f"ct1{c}") for c in range(2)]
        ots = [k.sb(st, [128, 512], BF16, f"cot{c}") for c in range(2)]
        for (i0, n, s0) in IBLK:
            pm, pmb = bank()
            pe2, pe2b = bank()
            for c in range(2):
                E.act(sq[c][0][:, 0:n], sq[c][1], acc[c][0][:, i0:i0 + n], acc[c][1], AF.Square)
            for c in range(2):
                E.mm(pm[:, 0:n], pmb, onesf[:], onesfb, acc[c][0][:, i0:i0 + n], acc[c][1], start=c == 0, stop=c == 1)
            for c in range(2):
                E.mm(pe2[:, 0:n], pe2b, onesf[:], onesfb, sq[c][0][:, 0:n], sq[c][1], start=c == 0, stop=c == 1)
            E.act(tm[:, 0:n], tmb, pm[:, 0:n], pmb, AF.Square)
            E.tt("dve", tv[:, 0:n], tvb, pe2[:, 0:n], pe2b, tm[:, 0:n], tmb, ALU.subtract)
            E.act(tv[:, 0:n], tvb, tv[:, 0:n], tvb, AF.Sqrt, bias=EPS)
            E.recip(tv[:, 0:n], tvb, tv[:, 0:n], tvb)
            for c in range(2):
                t1, t1b = t1s[c]
                E.tt("dve", t1[:, 0:n], t1b, acc[c][0][:, i0:i0 + n], acc[c][1], pm[:, 0:n], pmb, ALU.subtract)
                E.tt("pool", t1[:, 0:n], t1b, t1[:, 0:n], t1b, tv[:, 0:n], tvb, ALU.mult)
                E.act(vs[c][0][:, 0:n], vs[c][1], t1[:, 0:n], t1b, AF.Silu, bias=lnb[:, c:c + 1], scale=lng[:, c:c + 1], xr=[lnbb, lngb])
            for c2 in range(2):
                po, pob = bank()
                for c in range(2):
                    E.mm(po[:, 0:n], pob, pw[:, c, c2 * 128:(c2 + 1) * 128], pwb, vs[c][0][:, 0:n], vs[c][1], start=c == 0, stop=c == 1)
                ot, otb = ots[c2]
                E.cp("act", ot[:, 0:n], otb, po[:, 0:n], pob)
                yst(c2 * 128, 128, s0, n, ot[:, 0:n], otb)
    k.barrier()

    k.sec('pool')
    with ExitStack() as st:
        u, ub = k.sb(st, [128, PADW], F32, "pU")
        A, Ab = k.sb(st, [128, PADW], F32, "pA")
        Bt, Btb = k.sb(st, [128, PADW], F32, "pB")
        PL, PLb_ = k.sb(st, [128, NI], F32, "pPL")
        PLh, PLhb = k.sb(st, [128, NI], BF16, "pPLh")
        invw, invwb = fload(st, [128, 2], "pinvw", I["pool_invw"][:, :])
        corr, corrb = fload(st, [128, 2, 4, 8], "pcorr", I["pool_corr"][:, :, :, :])
        psc, pscb = fload(st, [128, 2], "ppsc", I["pool_scT"][l])
        pwbd, pwbdb = castload(st, [128, 2, 128], "ppw", I["pool_wbd"][l].rearrange("c p n -> p c n"))
        ots = [k.sb(st, [128, 512], BF16, f"pot{c}") for c in range(2)]
        for (c0, c1) in ((0, 16), (272, 288), (2336, PADW)):
            E.memset("pool", u[:, c0:c1], ub, 0.0)
        for c in range(2):
            r0 = OFF_POOL + c * 128
            k.dma("sp", lambda e, r0=r0: e.dma_start(out=u[:, 16:272], in_=Zv[r0:r0 + 128, 0:LC]), r=[zb], w=[ub], multi=True)
            k.dma("sp", lambda e, r0=r0: e.dma_start(out=u[:, 288:2336], in_=Zv[r0:r0 + 128, LC:S]), r=[zb], w=[ub], multi=True)
            W_ = PADW
            E.tt("dve", A[:, 1:W_], Ab, u[:, 0:W_ - 1], ub, u[:, 1:W_], ub, ALU.add)
            E.tt("pool", Bt[:, 2:W_ - 1], Btb, A[:, 1:W_ - 2], Ab, A[:, 3:W_], Ab, ALU.add)
            if c == 1:
                E.tt("dve", A[:, 4:W_ - 3], Ab, Bt[:, 2:W_ - 5], Btb, Bt[:, 6:W_ - 1], Btb, ALU.add)
                E.tt("pool", Bt[:, 8:W_ - 7], Btb, A[:, 4:W_ - 11], Ab, A[:, 12:W_ - 3], Ab, ALU.add)
            E.ts("dve", PL[0:64, :], PLb_, A[0:64, 16:2336], Ab, invw[0:64, c:c + 1], None, ALU.mult, xr=[invwb])
            E.ts("dve", PL[64:128, :], PLb_, Bt[64:128, 16:2336], Btb, invw[64:128, c:c + 1], None, ALU.mult, xr=[invwb])
            for r_, i0 in enumerate((0, 248, 272, 2312)):
                E.tt("dve", PL[:, i0:i0 + 8], PLb_, PL[:, i0:i0 + 8], PLb_, corr[:, c, r_, :], corrb, ALU.mult)
            E.tt("dve", PLh[:], PLhb, PL[:], PLb_, u[:, 16:2336], ub, ALU.subtract)
            for n_, (i0, n, s0) in enumerate(IBLK):
                po, pob = bank()
                E.mm(po[:, 0:n], pob, pwbd[:, c, :], pwbdb, PLh[:, i0:i0 + n], PLhb)
                ot, otb = ots[n_ % 2]
                E.ts("dve", ot[:, 0:n], otb, po[:, 0:n], pob, psc[:, c:c + 1], None, ALU.mult, xr=[pscb])
                yst(256 + c * 128, 128, s0, n, ot[:, 0:n], otb)
    k.barrier()

    k.sec('s5')
    with ExitStack() as st:
        are, areb = fload(st, [128, 2, 8], "sare", I["s5_are"][l].rearrange("d p s -> p d s"))
        aim, aimb = fload(st, [128, 2, 8], "saim", I["s5_aim"][l].rearrange("d p s -> p d s"))
        dt_, dtb = fload(st, [128, 2, 8], "sldt", I["s5_ldt"][l].rearrange("d p s -> p d s"))
        bre, breb = fload(st, [128, 2, 8, 32], "sbre", I["s5_bre"][l].rearrange("d s p c -> p d s c"))
        bim, bimb = fload(st, [128, 2, 8, 32], "sbim", I["s5_bim"][l].rearrange("d s p c -> p d s c"))
        tau, taub = fload(st, [128, 2, TS5], "stau", I["tau"][:, :, :])
        dsk, dskb = fload(st, [128, 2], "sd", I["s5_dT"][l])
        glub, glubb = fload(st, [128, 2], "sglub", I["s5_glubT"][l])
        gluw, gluwb = castload(st, [128, 2, 256], "sgluw", I["s5_gluw"][l].rearrange("(c p) n -> p c n", p=128))
        CB, CBb = k.sb(st, [128, 2, 8, 2, 128], BF16, "sCB")
        for d in range(2):
            E.dma("pool", CB[:, d, :, 0, :], CBb, I["s5_cre"][l, d].rearrange("s p n -> p s n"), inb, multi=True)
            E.dma("pool", CB[:, d, :, 1, :], CBb, I["s5_cim"][l, d].rearrange("s p n -> p s n"), inb, multi=True)
        for d in range(2):
            k.op("act", lambda e, d=d: e.mul(out=CB[:, d, :, 1, :], in_=CB[:, d, :, 1, :], constant=-1.0) if False else e.activation(out=CB[:, d, :, 1, :], in_=CB[:, d, :, 1, :], func=AF.Copy, scale=-1.0), r=[CBb], w=[CBb])
        LB, LBb = k.sb(st, [128, 2, 8, 2, 128], BF16, "sLB")
        E.memset("pool", LB[:], LBb, 0.0)
        mag, magb = k.sb(st, [128, 2, 8], F32, "smag")
        th, thb = k.sb(st, [128, 2, 8], F32, "sth")
        sc_ = {n_: k.sb(st, [128, 2, 8], F32, "s" + n_) for n_ in ("cs", "sn", "abr", "abi", "den", "fre", "fim", "nfim", "q1", "q2")}
        rt1, rt1b = k.sb(st, [128, TS5], F32, "rt1")
        rti, rtib = k.sb(st, [128, TS5], I32, "rti")

        def rr(x, xb, n, both=True):
            E.ts("dve", rt1[:, 0:n], rt1b, x, xb, 1.0 / TWO_PI, None, ALU.mult)
            E.cp("dve", rti[:, 0:n], rtib, rt1[:, 0:n], rt1b)
            E.cp("dve", rt1[:, 0:n], rt1b, rti[:, 0:n], rtib)
            E.stt(x, xb, rt1[:, 0:n], rt1b, -TWO_PI, x, xb, ALU.mult, ALU.add)
            E.ts("dve", rt1[:, 0:n], rt1b, x, xb, math.pi, -TWO_PI, ALU.is_gt, ALU.mult)
            E.tt("dve", x, xb, x, xb, rt1[:, 0:n], rt1b, ALU.add)
            E.ts("dve", rt1[:, 0:n], rt1b, x, xb, -math.pi, TWO_PI, ALU.is_lt, ALU.mult)
            E.tt("dve", x, xb, x, xb, rt1[:, 0:n], rt1b, ALU.add)

        def sincos(ang, angb, n, osin, osinb, ocos, ocosb):
            rr(ang, angb, n)
            E.act(osin, osinb, ang, angb, AF.Sin)
            E.ts("dve", ang, angb, ang, angb, math.pi / 2, None, ALU.add)
            rr(ang, angb, n)
            E.act(ocos, ocosb, ang, angb, AF.Sin)

        f2 = lambda t: t[:].rearrange("p d s -> p (d s)")
        V = {n_: (f2(t), tb) for n_, (t, tb) in sc_.items()}
        aref, aimf, dtf, magf, thf = f2(are), f2(aim), f2(dt_), f2(mag), f2(th)
        E.ts("dve", aref, areb, aref, areb, -1e-4, None, ALU.min)
        E.act(dtf, dtb, dtf, dtb, AF.Exp)
        E.tt("dve", magf, magb, aref, areb, dtf, dtb, ALU.mult)
        E.act(magf, magb, magf, magb, AF.Exp)
        E.tt("dve", thf, thb, aimf, aimb, dtf, dtb, ALU.mult)
        q1, q1b = V["q1"]
        q2, q2b = V["q2"]
        E.cp("dve", q1, q1b, thf, thb)
        sincos(q1, q1b, 16, V["sn"][0], V["sn"][1], V["cs"][0], V["cs"][1])
        E.tt("dve", V["abr"][0], V["abr"][1], magf, magb, V["cs"][0], V["cs"][1], ALU.mult)
        E.tt("dve", V["abi"][0], V["abi"][1], magf, magb, V["sn"][0], V["sn"][1], ALU.mult)
        E.tt("dve", q1, q1b, aref, areb, aref, areb, ALU.mult)
        E.tt("dve", q2, q2b, aimf, aimb, aimf, aimb, ALU.mult)
        E.tt("dve", V["den"][0], V["den"][1], q1, q1b, q2, q2b, ALU.add)
        E.recip(V["den"][0], V["den"][1], V["den"][0], V["den"][1])
        E.ts("dve", V["abr"][0], V["abr"][1], V["abr"][0], V["abr"][1], -1.0, None, ALU.add)
        E.tt("dve", q1, q1b, V["abr"][0], V["abr"][1], aref, areb, ALU.mult)
        E.tt("dve", q2, q2b, V["abi"][0], V["abi"][1], aimf, aimb, ALU.mult)
        E.tt("dve", q1, q1b, q1, q1b, q2, q2b, ALU.add)
        E.tt("dve", V["fre"][0], V["fre"][1], q1, q1b, V["den"][0], V["den"][1], ALU.mult)
        E.tt("dve", q1, q1b, V["abi"][0], V["abi"][1], aref, areb, ALU.mult)
        E.tt("dve", q2, q2b, V["abr"][0], V["abr"][1], aimf, aimb, ALU.mult)
        E.tt("dve", q1, q1b, q1, q1b, q2, q2b, ALU.subtract)
        E.tt("dve", V["fim"][0], V["fim"][1], q1, q1b, V["den"][0], V["den"][1], ALU.mult)
        E.ts("dve", V["nfim"][0], V["nfim"][1], V["fim"][0], V["fim"][1], -1.0, None, ALU.mult)
        fre, freb = sc_["fre"]
        fim, fimb = sc_["fim"]
        nfim, nfimb = sc_["nfim"]
        bbs = [k.sb(st, [128, 32], F32, f"sbb{i}") for i in range(2)]
        cosT, cosTb = k.sb(st, [128, 2, 8, TS5], F32, "scosT")
        sinT, sinTb = k.sb(st, [128, 2, 8, TS5], F32, "ssinT")
        ang, angb = k.sb(st, [128, TS5], F32, "sang")
        for d in range(2):
            for sc in range(8):
                bbr, bbrb = bbs[0]
                bbi, bbib = bbs[1]
                E.ts("dve", bbr[:], bbrb, bre[:, d, sc, :], breb, fre[:, d, sc:sc + 1], None, ALU.mult, xr=[freb])
                E.stt(bbr[:], bbrb, bim[:, d, sc, :], bimb, nfim[:, d, sc:sc + 1], bbr[:], bbrb, ALU.mult, ALU.add, xr=[nfimb])
                E.ts("dve", bbi[:], bbib, bim[:, d, sc, :], bimb, fre[:, d, sc:sc + 1], None, ALU.mult, xr=[freb])
                E.stt(bbi[:], bbib, bre[:, d, sc, :], breb, fim[:, d, sc:sc + 1], bbi[:], bbib, ALU.mult, ALU.add, xr=[fimb])
                r0 = (sc % 4) * 32
                for ri, (bb_, bbb_) in enumerate(((bbr, bbrb), (bbi, bbib))):
                    p_, pb_ = bank()
                    E.tr(p_[0:32, 0:128], pb_, bb_[:], bbb_, ident_f[:], ident_fb)
                    E.cp("act", LB[r0:r0 + 32, d, sc, ri, :], LBb, p_[0:32, 0:128], pb_)
                E.ts("dve", ang[:], angb, tau[:, d, :], taub, th[:, d, sc:sc + 1], None, ALU.mult, xr=[thb])
                sincos(ang[:], angb, TS5, sinT[:, d, sc, :], sinTb, cosT[:, d, sc, :], cosTb)
        Uf = [k.sb(st, [128, S], F32, f"sUf{c}") for c in range(2)]
        Ubf, Ubfb = k.sb(st, [128, 2, S], BF16, "sUb")
        yac = [k.sb(st, [128, S], F32, f"syac{c}") for c in range(2)]
        for c in range(2):
            r0 = OFF_SSM + c * 128
            E.ld(Uf[c][0][:], Uf[c][1], Zv[r0:r0 + 128, :], zb)
            E.cp("act", Ubf[:, c, :], Ubfb, Uf[c][0][:], Uf[c][1])
            E.ts("dve", yac[c][0][:], yac[c][1], Uf[c][0][:], Uf[c][1], dsk[:, c:c + 1], None, ALU.mult, xr=[dskb])
        carry, carryb = k.sb(st, [128, 2, 8], F32, "scarry")
        cbufs = [Buf(f"carry{i}") for i in range(8)]
        names = ("t1", "t2", "t3", "t4", "wre", "wim", "gre", "gim", "u1", "u2", "u3", "u4", "hre", "him")
        sets = [{n_: k.sb(st, [128, TS5], F32, f"s{n_}{i}") for n_ in names} for i in range(2)]
        hb, hbb = k.sb(st, [128, 8, 2, TS5], BF16, "shb")
        hbufs = [[Buf(f"hb{sc}_{ri}") for ri in range(2)] for sc in range(8)]
        MUL, ADD, SUB = ALU.mult, ALU.add, ALU.subtract
        for d in range(2):
            k.op("pool", lambda e: e.memset(carry[:], 0.0), w=cbufs)
            nctx = LC // TS5
            order = list(range(S // TS5)) if d == 0 else list(range(nctx - 1, -1, -1)) + list(range(S // TS5 - 1, nctx - 1, -1))
            for ch in order:
                cols = slice(ch * TS5, (ch + 1) * TS5)
                for sc in range(8):
                    c = sc // 4
                    T_ = sets[sc % 2]
                    pB, pBb = bank()
                    E.mm(pB[:, 0:TS5], pBb, LB[:, d, sc, 0, :], LBb, Ubf[:, c, cols], Ubfb)
                    E.mm(pB[:, TS5:2 * TS5], pBb, LB[:, d, sc, 1, :], LBb, Ubf[:, c, cols], Ubfb)
                    br, bi_ = pB[:, 0:TS5], pB[:, TS5:2 * TS5]
                    cs, sn = cosT[:, d, sc, :], sinT[:, d, sc, :]
                    g = lambda n_: (T_[n_][0][:], T_[n_][1])
                    E.tt("dve", *g("t1"), br, pBb, cs, cosTb, MUL)
                    E.tt("dve", *g("t2"), bi_, pBb, sn, sinTb, MUL)
                    E.tt("dve", *g("t3"), bi_, pBb, cs, cosTb, MUL)
                    E.tt("dve", *g("t4"), br, pBb, sn, sinTb, MUL)
                    E.tt("pool", *g("wre"), *g("t1"), *g("t2"), ADD)
                    E.tt("pool", *g("wim"), *g("t3"), *g("t4"), SUB)
                    mg = mag[:, d, sc:sc + 1].to_broadcast([128, TS5])
                    for ri, (gn, wn) in enumerate((("gre", "wre"), ("gim", "wim"))):
                        go, gob = T_[gn]
                        wi, wib = T_[wn]
                        if d == 0:
                            E.scan(go[:], gob, mg, magb, wi[:], wib, carry[:, ri, sc:sc + 1], cbufs[sc])
                        else:
                            E.scan(go[:, ::-1], gob, mg, magb, wi[:, ::-1], wib, carry[:, ri, sc:sc + 1], cbufs[sc])
                    E.tt("pool", *g("u1"), cs, cosTb, *g("gre"), MUL)
                    E.tt("pool", *g("u2"), sn, sinTb, *g("gim"), MUL)
                    E.tt("pool", *g("hre"), *g("u1"), *g("u2"), SUB)
                    E.tt("pool", *g("u3"), sn, sinTb, *g("gre"), MUL)
                    E.tt("pool", *g("u4"), cs, cosTb, *g("gim"), MUL)
                    E.tt("pool", *g("him"), *g("u3"), *g("u4"), ADD)
                    lc = TS5 - 1 if d == 0 else 0
                    E.cp("act", carry[:, 0, sc:sc + 1], cbufs[sc], T_["hre"][0][:, lc:lc + 1], T_["hre"][1])
                    E.cp("act", carry[:, 1, sc:sc + 1], cbufs[sc], T_["him"][0][:, lc:lc + 1], T_["him"][1])
                    E.cp("act", hb[:, sc, 0, :], hbufs[sc][0], *g("hre"))
                    E.cp("act", hb[:, sc, 1, :], hbufs[sc][1], *g("him"))
                for c in range(2):
                    po, pob = bank()
                    n_ = 0
                    for sc in range(4 * c, 4 * c + 4):
                        for ri in range(2):
                            E.mm(po[:, 0:TS5], pob, CB[:, d, sc, ri, :], CBb, hb[:, sc, ri, :], hbufs[sc][ri], start=n_ == 0, stop=n_ == 7)
                            n_ += 1
                    E.tt("dve", yac[c][0][:, cols], yac[c][1], yac[c][0][:, cols], yac[c][1], po[:, 0:TS5], pob, ADD)
        ta = [k.sb(st, [128, 512], F32, f"sta{c}") for c in range(2)]
        tb_ = [k.sb(st, [128, 512], F32, f"stb{c}") for c in range(2)]
        zf = [k.sb(st, [128, 512], F32, f"szf{c}") for c in range(2)]
        zbh = [k.sb(st, [128, 512], BF16, f"szb{c}") for c in range(2)]
        ots = [k.sb(st, [128, 512], BF16, f"sot{c}") for c in range(2)]
        for (s0, n) in XBLK:
            for c in range(2):
                y, yb_ = yac[c][0][:, s0:s0 + n], yac[c][1]
                a_, ab_ = ta[c][0][:, 0:n], ta[c][1]
                b_, bb_ = tb_[c][0][:, 0:n], tb_[c][1]
                E.act(a_, ab_, y, yb_, AF.Square)
                E.ts("dve", b_, bb_, a_, ab_, 0.044715, 1.0, MUL, ADD)
                E.tt("dve", b_, bb_, b_, bb_, y, yb_, MUL)
                E.act(a_, ab_, b_, bb_, AF.Sigmoid, scale=1.5957691216057308)
                E.tt("pool", zf[c][0][:, 0:n], zf[c][1], y, yb_, a_, ab_, MUL)
                E.cp("act", zbh[c][0][:, 0:n], zbh[c][1], zf[c][0][:, 0:n], zf[c][1])
            for c2 in range(2):
                po, pob = bank()
                for c in range(2):
                    E.mm(po[:, 0:n], pob, gluw[:, c, c2 * 128:(c2 + 1) * 128], gluwb, zbh[c][0][:, 0:n], zbh[c][1], start=c == 0, stop=c == 1)
                a_, ab_ = ta[c2][0][:, 0:n], ta[c2][1]
                E.act(a_, ab_, po[:, 0:n], pob, AF.Sigmoid, bias=glub[:, c2:c2 + 1], xr=[glubb])
                ot, otb = ots[c2]
                E.tt("dve", ot[:, 0:n], otb, zf[c2][0][:, 0:n], zf[c2][1], a_, ab_, MUL)
                yst(512 + c2 * 128, 128, s0, n, ot[:, 0:n], otb)
    k.barrier()
    k.sec('mla')
    mla(nc, k, E, I, l, b, NB, last, Zv, zb, yst, bank, ones_b, ones_bb, inb, castload, fload)


def mla(nc, k, E, I, l, b, NB, last, Zv, zb, yst, bank, ones_b, ones_bb, inb, castload, fload):
    MUL, ADD = ALU.mult, ALU.add
    with ExitStack() as st:
        qag, qagb = fload(st, [128, 2], "mqag", I["qagT"][l])
        kvag, kvagb = fload(st, [128, 1], "mkvag", I["kvagT"][l])
        qg, qgb = fload(st, [96, 1], "mqg", I["qgT"][l])
        kg, kgb = fload(st, [96, 1], "mkg", I["kgT"][l])
        kgpe, kgpeb = fload(st, [32, 1], "mkgpe", I["kgpeT"][l])
        wqb, wqbb = k.sb(st, [128, 2, 384], BF16, "mwqb")
        E.dma("pool", wqb[:, 0, :], wqbb, I["wq_b"][l, 0:128, :], inb, multi=True)
        E.dma("pool", wqb[0:64, 1, :], wqbb, I["wq_b"][l, 128:192, :], inb, multi=True)
        wkvb, wkvbb = castload(st, [128, 512], "mwkvb", I["wkv_b"][l])
        prot, protb = k.sb(st, [96, 96], BF16, "mprot")
        E.ld(prot[:], protb, I["prot"][:, :])
        cqn, cqnb = k.sb(st, [128, 2, S], BF16, "mcqn")
        ckvn, ckvnb = k.sb(st, [128, S], BF16, "mckvn")
        kpef, kpefb = k.sb(st, [32, S], F32, "mkpef")
        kpe2, kpe2b = k.sb(st, [32, S], BF16, "mkpe2")
        z0s = [k.sb(st, [128, 512], F32, f"mz{i}") for i in range(3)]
        sqs = [k.sb(st, [128, 512], BF16, f"msq{i}") for i in range(2)]
        rs, rsb = k.sb(st, [128, 512], F32, "mrs")

        def rstd_from(pm, pmb, m, n, scale):
            E.act(rs[0:m, 0:n], rsb, pm[0:m, 0:n], pmb, AF.Sqrt, bias=EPS, scale=scale)
            E.recip(rs[0:m, 0:n], rsb, rs[0:m, 0:n], rsb)

        E.ld(kpef[:], kpefb, Zv[OFF_KPE:OFF_KPE + 32, :], zb)
        E.act(kpe2[:], kpe2b, kpef[:], kpefb, AF.Square)
        for (s0, n) in XBLK:
            z0, z0b = z0s[0]
            z1, z1b = z0s[1]
            z2, z2b = z0s[2]
            E.ld(z0[:, 0:n], z0b, Zv[OFF_Q:OFF_Q + 128, s0:s0 + n], zb)
            E.ld(z1[0:64, 0:n], z1b, Zv[OFF_Q + 128:OFF_Q + 192, s0:s0 + n], zb)
            E.ld(z2[:, 0:n], z2b, Zv[OFF_KV:OFF_KV + 128, s0:s0 + n], zb)
            E.act(sqs[0][0][:, 0:n], sqs[0][1], z0[:, 0:n], z0b, AF.Square)
            E.act(sqs[1][0][0:64, 0:n], sqs[1][1], z1[0:64, 0:n], z1b, AF.Square)
            pm, pmb = bank()
            E.mm(pm[:, 0:n], pmb, ones_b[:, :], ones_bb, sqs[0][0][:, 0:n], sqs[0][1], start=True, stop=False)
            E.mm(pm[:, 0:n], pmb, ones_b[0:64, :], ones_bb, sqs[1][0][0:64, 0:n], sqs[1][1], start=False, stop=True)
            rstd_from(pm, pmb, 128, n, 1.0 / 192)
            E.stt(cqn[:, 0, s0:s0 + n], cqnb, z0[:, 0:n], z0b, qag[:, 0:1], rs[:, 0:n], rsb, MUL, MUL, xr=[qagb])
            E.stt(cqn[0:64, 1, s0:s0 + n], cqnb, z1[0:64, 0:n], z1b, qag[0:64, 1:2], rs[0:64, 0:n], rsb, MUL, MUL, xr=[qagb])
            E.act(sqs[0][0][:, 0:n], sqs[0][1], z2[:, 0:n], z2b, AF.Square)
            pm, pmb = bank()
            E.mm(pm[:, 0:n], pmb, ones_b[:, :], ones_bb, sqs[0][0][:, 0:n], sqs[0][1])
            rstd_from(pm, pmb, 128, n, 1.0 / 128)
            E.stt(ckvn[:, s0:s0 + n], ckvnb, z2[:, 0:n], z2b, kvag[:, 0:1], rs[:, 0:n], rsb, MUL, MUL, xr=[kvagb])
        Kt, Ktb = k.sb(st, [96, S], BF16, "mKt")
        Qt, Qtb = k.sb(st, [96, S], BF16, "mQt")
        Vh, Vhb = k.sb(st, [128, S // 128, 128], BF16, "mVh")
        E.memset("pool", Vh[:, :, 64:128], Vhb, 1.0)
        knf, knfb = k.sb(st, [96, 512], F32, "mknf")
        knb, knbb = k.sb(st, [96, 512], BF16, "mknb")
        cosb, cosbb = k.sb(st, [96, 512], F32, "mcos")
        sinb, sinbb = k.sb(st, [96, 512], F32, "msin")
        r1, r1b = k.sb(st, [96, 512], F32, "mr1")
        r2, r2b = k.sb(st, [96, 512], F32, "mr2")
        pts = [k.sb(st, [128, 512], BF16, f"mpt{i}") for i in range(3)]
        rd, rdb = k.sb(st, [64, 512], F32, "mrd")
        ots = [k.sb(st, [64, 512], BF16, f"mot{i}") for i in range(2)]
        acc0 = psum0 = None
        SC = 96 ** -0.5

        def norm_rope(pq, pqb, gain_parts, s0, n, dst, dstb):
            isx = s0 >= LC
            for (o0, m, src, srcb, gap, gb) in gain_parts:
                E.stt(knf[o0:o0 + m, 0:n], knfb, src, srcb, gap, rs[0:m, 0:n], rsb, MUL, MUL, xr=[gb])
            if not isx:
                E.cp("act", dst[:, s0:s0 + n], dstb, knf[:, 0:n], knfb)
                return
            xs = s0 - LC
            E.cp("act", knb[:, 0:n], knbb, knf[:, 0:n], knfb)
            pk, pkb = bank()
            E.mm(pk[0:96, 0:n], pkb, prot[:, :], protb, knb[:, 0:n], knbb)
            E.ld(cosb[:, 0:n], cosbb, I["ropecos"][:, xs:xs + n])
            E.ld(sinb[:, 0:n], sinbb, I["ropesin"][:, xs:xs + n])
            E.tt("pool", r1[:, 0:n], r1b, knf[:, 0:n], knfb, cosb[:, 0:n], cosbb, MUL)
            E.tt("dve", r2[:, 0:n], r2b, pk[0:96, 0:n], pkb, sinb[:, 0:n], sinbb, MUL)
            E.tt("dve", dst[:, s0:s0 + n], dstb, r1[:, 0:n], r1b, r2[:, 0:n], r2b, ADD)

        for h in range(4):
            for (s0, n) in XBLK:
                pn, pnb = bank()
                E.mm(pn[0:64, 0:n], pnb, wkvb[:, h * 128:h * 128 + 64], wkvbb, ckvn[:, s0:s0 + n], ckvnb)
                E.act(sqs[0][0][0:64, 0:n], sqs[0][1], pn[0:64, 0:n], pnb, AF.Square)
                pm, pmb = bank()
                E.mm(pm[0:96, 0:n], pmb, ones_b[0:64, 0:96], ones_bb, sqs[0][0][0:64, 0:n], sqs[0][1], start=True, stop=False)
                E.mm(pm[0:96, 0:n], pmb, ones_b[0:32, 0:96], ones_bb, kpe2[0:32, s0:s0 + n], kpe2b, start=False, stop=True)
                rstd_from(pm, pmb, 96, n, 1.0 / 96)
                norm_rope(None, None, [(0, 64, pn[0:64, 0:n], pnb, kg[0:64, 0:1], kgb),
                                       (64, 32, kpef[0:32, s0:s0 + n], kpefb, kgpe[0:32, 0:1], kgpeb)], s0, n, Kt, Ktb)
                if last and s0 < LC:
                    continue
                pq, pqb = bank()
                E.mm(pq[0:96, 0:n], pqb, wqb[:, 0, h * 96:(h + 1) * 96], wqbb, cqn[:, 0, s0:s0 + n], cqnb, start=True, stop=False)
                E.mm(pq[0:96, 0:n], pqb, wqb[0:64, 1, h * 96:(h + 1) * 96], wqbb, cqn[0:64, 1, s0:s0 + n], cqnb, start=False, stop=True)
                E.act(sqs[1][0][0:96, 0:n], sqs[1][1], pq[0:96, 0:n], pqb, AF.Square)
                pm, pmb = bank()
                E.mm(pm[0:96, 0:n], pmb, ones_b[0:96, 0:96], ones_bb, sqs[1][0][0:96, 0:n], sqs[1][1])
                rstd_from(pm, pmb, 96, n, 1.0 / 96)
                norm_rope(None, None, [(0, 96, pq[0:96, 0:n], pqb, qg[0:96, 0:1], qgb)], s0, n, Qt, Qtb)
            for kc in range(S // 128):
                pv, pvb = bank()
                E.mm(pv[:, 0:64], pvb, ckvn[:, kc * 128:(kc + 1) * 128], ckvnb, wkvb[:, h * 128 + 64:h * 128 + 128], wkvbb)
                E.cp("act" if kc % 2 else "dve", Vh[:, kc, 0:64], Vhb, pv[:, 0:64], pvb)

            def attend(q0, nq, kcs, n_):
                po, pob = bank.acc()
                for i, kc in enumerate(kcs):
                    psc, pscb = bank()
                    E.mm(psc[:, 0:nq], pscb, Kt[:, kc * 128:(kc + 1) * 128], Ktb, Qt[:, q0:q0 + nq], Qtb)
                    pt, ptb = pts[i % 3]
                    E.act(pt[:, 0:nq], ptb, psc[:, 0:nq], pscb, AF.Exp, scale=SC)
                    E.mm(po[:, 0:nq], pob, Vh[:, kc, :], Vhb, pt[:, 0:nq], ptb, start=i == 0, stop=i == len(kcs) - 1)
                E.recip(rd[:, 0:nq], rdb, po[64:128, 0:nq], pob)
                ot, otb = ots[n_ % 2]
                E.tt("dve", ot[:, 0:nq], otb, po[0:64, 0:nq], pob, rd[:, 0:nq], rdb, MUL)
                yst(768 + h * 64, 64, q0, nq, ot[:, 0:nq], otb)

            for j in range(4):
                attend(LC + 512 * j, 512, list(range(S // 128)), j)
            if not last:
                attend(0, LC, [0, 1], 0)


def moe(nc, k, E, I, l, NB, last, R, Rb, out, outb, modrows, modrows_b, H2, H2b, Xg, Xgb, Yg, Ygb, bank,
        ident_f, ident_fb, ident_b, ident_bb, ones_b, ones_bb, inb):
    NB1 = NB + 1
    MUL, ADD = ALU.mult, ALU.add
    tiles = [(b, ti) for b in range(NB) for ti in range(2 if last else 0, S // 128)]
    NT = len(tiles)
    NTOK = NT * 128
    NBLK = -(-NTOK * 4 // MB) + NE
    with ExitStack() as top:
        Wall, Wallb = k.sb(top, [128, NT, 4], F32, "Wall")
        IDX, IDXb = k.sb(top, [128, NT, 4], F32, "IDXall")
        Dall, Dallb = k.sb(top, [128, NT, NE], F32, "Dall")
        dest, destb = k.sb(top, [128, NT, 4], I32, "dest")
        base, baseb = k.sb(top, [128, NE], F32, "base")
        iota, iotab = k.sb(top, [128, NE], F32, "iota")
        bexi, bexib = k.sb(top, [128, NBLK], I32, "bexi")
        pstart, pstartb = k.sb(top, [128, NE], F32, "pstart")
        widx, widxb = k.sb(top, [128, NBLK, 8], I32, "widx")
        bidx, bidxb = k.sb(top, [128, NBLK], I32, "bidx")
        oidx, oidxb = k.sb(top, [128, NBLK], I32, "oidx")
        pofs, pofsb = k.sb(top, [128, 9], F32, "pofs")
        E.ld(pofs[:], pofsb, I["pofs"][l])
        cst, cstb = k.sb(top, [128, 4], F32, "cst")
        for ci, cv in enumerate((7.0, 1024.0, 128.0, -7.0)):
            E.memset("pool", cst[:, ci:ci + 1], cstb, cv)
        idxt = [k.sb(top, [128, 1], I32, f"idxt{i}") for i in range(12)]
        ni = [0]

        def stage_idx(src_ap, srcb, eng="dve"):
            t, tb = idxt[ni[0] % 12]
            ni[0] += 1
            E.cp(eng, t[:], tb, src_ap, srcb)
            return t, tb
        E.ld(iota[:], iotab, I["iota32"][:, :])
        E.memset("pool", base[:], baseb, 0.0)
        k.sec('moeA')
        with ExitStack() as st:
            n2g, n2gb = k.sb(st, [128, D], F32, "n2g")
            E.ld(n2g[:], n2gb, I["n2g"][l, 0:1, :].partition_broadcast(128))
            A2, S2 = [], []
            for bi in range(NB1):
                a_, ab_ = k.sb(st, [128, D], F32, f"A2_{bi}")
                s_, sb_ = k.sb(st, [128, D], F32, f"S2_{bi}")
                E.ld(a_[:], ab_, modrows[l, bi:bi + 1, 4 * D:5 * D].partition_broadcast(128), modrows_b)
                E.ld(s_[:], sb_, modrows[l, bi:bi + 1, 3 * D:4 * D].partition_broadcast(128), modrows_b)
                E.stt(a_[:], ab_, a_[:], ab_, 1.0, n2g[:], n2gb, ADD, MUL)
                A2.append((a_, ab_))
                S2.append((s_, sb_))
            rw, rwb = k.sb(st, [128, 8, NE], F32, "rw")
            E.ld(rw[:], rwb, I["router_w"][l].rearrange("(k p) e -> p k e", p=128))
            rbt, rbtb = k.sb(st, [128, NE], F32, "rb")
            E.ld(rbt[:], rbtb, I["router_b"][l, 0:1, :].partition_broadcast(128))
            ustr, ustrb = k.sb(st, [128, 128], BF16, "ustr")
            E.ld(ustr[:], ustrb, I["ustrict"][:, :])
            xts = [k.sb(st, [128, D], F32, f"mxt{i}") for i in range(2)]
            h2s = [k.sb(st, [128, D], F32, f"mh2{i}") for i in range(2)]
            h2bs = [k.sb(st, [128, D], BF16, f"mh2b{i}") for i in range(2)]
            junk, junkb = k.sb(st, [128, D], BF16, "mjunk")
            h2T, h2Tb = k.sb(st, [128, 8, 128], F32, "mh2T")
            st8 = [k.sb(st, [128, 4], F32, f"mst{i}") for i in range(2)]
            lg, lgb = k.sb(st, [128, NE], F32, "mlg")
            v8, v8b = k.sb(st, [128, 8], F32, "mv8")
            i8, i8b = k.sb(st, [128, 8], U32, "mi8")
            ex, exb = k.sb(st, [128, 8], F32, "mex")
            msk, mskb = k.sb(st, [128, NE], F32, "mmsk")
            mskh, mskhb = k.sb(st, [128, NE], BF16, "mmskh")
            for i, (b, ti) in enumerate(tiles):
                bi = b if ti >= 2 else NB
                xt, xtb = xts[i % 2]
                h2, h2b_ = h2s[i % 2]
                hb_, hbb_ = h2bs[i % 2]
                s8, s8b = st8[i % 2]
                E.ld(xt[:], xtb, R[b, ti * 128:(ti + 1) * 128, :], Rb[b][ti])
                E.act(junk[:], junkb, xt[:], xtb, AF.Square, accum=s8[:, 0:1], xw=[s8b])
                E.act(s8[:, 1:2], s8b, s8[:, 0:1], s8b, AF.Sqrt, bias=EPS, scale=1.0 / D)
                E.recip(s8[:, 2:3], s8b, s8[:, 1:2], s8b)
                E.stt(h2[:], h2b_, xt[:], xtb, s8[:, 2:3], A2[bi][0][:], A2[bi][1], MUL, MUL, xr=[s8b])
                E.tt("pool", h2[:], h2b_, h2[:], h2b_, S2[bi][0][:], S2[bi][1], ADD)
                E.cp("act", hb_[:], hbb_, h2[:], h2b_)
                E.dma("sp", H2[i * 128:(i + 1) * 128, :], H2b[i], hb_[:], hbb_, own=hbb_)
                for hf in range(2):
                    p_, pb_ = bank()
                    for q_ in range(4):
                        kk = hf * 4 + q_
                        E.tr(p_[:, q_ * 128:(q_ + 1) * 128], pb_, h2[:, kk * 128:(kk + 1) * 128], h2b_, ident_f[:], ident_fb)
                    E.cp("act" if hf else "dve", h2T[:, hf * 4:hf * 4 + 4, :], h2Tb, p_[:, :].rearrange("p (a b) -> p a b", a=4), pb_)
                pl, plb = bank()
                for kk in range(8):
                    E.mm(pl[:, 0:NE], plb, h2T[:, kk, :], h2Tb, rw[:, kk, :], rwb, start=kk == 0, stop=kk == 7)
                E.tt("dve", lg[:], lgb, pl[:, 0:NE], plb, rbt[:], rbtb, ADD)
                k.op("dve", lambda e: e.max(out=v8[:], in_=lg[:]), r=[lgb], w=[v8b])
                k.op("dve", lambda e: e.max_index(out=i8[:], in_max=v8[:], in_values=lg[:]), r=[lgb, v8b], w=[i8b])
                E.cp("dve", IDX[:, i, :], IDXb, i8[:, 0:4], i8b)
                E.ts("dve", ex[:, 4:5], exb, v8[:, 0:1], v8b, -1.0, None, MUL)
                E.act(ex[:, 0:4], exb, v8[:, 0:4], v8b, AF.Exp, bias=ex[:, 4:5], accum=ex[:, 5:6], xr=[exb])
                E.recip(ex[:, 6:7], exb, ex[:, 5:6], exb)
                E.ts("dve", Wall[:, i, :], Wallb, ex[:, 0:4], exb, ex[:, 6:7], None, MUL)
                E.ts("dve", msk[:], mskb, iota[:], iotab, IDX[:, i, 0:1], None, ALU.is_equal, xr=[IDXb])
                for k4 in range(1, 4):
                    E.stt(msk[:], mskb, iota[:], iotab, IDX[:, i, k4:k4 + 1], msk[:], mskb, ALU.is_equal, ADD, xr=[IDXb])
                E.cp("dve", mskh[:], mskhb, msk[:], mskb)
                pp, ppb = bank()
                E.mm(pp[:, 0:NE], ppb, ustr[:], ustrb, mskh[:], mskhb)
                E.tt("dve", Dall[:, i, :], Dallb, pp[:, 0:NE], ppb, base[:], baseb, ADD)
                pc, pcb = bank()
                E.mm(pc[:, 0:NE], pcb, ones_b[:], ones_bb, mskh[:], mskhb)
                E.tt("dve", base[:], baseb, base[:], baseb, pc[:, 0:NE], pcb, ADD)
        k.barrier()
        k.sec(None)
        with ExitStack() as st:
            nbk, nbkb = k.sb(st, [128, NE], F32, "nbk")
            pend, pendb = k.sb(st, [128, NE], F32, "pend")
            one32, one32b = k.sb(st, [128, NE], F32, "one32")
            blki, blkib = k.sb(st, [128, NBLK], F32, "blki")
            bexf, bexfb = k.sb(st, [128, NBLK], F32, "bexf")
            E.ld(blki[:], blkib, I["blkiota"][:, 0:NBLK])
            E.memset("pool", one32[:], one32b, 1.0)
            E.memset("pool", nbk[:], nbkb, 0.0)
            E.memset("pool", bexf[:], bexfb, 0.0)
            for j in range(-(-NTOK // MB)):
                E.stt(nbk[:], nbkb, base[:], baseb, float(MB * j), nbk[:], nbkb, ALU.is_gt, ADD)
            E.ts("dve", nbk[:], nbkb, nbk[:], nbkb, float(MB), None, MUL)
            E.scan(pend[:], pendb, one32[:], one32b, nbk[:], nbkb, 0.0, None)
            E.tt("dve", pstart[:], pstartb, pend[:], pendb, nbk[:], nbkb, ALU.subtract)
            for e_ in range(NE):
                E.stt(bexf[:], bexfb, blki[:], blkib, pend[:, e_:e_ + 1], bexf[:], bexfb, ALU.is_ge, ADD, xr=[pendb])
            E.ts("dve", bexf[:], bexfb, bexf[:], bexfb, float(NE - 1), None, ALU.min)
            E.cp("dve", bexi[:], bexib, bexf[:], bexfb)
            for kk in range(8):
                E.ts("dve", widx[:, :, kk], widxb, bexf[:], bexfb, cst[:, 1:2], pofs[:, kk:kk + 1], MUL, ADD, xr=[pofsb, cstb])
            E.ts("dve", bidx[:], bidxb, bexf[:], bexfb, cst[:, 2:3], pofs[:, 8:9], MUL, ADD, xr=[pofsb, cstb])
            E.ts("dve", oidx[:], oidxb, bexf[:], bexfb, float(l * NE), None, ADD)
        k.barrier()
        k.sec('moeB')
        with ExitStack() as st:
            Dp, Dpb = k.sb(st, [128, NE], F32, "Dp")
            oh, ohb = k.sb(st, [128, NE], F32, "oh")
            jk, jkb = k.sb(st, [128, NE], F32, "jk")
            dfs = [k.sb(st, [128, 4], F32, f"df{i}") for i in range(2)]
            hts = [k.sb(st, [128, D], BF16, f"ht{i}") for i in range(3)]
            for i in range(NT):
                df, dfb = dfs[i % 2]
                ht, htb = hts[i % 3]
                E.ld(ht[:], htb, H2[i * 128:(i + 1) * 128, :], H2b[i])
                E.tt("dve", Dp[:], Dpb, Dall[:, i, :], Dallb, pstart[:], pstartb, ADD)
                for k4 in range(4):
                    E.ts("dve", oh[:], ohb, iota[:], iotab, IDX[:, i, k4:k4 + 1], None, ALU.is_equal, xr=[IDXb])
                    E.tt("dve", jk[:], jkb, oh[:], ohb, Dp[:], Dpb, MUL)
                    k.op("dve", lambda e, k4=k4, df=df: e.tensor_reduce(out=df[:, k4:k4 + 1], in_=jk[:], axis=mybir.AxisListType.X, op=ADD), r=[jkb], w=[dfb])
                E.cp("dve", dest[:, i, :], destb, df[:], dfb)
                for k4 in range(4):
                    it_, itb_ = stage_idx(dest[:, i, k4:k4 + 1], destb)
                    k.dma("pool", lambda e, it_=it_, ht=ht: e.indirect_dma_start(out=Xg[:, :], out_offset=bass.IndirectOffsetOnAxis(ap=it_[:, 0:1], axis=0), in_=ht[:], in_offset=None),
                          r=[htb, itb_], w=[Xgb], own=htb, multi=True)
        if l == 0:
            dmp = _DUMP[0]
            dmp("dest", dest[:], destb, [128, NT, 4], I32)
            dmp("Wall", Wall[:], Wallb, [128, NT, 4], F32)
            dmp("IDX", IDX[:], IDXb, [128, NT, 4], F32)
            dmp("Dall", Dall[:], Dallb, [128, NT, NE], F32)
            dmp("bexi", bexi[:], bexib, [128, NBLK], I32)
            dmp("widx", widx[:], widxb, [128, NBLK, 8], I32)
            dmp("pstart", pstart[:], pstartb, [128, NE], F32)
            dmp("base", base[:], baseb, [128, NE], F32)
        k.barrier()
        k.sec('moeC')
        with ExitStack() as st:
            wins = [k.sb(st, [128, 8, 2 * D], BF16, f"win{i}") for i in range(2)]
            wouts = [k.sb(st, [128, 8, D], BF16, f"wout{i}") for i in range(2)]
            xrs = [k.sb(st, [128, 4, D], BF16, f"xr{i}") for i in range(2)]
            xTs = [k.sb(st, [128, 8, 512], BF16, f"xT{i}") for i in range(2)]
            aTs = [k.sb(st, [128, 8, 512], BF16, f"aT{i}") for i in range(2)]
            bins = [k.sb(st, [128, 16], F32, f"bin{i}") for i in range(2)]
            bouts = [k.sb(st, [128, D], F32, f"bout{i}") for i in range(2)]
            ysbs = [k.sb(st, [128, D], F32, f"ysb{i}") for i in range(2)]
            tgs = [k.sb(st, [128, 512], F32, f"tg{i}") for i in range(2)]
            tss = [k.sb(st, [128, 512], F32, f"tsg{i}") for i in range(2)]
            tus = [k.sb(st, [128, 512], F32, f"tu{i}") for i in range(2)]
            ny = 0
            WIN = I["exp_w_in"].rearrange("l e k n -> (l e k) n")
            WOUT = I["exp_w_out"].rearrange("l e k n -> (l e k) n")
            BIN = I["exp_b_inT"].rearrange("l e p c -> (l e p) c")
            BOUT = I["exp_b_out"].rearrange("l e o d -> (l e o) d")
            for j in range(NBLK):
                win, winb = wins[j % 2]
                wout, woutb = wouts[j % 2]
                bin_, binb = bins[j % 2]
                bout, boutb = bouts[j % 2]

                for kk in range(8):
                    it_, itb_ = stage_idx(widx[:, j, kk:kk + 1], widxb)
                    k.dma("pool", lambda e, kk=kk, win=win, it_=it_: e.indirect_dma_start(out=win[:, kk, :], out_offset=None, in_=WIN[:, :], in_offset=bass.IndirectOffsetOnAxis(ap=it_[:, 0:1], axis=0)),
                          r=[itb_, inb], w=[winb], multi=True)
                    k.dma("pool", lambda e, kk=kk, wout=wout, it_=it_: e.indirect_dma_start(out=wout[:, kk, :], out_offset=None, in_=WOUT[:, :], in_offset=bass.IndirectOffsetOnAxis(ap=it_[:, 0:1], axis=0)),
                          r=[itb_, inb], w=[woutb], multi=True)
                it_, itb_ = stage_idx(bidx[:, j:j + 1], bidxb)
                k.dma("pool", lambda e, bin_=bin_, it_=it_: e.indirect_dma_start(out=bin_[:], out_offset=None, in_=BIN[:, :], in_offset=bass.IndirectOffsetOnAxis(ap=it_[:, 0:1], axis=0)),
                      r=[itb_, inb], w=[binb])
                it_, itb_ = stage_idx(oidx[:, j:j + 1], oidxb)
                k.dma("pool", lambda e, bout=bout, it_=it_: e.indirect_dma_start(out=bout[:], out_offset=None, in_=BOUT[:, :], in_offset=bass.IndirectOffsetOnAxis(ap=it_[:, 0:1], axis=0)),
                      r=[itb_, inb], w=[boutb])
                for hl in range(MB // 512):
                    r0 = j * MB + hl * 512
                    xr_, xrb = xrs[(j * (MB // 512) + hl) % 2]
                    xT, xTb = xTs[(j * (MB // 512) + hl) % 2]
                    aT, aTb = aTs[(j * (MB // 512) + hl) % 2]
                    E.ld(xr_[:], xrb, Xg[r0:r0 + 512, :].rearrange("(t p) d -> p t d", p=128), Xgb)
                    for kk in range(8):
                        p_, pb_ = bank()
                        pv = p_[:].bitcast(BF16)
                        for tt in range(4):
                            E.tr(pv[:, tt * 128:(tt + 1) * 128], pb_, xr_[:, tt, kk * 128:(kk + 1) * 128], xrb, ident_b[:], ident_bb)
                        E.cp("act" if kk % 2 else "dve", xT[:, kk, :], xTb, pv[:, 0:512], pb_)
                    for fc in range(8):
                        tg, tgb = tgs[fc % 2]
                        tsg, tsgb = tss[fc % 2]
                        tu, tub = tus[fc % 2]
                        pg, pgb = bank()
                        for kk in range(8):
                            E.mm(pg[:, :], pgb, win[:, kk, fc * 128:(fc + 1) * 128], winb, xT[:, kk, :], xTb, start=kk == 0, stop=kk == 7)
                        pu, pub = bank()
                        for kk in range(8):
                            E.mm(pu[:, :], pub, win[:, kk, D + fc * 128:D + (fc + 1) * 128], winb, xT[:, kk, :], xTb, start=kk == 0, stop=kk == 7)
                        E.ts("dve", tg[:], tgb, pg[:, :], pgb, bin_[:, fc:fc + 1], cst[:, 0:1], ADD, ALU.min, xr=[binb, cstb])
                        E.act(tsg[:], tsgb, tg[:], tgb, AF.Silu, scale=1.702)
                        E.ts("dve", tu[:], tub, pu[:, :], pub, bin_[:, 8 + fc:9 + fc], cst[:, 0:1], ADD, ALU.min, xr=[binb, cstb])
                        E.ts("dve", tu[:], tub, tu[:], tub, -7.0, 1.0, ALU.max, ADD)
                        E.stt(aT[:, fc, :], aTb, tu[:], tub, 1.0 / 1.702, tsg[:], tsgb, MUL, MUL)
                    for tt in range(4):
                        ysb, ysbb = ysbs[ny % 2]
                        ny += 1
                        for hf in range(2):
                            py, pyb = bank()
                            for kk in range(8):
                                E.mm(py[:, :], pyb, aT[:, kk, tt * 128:(tt + 1) * 128], aTb, wout[:, kk, hf * 512:(hf + 1) * 512], woutb, start=kk == 0, stop=kk == 7)
                            E.tt("dve", ysb[:, hf * 512:(hf + 1) * 512], ysbb, py[:, :], pyb, bout[:, hf * 512:(hf + 1) * 512], boutb, ADD)
                        for hf in range(2):
                            E.dma("sp", Yg[hf][r0 + tt * 128:r0 + (tt + 1) * 128, :], Ygb, ysb[:, hf * 512:(hf + 1) * 512], ysbb, own=ysbb, multi=True)
        k.barrier()
        k.sec('moeD')
        with ExitStack() as st:
            G2 = []
            for bi in range(NB1):
                g_, gb_ = k.sb(st, [128, D], F32, f"G2_{bi}")
                E.ld(g_[:], gb_, modrows[l, bi:bi + 1, 5 * D:6 * D].partition_broadcast(128), modrows_b)
                G2.append((g_, gb_))
            xts = [k.sb(st, [128, D], F32, f"dxt{i}") for i in range(2)]
            yks = [[k.sb(st, [128, D], F32, f"dy{i}_{q_}") for q_ in range(4)] for i in range(2)]
            ms = [k.sb(st, [128, D], F32, f"dm{i}") for i in range(2)]
            for i, (b, ti) in enumerate(tiles):
                bi = b if ti >= 2 else NB
                xt, xtb = xts[i % 2]
                m_, mb_ = ms[i % 2]
                E.ld(xt[:], xtb, R[b, ti * 128:(ti + 1) * 128, :], Rb[b][ti])
                for k4 in range(4):
                    y_, yb_ = yks[i % 2][k4]
                    it_, itb_ = stage_idx(dest[:, i, k4:k4 + 1], destb)
                    for hf in range(2):
                        k.dma("pool", lambda e, it_=it_, y_=y_, hf=hf: e.indirect_dma_start(out=y_[:, hf * 512:(hf + 1) * 512], out_offset=None, in_=Yg[hf][:, :], in_offset=bass.IndirectOffsetOnAxis(ap=it_[:, 0:1], axis=0)),
                              r=[Ygb, itb_], w=[yb_], multi=True)
                y_, yb_ = yks[i % 2][0]
                E.ts("dve", m_[:], mb_, y_[:], yb_, Wall[:, i, 0:1], None, MUL, xr=[Wallb])
                for k4 in range(1, 4):
                    y_, yb_ = yks[i % 2][k4]
                    E.stt(m_[:], mb_, y_[:], yb_, Wall[:, i, k4:k4 + 1], m_[:], mb_, MUL, ADD, xr=[Wallb])
                E.tt("pool", m_[:], mb_, m_[:], mb_, G2[bi][0][:], G2[bi][1], MUL)
                E.tt("dve", m_[:], mb_, m_[:], mb_, xt[:], xtb, ADD)
                if last:
                    E.dma("sp", out[b, (ti - 2) * 128:(ti - 1) * 128, :], outb[b][ti - 2], m_[:], mb_, own=mb_)
                else:
                    E.dma("sp", R[b, ti * 128:(ti + 1) * 128, :], Rb[b][ti], m_[:], mb_, own=mb_)


_NC_CACHE = {}


def _core_inputs(inp, shared, b0, NB):
    m = dict(shared)
    m["x"] = np.ascontiguousarray(inp["x"][b0:b0 + NB])
    m["ctx"] = np.ascontiguousarray(inp["ctx"][b0:b0 + NB])
    call = np.concatenate([inp["c"][b0:b0 + NB], inp["c_ctx"][None, :]], axis=0)
    m["cT"] = np.ascontiguousarray(np.transpose(call.reshape(NB + 1, 8, 128), (2, 1, 0)))
    return m


def run(inp, NB, n_cores, dbg=(), depth=2):
    inp = {k_: np.asarray(v) for k_, v in inp.items()}
    shared = _prep_shared(inp)
    nblk0 = -(-NB * S * 4 // MB) + NE
    pofs = np.zeros((2, 128, 9), np.float32)
    for l_ in range(2):
        for kk in range(8):
            pofs[l_, :, kk] = l_ * NE * 1024 + kk * 128 + np.arange(128)
        pofs[l_, :, 8] = l_ * NE * 128 + np.arange(128)
    shared["pofs"] = pofs
    shared["blkiota"] = np.broadcast_to((np.arange(nblk0, dtype=np.float32) * MB), (128, nblk0)).copy()
    in_maps = [_core_inputs(inp, shared, c * NB, NB) for c in range(n_cores)]
    specs = {k_: (v.shape, v.dtype) for k_, v in in_maps[0].items()}
    key = (NB, tuple(dbg), depth)
    if key not in _NC_CACHE:
        _NC_CACHE[key] = build(NB, specs, dbg, depth)
    nc = _NC_CACHE[key]
    res = run_bass_kernel_spmd(nc, in_maps, core_ids=list(range(n_cores)))
    return res


def kernel(**inputs):
    n_cores = 8
    NB = inputs["x"].shape[0] // n_cores
    res = run(inputs, NB, n_cores)
    return np.concatenate([r["out"] for r in res.results], axis=0).astype(np.float32)
```
